# Optimizing a Trainium2 kernel written in Bass

```python
import jax, jax.numpy as jnp
from jax import lax
import numpy as np

D_MODEL = 2048
BATCH = 4
SEQ = 8192
DEPTH = 2

CHUNK = 64
N_MIXERS = 2
PLE_DIM = 256
D_FF = 4 * D_MODEL
N_HEADS = 16
HEAD_DIM = 128
N_KV_HEADS = 4
GROUP = N_HEADS // N_KV_HEADS
N_IDX_HEADS = 8
IDX_DIM = 128
TOPK_MAX = 256
KEY_FRAC = 4
Q_BLOCK = 128
ROPE_THETA = 500000.0
ROPE_FRAC = 4
CONV_WIDTH = 31
NORM_EPS = 1e-6

Q_DIM = N_HEADS * HEAD_DIM
KV_DIM = N_KV_HEADS * HEAD_DIM
IDXQ_DIM = N_IDX_HEADS * IDX_DIM
DSA_IN = Q_DIM + 2 * KV_DIM + IDXQ_DIM + IDX_DIM + N_IDX_HEADS
DSA_SPLITS = [Q_DIM, Q_DIM + KV_DIM, Q_DIM + 2 * KV_DIM, Q_DIM + 2 * KV_DIM + IDXQ_DIM,
              Q_DIM + 2 * KV_DIM + IDXQ_DIM + IDX_DIM]

kernel_name = "hybrid_dsa_conformer_chunk_causal"


def rms_norm(x, g):
    x32 = x.astype(jnp.float32)
    y = x32 * lax.rsqrt(jnp.mean(x32 * x32, axis=-1, keepdims=True) + NORM_EPS) * g.astype(jnp.float32)
    return y.astype(x.dtype)


def layer_norm(x, g, b):
    x32 = x.astype(jnp.float32)
    mu = jnp.mean(x32, axis=-1, keepdims=True)
    var = jnp.mean(jnp.square(x32 - mu), axis=-1, keepdims=True)
    y = (x32 - mu) * lax.rsqrt(var + NORM_EPS) * g.astype(jnp.float32) + b.astype(jnp.float32)
    return y.astype(x.dtype)


def partial_rope(x, pos):
    rd = x.shape[-1] // ROPE_FRAC
    half = rd // 2
    inv = ROPE_THETA ** (-jnp.arange(half, dtype=jnp.float32) * 2.0 / rd)
    ang = pos[:, None] * inv[None, :]
    cos = jnp.cos(ang)[:, None, :].astype(x.dtype)
    sin = jnp.sin(ang)[:, None, :].astype(x.dtype)
    x1, x2, rest = x[..., :half], x[..., half:rd], x[..., rd:]
    return jnp.concatenate([x1 * cos - x2 * sin, x2 * cos + x1 * sin, rest], axis=-1)


def dsa_mixer(u, w_in, w_out, pos, topk):
    B, S, _ = u.shape
    proj = u @ w_in
    q, k, v, qi, ki, wi = jnp.split(proj, DSA_SPLITS, axis=-1)
    q = partial_rope(q.reshape(B, S, N_HEADS, HEAD_DIM), pos)
    k = partial_rope(k.reshape(B, S, N_KV_HEADS, HEAD_DIM), pos)
    v = v.reshape(B, S, N_KV_HEADS, HEAD_DIM)
    qi = partial_rope(qi.reshape(B, S, N_IDX_HEADS, IDX_DIM), pos) * (IDX_DIM ** -0.5)
    ki = partial_rope(ki[:, :, None, :], pos)[:, :, 0, :]
    wi = wi * (N_IDX_HEADS ** -0.5)

    nb = S // Q_BLOCK
    qb = q.reshape(B, nb, Q_BLOCK, N_KV_HEADS, GROUP, HEAD_DIM).transpose(1, 0, 2, 3, 4, 5)
    qib = qi.reshape(B, nb, Q_BLOCK, N_IDX_HEADS, IDX_DIM).transpose(1, 0, 2, 3, 4)
    wib = wi.reshape(B, nb, Q_BLOCK, N_IDX_HEADS).transpose(1, 0, 2, 3)
    key_chunk = jnp.arange(S) // CHUNK
    bidx = jnp.arange(B)[:, None, None]
    scale = HEAD_DIM ** -0.5

    def one_block(args):
        bi, q_blk, qi_blk, w_blk = args
        t = bi * Q_BLOCK + jnp.arange(Q_BLOCK)
        q_chunk = t // CHUNK
        rel = jax.nn.relu(jnp.einsum('bqhd,bsd->bqhs', qi_blk, ki).astype(jnp.float32))
        score = jnp.einsum('bqh,bqhs->bqs', w_blk.astype(jnp.float32), rel)
        admissible = key_chunk[None, :] <= q_chunk[:, None]
        score = jnp.where(admissible[None], score, -jnp.inf)
        _, idx = lax.top_k(score, topk)
        valid = (idx // CHUNK) <= q_chunk[None, :, None]
        k_sel = k[bidx, idx]
        v_sel = v[bidx, idx]
        s = jnp.einsum('bqgrd,bqkgd->bqgrk', q_blk, k_sel).astype(jnp.float32) * scale
        s = jnp.where(valid[:, :, None, None, :], s, -jnp.inf)
        prob = jax.nn.softmax(s, axis=-1).astype(v.dtype)
        return jnp.einsum('bqgrk,bqkgd->bqgrd', prob, v_sel)

    o = lax.map(one_block, (jnp.arange(nb), qb, qib, wib))
    o = o.transpose(1, 0, 2, 3, 4, 5).reshape(B, S, Q_DIM)
    return o @ w_out


def conformer_conv(u, w_in, b_in, w_dw, b_dw, ln_g, ln_b, w_out, b_out):
    D = u.shape[-1]
    a, g = jnp.split(u @ w_in + b_in, 2, axis=-1)
    y = a * jax.nn.sigmoid(g)
    y = lax.conv_general_dilated(
        y, w_dw[:, None, :].astype(y.dtype), window_strides=(1,),
        padding=[(CONV_WIDTH - 1, 0)],
        dimension_numbers=('NWC', 'WIO', 'NWC'),
        feature_group_count=D) + b_dw
    y = jax.nn.silu(layer_norm(y, ln_g, ln_b))
    return y @ w_out + b_out


def sqrelu_mlp(u, w1, w2):
    return jnp.square(jax.nn.relu(u @ w1)) @ w2


def setup_inputs(seed: int = 0) -> dict:
    key = jax.random.key(seed)
    ks = jax.random.split(key, 24)
    n_a = (DEPTH + 1) // 2
    n_b = DEPTH // 2
    f32 = jnp.float32

    def nrm(k, shape, scale):
        return jax.random.normal(k, shape, f32) * scale

    def gain(k, shape):
        return 1.0 + 0.01 * jax.random.normal(k, shape, f32)

    return {
        "x": jax.random.normal(ks[0], (BATCH, SEQ, D_MODEL), f32),
        "p": jax.random.normal(ks[1], (DEPTH, BATCH, SEQ, PLE_DIM), f32),
        "mix_norm": gain(ks[2], (DEPTH, D_MODEL)),
        "mlp_norm": gain(ks[3], (DEPTH, D_MODEL)),
        "mlp_w1": nrm(ks[4], (DEPTH, D_MODEL, D_FF), D_MODEL ** -0.5),
        "mlp_w2": nrm(ks[5], (DEPTH, D_FF, D_MODEL), D_FF ** -0.5),
        "pe_proj": nrm(ks[6], (DEPTH, PLE_DIM, D_MODEL), PLE_DIM ** -0.5),
        "pe_gate_norm": gain(ks[7], (DEPTH, D_MODEL)),
        "pe_gate": nrm(ks[8], (DEPTH, D_MODEL, D_MODEL), D_MODEL ** -0.5),
        "dsa_w_in": nrm(ks[9], (n_a, D_MODEL, DSA_IN), D_MODEL ** -0.5),
        "dsa_w_out": nrm(ks[10], (n_a, Q_DIM, D_MODEL), Q_DIM ** -0.5),
        "conv_w_in": nrm(ks[11], (n_b, D_MODEL, 2 * D_MODEL), D_MODEL ** -0.5),
        "conv_b_in": nrm(ks[12], (n_b, 2 * D_MODEL), 0.01),
        "conv_w_dw": nrm(ks[13], (n_b, CONV_WIDTH, D_MODEL), CONV_WIDTH ** -0.5),
        "conv_b_dw": nrm(ks[14], (n_b, D_MODEL), 0.01),
        "conv_ln_g": gain(ks[15], (n_b, D_MODEL)),
        "conv_ln_b": nrm(ks[16], (n_b, D_MODEL), 0.01),
        "conv_w_out": nrm(ks[17], (n_b, D_MODEL, D_MODEL), D_MODEL ** -0.5),
        "conv_b_out": nrm(ks[18], (n_b, D_MODEL), 0.01),
        "final_norm": gain(ks[19], (D_MODEL,)),
    }


def reference(x, p, mix_norm, mlp_norm, mlp_w1, mlp_w2, pe_proj, pe_gate_norm, pe_gate,
              dsa_w_in, dsa_w_out, conv_w_in, conv_b_in, conv_w_dw, conv_b_dw,
              conv_ln_g, conv_ln_b, conv_w_out, conv_b_out, final_norm):
    S = x.shape[1]
    topk = min(TOPK_MAX, S // KEY_FRAC)
    pos = jnp.arange(S, dtype=jnp.float32)
    h = x
    for i in range(DEPTH):
        j = i // N_MIXERS
        u = rms_norm(h, mix_norm[i])
        if i % N_MIXERS == 0:
            h = h + dsa_mixer(u, dsa_w_in[j], dsa_w_out[j], pos, topk)
        else:
            h = h + conformer_conv(u, conv_w_in[j], conv_b_in[j], conv_w_dw[j], conv_b_dw[j],
                                   conv_ln_g[j], conv_ln_b[j], conv_w_out[j], conv_b_out[j])
        h = h + sqrelu_mlp(rms_norm(h, mlp_norm[i]), mlp_w1[i], mlp_w2[i])
        gate = jax.nn.sigmoid(rms_norm(h, pe_gate_norm[i]) @ pe_gate[i])
        h = h + (p[i] @ pe_proj[i]) * gate
    return rms_norm(h, final_norm)
```

```python
import numpy as np
from contextlib import ExitStack
import concourse.bass as bass
import concourse.mybir as mybir
from concourse.bass_utils import run_bass_kernel_spmd

F32 = mybir.dt.float32
BF16 = mybir.dt.bfloat16
ALU = mybir.AluOpType
AF = mybir.ActivationFunctionType
AX = mybir.AxisListType

D = 2048
KC = 16
SEQ = 8192
NB_BIAS = 17
NEG = -1.0e30
EPS = 1e-6
TOPK = 256.0


class Res:
    __slots__ = ("name", "lw", "rd", "sem", "dcount")

    def __init__(self, name):
        self.name = name
        self.lw = None
        self.rd = []
        self.sem = None
        self.dcount = 0


class Op:
    __slots__ = ("eng", "fn", "deps", "inc", "seq", "dma_res", "dma_val", "idx")

    def __init__(self, eng, fn):
        self.eng = eng
        self.fn = fn
        self.idx = 0
        self.deps = {}
        self.inc = False
        self.seq = 0
        self.dma_res = None
        self.dma_val = 0


ENGS = ("pe", "act", "dve", "pool", "sp")


class Sched:
    def __init__(self, nc, stack):
        self.nc = nc
        self.stack = stack
        self.ops = {e: [] for e in ENGS}
        self.esem = {}
        for e in ("pe", "act", "dve", "pool"):
            self.esem[e] = stack.enter_context(nc.semaphore("sem_" + e))
        self.nres = 0
        self.dma_res = []

    def res(self, name=None):
        self.nres += 1
        return Res(name or ("r%d" % self.nres))

    @staticmethod
    def _add(deps, d):
        if d[0] == "op":
            key = d[1].eng
            cur = deps.get(key)
            if cur is None or cur[1].idx < d[1].idx:
                deps[key] = d
        else:
            key = d[1]
            cur = deps.get(key)
            if cur is None or cur[2] < d[2]:
                deps[key] = d

    def _collect(self, op, reads, writes):
        deps = op.deps
        for r in reads:
            if r.lw is not None:
                self._add(deps, r.lw)
        for w in writes:
            if w.lw is not None:
                self._add(deps, w.lw)
            for d in w.rd:
                self._add(deps, d)

    def op(self, eng, fn, reads=(), writes=()):
        o = Op(eng, fn)
        self._collect(o, reads, writes)
        me = ("op", o)
        for r in reads:
            r.rd = [d for d in r.rd if not (d[0] == "op" and d[1].eng == eng)]
            r.rd.append(me)
        for w in writes:
            w.lw = me
            w.rd = []
        o.idx = len(self.ops[eng])
        self.ops[eng].append(o)
        return o

    def dma(self, queue, fn, reads=(), writes=()):
        o = Op(queue, fn)
        self._collect(o, reads, writes)
        pr = writes[0]
        if pr.sem is None:
            pr.sem = self.stack.enter_context(self.nc.semaphore("d_" + pr.name))
            self.dma_res.append(pr)
        pr.dcount += 16
        o.dma_res = pr
        o.dma_val = pr.dcount
        me = ("dma", pr, pr.dcount)
        for r in reads:
            r.rd.append(me)
        for w in writes:
            w.lw = me
            w.rd = []
        o.idx = len(self.ops[queue])
        self.ops[queue].append(o)
        return o

    def wait_all(self, eng, reads):
        o = Op(eng, None)
        self._collect(o, reads, ())
        o.idx = len(self.ops[eng])
        self.ops[eng].append(o)
        return o

    def barrier(self):
        deps = []
        for e in ("pe", "act", "dve", "pool"):
            if self.ops[e]:
                for o in reversed(self.ops[e]):
                    if o.fn is not None and o.dma_res is None:
                        deps.append(("op", o))
                        break
        for r in self.dma_res:
            deps.append(("dma", r, r.dcount))
        for e in ENGS:
            o = Op(e, None)
            for d in deps:
                self._add(o.deps, d)
            o.idx = len(self.ops[e])
            self.ops[e].append(o)

    def finalize(self, block):
        for e in ENGS:
            for o in self.ops[e]:
                for d in o.deps.values():
                    if d[0] == "op":
                        p = d[1]
                        if p.eng == "pe" and o.eng == "pe":
                            continue
                        p.inc = True
        for e in ENGS:
            n = 0
            for o in self.ops[e]:
                if o.inc:
                    n += 1
                    o.seq = n
        stats = {}

        def emit(engname, engobj):
            seen = {}
            nw = 0
            for o in self.ops[engname]:
                for d in o.deps.values():
                    if d[0] == "op":
                        p = d[1]
                        if p.eng == "pe" and engname == "pe":
                            continue
                        sem = self.esem[p.eng]
                        val = p.seq
                        key = p.eng
                    else:
                        sem = d[1].sem
                        val = d[2]
                        key = d[1]
                    if seen.get(key, 0) >= val:
                        continue
                    seen[key] = val
                    engobj.wait_ge(sem, val)
                    nw += 1
                if o.fn is None:
                    continue
                ins = o.fn(engobj)
                if o.dma_res is not None:
                    ins.then_inc(o.dma_res.sem, 16)
                elif o.inc:
                    ins.then_inc(self.esem[engname], 1)
            stats[engname] = (len(self.ops[engname]), nw)

        @block.tensor
        def _(e):
            emit("pe", e)

        @block.scalar
        def _(e):
            emit("act", e)

        @block.vector
        def _(e):
            emit("dve", e)

        @block.gpsimd
        def _(e):
            emit("pool", e)

        @block.sync
        def _(e):
            emit("sp", e)

        return stats


class Cfg:
    def __init__(self, seg_starts=((0, 48), (16, 32)), seg_tiles=16, nkg=16, niter=18):
        self.seg_starts = seg_starts
        self.nseg = len(seg_starts[0])
        self.seg_tiles = seg_tiles
        self.nkg = nkg
        self.niter = niter
        self.tiles = []
        for s in range(self.nseg):
            self.tiles.append((s, -1))
            for i in range(seg_tiles):
                self.tiles.append((s, i))
        self.ntl = len(self.tiles)
        self.nkb = []
        for (s, i) in self.tiles:
            m = max(seg_starts[0][s], seg_starts[1][s])
            self.nkb.append(max(m + i + 1, 1) if i >= 0 else max(m, 1))
        assert max(self.nkb) <= nkg * 4

    def block_of(self, half, t):
        s, i = self.tiles[t]
        st = self.seg_starts[half][s]
        if i >= 0:
            return st + i
        return st - 1 if st > 0 else 0

    def halo_scale(self, half, s):
        return 1.0 if self.seg_starts[half][s] > 0 else 0.0


def small_param_layout():
    lay = {}
    off = 0
    for nm, n in (("g_mix0", 16), ("g_mlp0", 16), ("g_gate0", 16), ("g_mix1", 16), ("g_mlp1", 16),
                  ("g_gate1", 16), ("g_final", 16), ("b_in", 32), ("b_dw", 16), ("ln_g", 16),
                  ("ln_b", 16), ("b_out", 16), ("w_dw", 16 * 31), ("hscale", 2)):
        lay[nm] = (off, n)
        off += n
    return lay, off


def fm(v):
    v = np.asarray(v, dtype=np.float32)
    return np.ascontiguousarray(v.reshape(-1, 128).T)


def build_nc(cfg):
    nc = bass.Bass("TRN2", target_bir_lowering=False)
    NTL = cfg.ntl
    NTOK = NTL * 128
    NOUT = cfg.nseg * cfg.seg_tiles * 128
    NKEY = cfg.nkg * 512
    lay, NSP = small_param_layout()

    def din(name, shape):
        return nc.dram_tensor(name, list(shape), F32, kind="ExternalInput").ap()

    xk = din("xk", [NKEY, D])
    xq = din("xq", [NTOK, D])
    pq = din("pq", [2, NTOK, 256])
    ropek = din("ropek", [2, 32, NKEY])
    ropeq = din("ropeq", [2, 32, NTOK])
    mbias = din("mbias", [2 * cfg.nseg, 128, NB_BIAS * 128])
    c_ident = din("c_ident", [128, 128])
    c_rot = din("c_rot", [32, 32])
    c_sel = din("c_sel", [128, 4, 128])
    c_eq = din("c_eq", [128, 4, 128])
    smallp = din("smallp", [128, NSP])
    w_in = din("dsa_w_in", [D, 4232])
    w_out = din("dsa_w_out", [D, D])
    mlp_w1 = din("mlp_w1", [2, D, 4 * D])
    mlp_w2 = din("mlp_w2", [2, 4 * D, D])
    pe_proj = din("pe_proj", [2, 256, D])
    pe_gate = din("pe_gate", [2, D, D])
    cw_in = din("conv_w_in", [D, 2 * D])
    cw_out = din("conv_w_out", [D, D])
    out = nc.dram_tensor("out", [NOUT, D], F32, kind="ExternalOutput").ap()

    def dscr(name, shape, dt):
        return nc.dram_tensor(name, list(shape), dt, kind="Internal").ap()

    KTs = dscr("KTs", [4, 128, NKEY], BF16)
    kiTs = dscr("kiTs", [128, NKEY], BF16)
    Vs = dscr("Vs", [NKEY, 512], BF16)
    QTs = dscr("QTs", [128, 24, NTOK], BF16)
    WIs = dscr("WIs", [NTOK, 8], F32)
    OTs = dscr("OTs", [128, 16, NTOK], BF16)

    st = ExitStack()
    with st:
        S = Sched(nc, st)
        ARENA = 51500
        arena = st.enter_context(nc.sbuf_tensor("arena", [128, ARENA], F32))
        psf = [st.enter_context(nc.psum_tensor("psf%d" % i, [128, 512], F32)) for i in range(7)]
        psb = st.enter_context(nc.psum_tensor("psb", [128, 1024], BF16))
        psf_res = [S.res("psf%d" % i) for i in range(7)]
        psb_res = [S.res("psbA"), S.res("psbB")]

        class Carver:
            def __init__(self, base):
                self.pos = base

            def f32(self, n):
                a = arena[:, self.pos:self.pos + n]
                self.pos += n
                assert self.pos <= ARENA, self.pos
                return a

            def bf(self, n):
                w = (n + 1) // 2
                a = arena[:, self.pos:self.pos + w].bitcast(BF16)
                self.pos += w
                assert self.pos <= ARENA, self.pos
                return a

        C0 = Carver(0)
        idf = C0.f32(128)
        idb = C0.bf(128)
        rotb = C0.bf(32)
        sel = C0.f32(512).rearrange("p (a b) -> p a b", a=4)
        eqb = C0.bf(512).rearrange("p (a b) -> p a b", a=4)
        sp_t = C0.f32(NSP)
        onesD = C0.bf(128)
        r_const = S.res("const")
        S.dma("sp", lambda e: e.dma_start(out=idf, in_=c_ident[:, :]), writes=[r_const])
        S.dma("sp", lambda e: e.dma_start(out=sel, in_=c_sel[:, :, :]), writes=[r_const])
        S.dma("sp", lambda e: e.dma_start(out=sp_t, in_=smallp[:, :]), writes=[r_const])
        r_constp = S.res("constp")
        S.dma("pool", lambda e: e.dma_start(out=idb, in_=c_ident[:, :]), writes=[r_constp])
        S.dma("pool", lambda e: e.dma_start(out=rotb[0:32, :], in_=c_rot[:, :]), writes=[r_constp])
        S.dma("pool", lambda e: e.dma_start(out=eqb, in_=c_eq[:, :, :]), writes=[r_constp])
        S.op("dve", lambda e: e.memset(onesD, 1.0 / D), writes=[r_const])
        S.barrier()
        PBASE = C0.pos

        def spc(name, j=0, n=1):
            o, _ = lay[name]
            return sp_t[:, o + j:o + j + n]

        class Rot:
            def __init__(self, items):
                self.items = items
                self.i = 0

            def next(self):
                it = self.items[self.i % len(self.items)]
                self.i += 1
                return it

        def mm(ps_ap, lhsT, rhs, start, stop, reads, wres):
            S.op("pe", lambda e: e.matmul(ps_ap, lhsT=lhsT, rhs=rhs, start=start, stop=stop), reads, [wres])

        def tr(ps_ap, in_ap, ident, reads, wres):
            S.op("pe", lambda e: e.transpose(ps_ap, in_ap, ident), reads, [wres])

        def load_xT(src_rows, ntok, kcx, dst, dst_res, xslots, psrot, out_bf=False):
            nsub = ntok // 128
            cnt = 0
            for sub in range(nsub):
                xt, xr = xslots.next()
                xv = xt[:, 0:kcx * 128]
                rows = src_rows[sub * 128:(sub + 1) * 128, :]
                S.dma("sp", (lambda xv=xv, rows=rows: lambda e: e.dma_start(out=xv, in_=rows))(), writes=[xr])
                for k0 in range(0, kcx, 4):
                    nk = min(4, kcx - k0)
                    ps, pr = psrot.next()
                    for j in range(nk):
                        tr(ps[:, j * 128:(j + 1) * 128], xv[:, (k0 + j) * 128:(k0 + j + 1) * 128], idf, [xr, r_const], pr)
                    src = ps[:, 0:nk * 128].rearrange("p (a b) -> p a b", a=nk)
                    dv = dst[:, k0:k0 + nk, sub * 128:(sub + 1) * 128]
                    if cnt % 2 == 0:
                        S.op("act", (lambda dv=dv, src=src: lambda e: e.copy(out=dv, in_=src))(), [pr], [dst_res])
                    else:
                        S.op("dve", (lambda dv=dv, src=src: lambda e: e.tensor_copy(out=dv, in_=src))(), [pr], [dst_res])
                    cnt += 1

        def rmsnorm(hT, h_res, gname, ntok, uT, u_res, tmp1, tmp2, t_res, psrot, out_view=None, out_res=None):
            hv = hT[:, :, 0:ntok]
            uv = uT[:, :, 0:ntok]
            S.op("act", lambda e: e.activation(out=uv, in_=hv, func=AF.Square), [h_res], [u_res])
            ps, pr = psrot.next()
            for kc in range(KC):
                mm(ps[:, 0:ntok], onesD, uT[:, kc, 0:ntok], kc == 0, kc == KC - 1, [u_res, r_const], pr)
            t1 = tmp1[:, 0:ntok]
            t2 = tmp2[:, 0:ntok]
            S.op("act", lambda e: e.activation(out=t1, in_=ps[:, 0:ntok], func=AF.Sqrt, bias=EPS, scale=1.0), [pr], [t_res])
            S.op("dve", lambda e: e.reciprocal(out=t2, in_=t1), [t_res], [t_res])
            ov = out_view if out_view is not None else uT
            ores = out_res if out_res is not None else u_res
            for kc in range(KC):
                g = spc(gname, kc)
                S.op("dve", (lambda kc=kc, g=g: lambda e: e.scalar_tensor_tensor(
                    out=ov[:, kc, 0:ntok], in0=hT[:, kc, 0:ntok], scalar=g, in1=t2, op0=ALU.mult, op1=ALU.mult))(),
                    [h_res, t_res, r_const], [ores])

        conv_res = {}

        def convert(name, src_ap, shape, after=()):
            dstb = dscr(name + "_b", shape, BF16)
            r = S.res("cv_" + name)
            conv_res[name + "_b"] = r
            if len(shape) == 3:
                s2 = src_ap.rearrange("a k o -> (a k) o")
                d2 = dstb.rearrange("a k o -> (a k) o")
                rows = shape[0] * shape[1]
            else:
                s2, d2, rows = src_ap, dstb, shape[0]
            rb = max(128, (2 * 1024 * 1024) // shape[-1])
            for r0 in range(0, rows, rb):
                r1 = min(rows, r0 + rb)
                S.dma("pool", (lambda r0=r0, r1=r1: lambda e: e.dma_start(out=d2[r0:r1, :], in_=s2[r0:r1, :]))(),
                      reads=list(after), writes=[r])
            return dstb

        def load_w(wslots, Wsrc, kcw, col0, ncols):
            wt, wr = wslots.next()
            wv = wt[:, 0:kcw * ncols].rearrange("p (a b) -> p a b", a=kcw)
            src = Wsrc[:, col0:col0 + ncols].rearrange("(kc p) o -> p kc o", p=128)
            S.dma("sp", lambda e: e.dma_start(out=wv, in_=src), reads=[conv_res[Wsrc.name]], writes=[wr])
            return wv, wr

        def linear(in_fn, in_res, Wsrc, kcw, ncols_total, ntok, wslots, psrot, evac, tile_cols=None, col_base=0):
            if tile_cols is None:
                tile_cols = min(ncols_total, 8192 // kcw)
            for c0 in range(0, ncols_total, tile_cols):
                ncols = min(tile_cols, ncols_total - c0)
                wv, wr = load_w(wslots, Wsrc, kcw, col_base + c0, ncols)
                for o in range(ncols // 128):
                    ps, pr = psrot.next()
                    for kc in range(kcw):
                        mm(ps[:, 0:ntok], wv[:, kc, o * 128:(o + 1) * 128], in_fn(kc), kc == 0, kc == kcw - 1,
                           in_res + [wr], pr)
                    evac((c0 // 128) + o, ps, pr)

        def rope(t, t_res, cs, sn, tab_res, ntok, ps, pr, tmpa, tmpb, tmp_res):
            mm(ps[0:32, 0:ntok], rotb[0:32, 0:32], t[0:32, 0:ntok], True, True, [t_res, r_const], pr)
            a = tmpa[0:32, 0:ntok]
            b = tmpb[0:32, 0:ntok]
            S.op("dve", lambda e: e.tensor_tensor(out=a, in0=ps[0:32, 0:ntok], in1=sn[0:32, 0:ntok], op=ALU.mult),
                 [pr, tab_res], [tmp_res])
            S.op("dve", lambda e: e.tensor_tensor(out=b, in0=t[0:32, 0:ntok], in1=cs[0:32, 0:ntok], op=ALU.mult),
                 [t_res, tab_res], [tmp_res])
            S.op("dve", lambda e: e.tensor_tensor(out=t[0:32, 0:ntok], in0=a, in1=b, op=ALU.add), [tmp_res], [t_res])

        C1 = Carver(PBASE)
        hT = C1.f32(KC * 512).rearrange("p (a b) -> p a b", a=KC)
        uT = C1.bf(KC * 512).rearrange("p (a b) -> p a b", a=KC)
        h_res, u_res = S.res("hT"), S.res("uT")
        xslots = Rot([(C1.f32(2048), S.res("xs%d" % i)) for i in range(2)])
        wkv = C1.bf(KC * 1152).rearrange("p (a b) -> p a b", a=KC)
        wkv_res = S.res("wkv")
        wwi = C1.bf(KC * 8).rearrange("p (a b) -> p a b", a=KC)
        wslots = Rot([(C1.bf(8192), S.res("ws%d" % i)) for i in range(2)])
        tmp1, tmp2 = C1.f32(512), C1.f32(512)
        t_res = S.res("ntmp")
        rtA, rtB = C1.f32(512), C1.f32(512)
        rt_res = S.res("rtmp")
        tabs = Rot([(C1.f32(1024), S.res("tab%d" % i)) for i in range(2)])
        kt_slots = [(C1.bf(512), S.res("kt%d" % i)) for i in range(3)]
        vt_slots = [(C1.bf(512), S.res("vt%d" % i)) for i in range(2)]
        wi_slots = [(C1.f32(8), S.res("wit%d" % i)) for i in range(2)]
        kt_store = [S.res("kst%d" % i) for i in range(3)]
        vt_store = [S.res("vst%d" % i) for i in range(2)]
        wi_store = [S.res("wst%d" % i) for i in range(2)]
        psrot = Rot([(psf[i], psf_res[i]) for i in range(7)])

        for (c0, n, dcol) in ((2048, 1024, 0), (4096, 128, 1024)):
            src = w_in[:, c0:c0 + n].rearrange("(kc p) o -> p kc o", p=128)
            dv = wkv[:, :, dcol:dcol + n]
            S.dma("pool", (lambda dv=dv, src=src: lambda e: e.dma_start(out=dv, in_=src))(), writes=[wkv_res])
        wwi_src = w_in[:, 4224:4232].rearrange("(kc p) o -> p kc o", p=128)
        S.dma("pool", lambda e: e.dma_start(out=wwi, in_=wwi_src), writes=[wkv_res])

        w_in = convert("w_in", w_in, [D, 4232])

        kt_i = [0]
        vt_i = [0]
        wi_i = [0]

        def proj_store(ps, pr, ntok, cs, sn, tab_res, dst_ap, do_rope=True):
            i = kt_i[0] % 3
            kt_i[0] += 1
            kt, kr = kt_slots[i]
            ktv = kt[:, 0:ntok]
            S.op("act", lambda e: e.copy(out=ktv, in_=ps[:, 0:ntok]), [pr], [kr])
            if do_rope:
                ps2, pr2 = psrot.next()
                rope(kt, kr, cs, sn, tab_res, ntok, ps2, pr2, rtA, rtB, rt_res)
            S.dma("sp", lambda e: e.dma_start(out=dst_ap, in_=ktv), reads=[kr], writes=[kt_store[i]])

        def load_tabs(src, tok0, ntok):
            tb, tr_ = tabs.next()
            cs = tb[:, 0:512]
            sn = tb[:, 512:1024]
            S.dma("sp", lambda e: e.dma_start(out=cs[0:32, 0:ntok], in_=src[0, :, tok0:tok0 + ntok]), writes=[tr_])
            S.dma("sp", lambda e: e.dma_start(out=sn[0:32, 0:ntok], in_=src[1, :, tok0:tok0 + ntok]), writes=[tr_])
            return cs, sn, tr_

        for kg in range(cfg.nkg):
            tok0 = kg * 512
            load_xT(xk[tok0:tok0 + 512, :], 512, KC, hT, h_res, xslots, psrot)
            rmsnorm(hT, h_res, "g_mix0", 512, uT, u_res, tmp1, tmp2, t_res, psrot)
            cs, sn, tab_res = load_tabs(ropek, tok0, 512)
            for oc in range(5):
                ps, pr = psrot.next()
                col = oc * 128 if oc < 4 else 1024
                for kc in range(KC):
                    mm(ps[:, 0:512], wkv[:, kc, col:col + 128], uT[:, kc, 0:512], kc == 0, kc == KC - 1,
                       [u_res, wkv_res], pr)
                dst = KTs[oc, :, tok0:tok0 + 512] if oc < 4 else kiTs[:, tok0:tok0 + 512]
                proj_store(ps, pr, 512, cs, sn, tab_res, dst)
            for sub in range(4):
                ps, pr = psrot.next()
                for kc in range(KC):
                    mm(ps[:, 0:512], uT[:, kc, sub * 128:(sub + 1) * 128], wkv[:, kc, 512:1024], kc == 0, kc == KC - 1,
                       [u_res, wkv_res], pr)
                i = vt_i[0] % 2
                vt_i[0] += 1
                vt, vr = vt_slots[i]
                S.op("act", (lambda vt=vt, ps=ps: lambda e: e.copy(out=vt, in_=ps[:, 0:512]))(), [pr], [vr])
                dst = Vs[tok0 + sub * 128:tok0 + (sub + 1) * 128, :]
                S.dma("sp", (lambda dst=dst, vt=vt: lambda e: e.dma_start(out=dst, in_=vt))(), reads=[vr],
                      writes=[vt_store[i]])

        w_out = convert("w_out", w_out, [D, D], after=[h_res])
        mlp_w1 = convert("mlp_w1", mlp_w1, [2, D, 4 * D], after=[h_res])
        mlp_w2 = convert("mlp_w2", mlp_w2, [2, 4 * D, D], after=[h_res])
        pe_gate = convert("pe_gate", pe_gate, [2, D, D], after=[h_res])
        pe_proj = convert("pe_proj", pe_proj, [2, 256, D], after=[h_res])
        cw_in = convert("cw_in", cw_in, [D, 2 * D], after=[h_res])
        cw_out = convert("cw_out", cw_out, [D, D], after=[h_res])

        groups = []
        t = 0
        while t < NTL:
            s, i = cfg.tiles[t]
            if i < 0:
                groups.append((t, 1))
                t += 1
            else:
                groups.append((t, 4))
                t += 4
        for (t0, ntl) in groups:
            ntok = ntl * 128
            tok0 = t0 * 128
            load_xT(xq[tok0:tok0 + ntok, :], ntok, KC, hT, h_res, xslots, psrot)
            rmsnorm(hT, h_res, "g_mix0", ntok, uT, u_res, tmp1, tmp2, t_res, psrot)
            cs, sn, tab_res = load_tabs(ropeq, tok0, ntok)

            def q_evac(base):
                def ev(oc, ps, pr):
                    proj_store(ps, pr, ntok, cs, sn, tab_res, QTs[:, base + oc, tok0:tok0 + ntok])
                return ev
            inq = lambda kc: uT[:, kc, 0:ntok]
            linear(inq, [u_res], w_in, KC, 2048, ntok, wslots, psrot, q_evac(0), col_base=0)
            linear(inq, [u_res], w_in, KC, 1024, ntok, wslots, psrot, q_evac(16), col_base=3072)
            for sub in range(ntl):
                ps, pr = psrot.next()
                for kc in range(KC):
                    mm(ps[:, 0:8], uT[:, kc, sub * 128:(sub + 1) * 128], wwi[:, kc, 0:8], kc == 0, kc == KC - 1,
                       [u_res, wkv_res], pr)
                i = wi_i[0] % 2
                wi_i[0] += 1
                wt_, wr_ = wi_slots[i]
                S.op("act", (lambda wt_=wt_, ps=ps: lambda e: e.copy(out=wt_, in_=ps[:, 0:8]))(), [pr], [wr_])
                dst = WIs[tok0 + sub * 128:tok0 + (sub + 1) * 128, :]
                S.dma("sp", (lambda dst=dst, wt_=wt_: lambda e: e.dma_start(out=dst, in_=wt_))(), reads=[wr_],
                      writes=[wi_store[i]])
        S.barrier()

        C2 = Carver(PBASE)
        score = C2.f32(SEQ)
        maskq = C2.bf(SEQ)
        score_b = C2.bf(SEQ)
        sb_res = S.res("score_b")
        maskT = C2.bf(SEQ)
        mb_t = C2.f32(2 * cfg.nseg * NB_BIAS * 128).rearrange("p (a b) -> p a b", a=2 * cfg.nseg)
        s_res, mq_res, mt_res, mb_res = S.res("score"), S.res("maskq"), S.res("maskT"), S.res("mb")
        q_slots = Rot([(C2.bf(24 * 128).rearrange("p (a b) -> p a b", a=24), C2.f32(8), S.res("qs%d" % i)) for i in range(2)])
        ki_slots = Rot([(C2.bf(512), S.res("kis%d" % i)) for i in range(3)])
        kv_slots = Rot([(C2.bf(2048).rearrange("p (a b) -> p a b", a=4), C2.bf(2048).rearrange("p (a b) -> p a b", a=4),
                         S.res("kvs%d" % i)) for i in range(3)])
        r_slots = Rot([(C2.f32(512), S.res("rs%d" % i)) for i in range(3)])
        p_slots = Rot([(C2.bf(512), S.res("pt%d" % i)) for i in range(4)])
        sm = C2.f32(64)
        sm_res = S.res("sm")
        rden = C2.f32(512)
        rbc = C2.f32(512)
        rd_res, rb_res = S.res("rden"), S.res("rbc")
        o_slots = [(C2.bf(512), S.res("ot%d" % i)) for i in range(2)]
        o_store = [S.res("ost%d" % i) for i in range(2)]
        o_i = [0]
        WABS, WSGN, AMAX, RNG, LO, MID, CNT, DD, ZERO, KTH = 0, 8, 16, 17, 18, 19, 20, 21, 22, 23
        S.op("dve", lambda e: e.memset(sm[:, ZERO:ZERO + 1], 0.0), [], [sm_res])
        S.op("dve", lambda e: e.memset(sm[:, KTH:KTH + 1], TOPK - 0.5), [sm_res], [sm_res])

        for pi in range(2 * cfg.nseg):
            S.dma("sp", (lambda pi=pi: lambda e: e.dma_start(out=mb_t[:, pi, :], in_=mbias[pi, :, :]))(), writes=[mb_res])

        po = [(psf[i], psf_res[i]) for i in range(4)]
        pd, pd_res = psf[4], psf_res[4]
        ps_s = Rot([(psf[5], psf_res[5]), (psf[6], psf_res[6])])
        ps_all = Rot([(psf[i], psf_res[i]) for i in range(7)])
        scale = 128.0 ** -0.5

        def attn_tile(t):
            s, ti = cfg.tiles[t]
            n = cfg.nkb[t]
            ncol = n * 128
            pat = 2 * s + (0 if ti < 0 else 1)
            qT, wi_t, q_res = q_slots.next()
            S.dma("sp", (lambda qT=qT, t=t: lambda e: e.dma_start(out=qT, in_=QTs[:, :, t * 128:(t + 1) * 128]))(),
                  writes=[q_res])
            S.dma("sp", (lambda wi_t=wi_t, t=t: lambda e: e.dma_start(out=wi_t, in_=WIs[t * 128:(t + 1) * 128, :]))(),
                  writes=[q_res])
            S.op("dve", (lambda wi_t=wi_t: lambda e: e.tensor_scalar(out=sm[:, 24:32], in0=wi_t, scalar1=-1.0,
                 scalar2=None, op0=ALU.mult))(), [q_res], [sm_res])
            S.op("dve", (lambda wi_t=wi_t: lambda e: e.tensor_tensor(out=sm[:, WABS:WABS + 8], in0=wi_t, in1=sm[:, 24:32],
                 op=ALU.max))(), [q_res, sm_res], [sm_res])
            S.op("dve", (lambda wi_t=wi_t: lambda e: e.tensor_scalar(out=sm[:, WSGN:WSGN + 8], in0=wi_t, scalar1=0.0,
                 scalar2=2.0, op0=ALU.is_ge, op1=ALU.mult))(), [q_res], [sm_res])
            S.op("dve", lambda e: e.tensor_scalar(out=sm[:, WSGN:WSGN + 8], in0=sm[:, WSGN:WSGN + 8], scalar1=-1.0,
                 scalar2=None, op0=ALU.add), [sm_res], [sm_res])
            ngrp = (n + 3) // 4
            for kg in range(ngrp):
                nb = min(4, n - kg * 4)
                cols = nb * 128
                kit, kir = ki_slots.next()
                S.dma("sp", (lambda kit=kit, kg=kg, cols=cols: lambda e: e.dma_start(
                    out=kit[:, 0:cols], in_=kiTs[:, kg * 512:kg * 512 + cols]))(), writes=[kir])
                sv = score[:, kg * 512:kg * 512 + cols]
                for h in range(8):
                    ps, pr = ps_all.next()
                    mm(ps[:, 0:cols], qT[:, 16 + h, :], kit[:, 0:cols], True, True, [q_res, kir], pr)
                    rt, rr = r_slots.next()
                    S.op("act", (lambda rt=rt, ps=ps, cols=cols, h=h: lambda e: e.activation(
                        out=rt[:, 0:cols], in_=ps[:, 0:cols], func=AF.Relu, scale=sm[:, WABS + h:WABS + h + 1]))(),
                        [pr, sm_res], [rr])
                    if h == 0:
                        S.op("dve", (lambda rt=rt, sv=sv, cols=cols: lambda e: e.tensor_scalar(
                            out=sv, in0=rt[:, 0:cols], scalar1=sm[:, WSGN:WSGN + 1], scalar2=None, op0=ALU.mult))(),
                            [rr, sm_res], [s_res])
                    else:
                        S.op("dve", (lambda rt=rt, sv=sv, cols=cols, h=h: lambda e: e.scalar_tensor_tensor(
                            out=sv, in0=rt[:, 0:cols], scalar=sm[:, WSGN + h:WSGN + h + 1], in1=sv,
                            op0=ALU.mult, op1=ALU.add))(), [rr, sm_res, s_res], [s_res])
            sc_v = score[:, 0:ncol]
            S.op("dve", lambda e, sc_v=sc_v: e.tensor_reduce(out=sm[:, AMAX:AMAX + 1], in_=sc_v, axis=AX.X, op=ALU.max,
                 apply_absolute_value=True), [s_res], [sm_res])
            S.op("dve", lambda e: e.tensor_scalar(out=sm[:, RNG:RNG + 1], in0=sm[:, AMAX:AMAX + 1], scalar1=2.0,
                 scalar2=None, op0=ALU.mult), [sm_res], [sm_res])
            S.op("dve", lambda e: e.tensor_scalar(out=sm[:, LO:LO + 1], in0=sm[:, AMAX:AMAX + 1], scalar1=-1.0,
                 scalar2=None, op0=ALU.mult), [sm_res], [sm_res])
            nbb = min(n, NB_BIAS)
            bv = score[:, (n - nbb) * 128:ncol]
            mv = mb_t[:, pat, (NB_BIAS - nbb) * 128:NB_BIAS * 128]
            S.op("dve", (lambda bv=bv, mv=mv: lambda e: e.tensor_tensor(out=bv, in0=bv, in1=mv, op=ALU.add))(),
                 [s_res, mb_res], [s_res])
            mq_v = maskq[:, 0:ncol]
            sc_f = sc_v
            sc_v = score_b[:, 0:ncol]
            S.op("act", (lambda sc_f=sc_f, sc_v=sc_v: lambda e: e.copy(out=sc_v, in_=sc_f))(), [s_res], [sb_res])
            for it in range(cfg.niter):
                ci = 0.5 ** (it + 1)
                S.op("dve", (lambda ci=ci: lambda e: e.scalar_tensor_tensor(
                    out=sm[:, MID:MID + 1], in0=sm[:, RNG:RNG + 1], scalar=ci, in1=sm[:, LO:LO + 1],
                    op0=ALU.mult, op1=ALU.add))(), [sm_res], [sm_res])
                S.op("dve", lambda e: e.memset(sm[:, CNT:CNT + 1], 0.0), [sm_res], [sm_res])
                S.op("dve", (lambda mq_v=mq_v, sc_v=sc_v: lambda e: e.tensor_scalar(
                    out=mq_v, in0=sc_v, scalar1=sm[:, MID:MID + 1], scalar2=sm[:, ZERO:ZERO + 1], op0=ALU.is_ge, op1=ALU.add,
                    accum_out=sm[:, CNT:CNT + 1]))(), [sb_res, sm_res, mq_res], [sm_res, mq_res])
                S.op("dve", lambda e: e.tensor_scalar(out=sm[:, DD:DD + 1], in0=sm[:, CNT:CNT + 1], scalar1=sm[:, KTH:KTH + 1],
                     scalar2=sm[:, RNG:RNG + 1], op0=ALU.is_ge, op1=ALU.mult), [sm_res], [sm_res])
                S.op("dve", (lambda ci=ci: lambda e: e.scalar_tensor_tensor(
                    out=sm[:, LO:LO + 1], in0=sm[:, DD:DD + 1], scalar=ci, in1=sm[:, LO:LO + 1],
                    op0=ALU.mult, op1=ALU.add))(), [sm_res], [sm_res])
            S.op("dve", (lambda mq_v=mq_v, sc_v=sc_v: lambda e: e.tensor_scalar(
                out=mq_v, in0=sc_v, scalar1=sm[:, LO:LO + 1], scalar2=None, op0=ALU.is_ge))(),
                [sb_res, sm_res], [mq_res])
            for b0 in range(0, n, 8):
                nb = min(8, n - b0)
                pbv = psb[:, 0:nb * 128]
                pbr = psb_res[0]
                for j in range(nb):
                    tr(psb[:, j * 128:(j + 1) * 128], maskq[:, (b0 + j) * 128:(b0 + j + 1) * 128],
                       idb, [mq_res, r_const], pbr)
                mtv = maskT[:, b0 * 128:(b0 + nb) * 128]
                S.op("act", (lambda mtv=mtv, pbv=pbv: lambda e: e.copy(out=mtv, in_=pbv))(), [pbr], [mt_res])
            state = {"kv": None}

            def emit_qk(blk, g):
                kg, j = blk // 4, blk % 4
                if j == 0 and g == 0:
                    nb = min(4, n - blk)
                    ktt, vtt, kvr = kv_slots.next()
                    S.dma("sp", (lambda ktt=ktt, kg=kg, nb=nb: lambda e: e.dma_start(
                        out=ktt[:, :, 0:nb * 128], in_=KTs[:, :, kg * 512:kg * 512 + nb * 128].rearrange("g d k -> d g k")))(),
                        writes=[kvr])
                    S.dma("sp", (lambda vtt=vtt, kg=kg, nb=nb: lambda e: e.dma_start(
                        out=vtt[:, 0:nb, :], in_=Vs[kg * 512:kg * 512 + nb * 128, :].rearrange("(b p) c -> p b c", p=128)))(),
                        writes=[kvr])
                    state["kv"] = (ktt, vtt, kvr)
                ktt, vtt, kvr = state["kv"]
                mbc = maskT[:, blk * 128:(blk + 1) * 128].unsqueeze(1).to_broadcast([128, 4, 128])
                ps, pr = ps_s.next()
                mm(ps[:, :], ktt[:, g, j * 128:(j + 1) * 128], qT[:, 4 * g:4 * g + 4, :], True, True, [kvr, q_res], pr)
                pt, ptr = p_slots.next()
                S.op("act", (lambda pt=pt, ps=ps: lambda e: e.activation(out=pt, in_=ps[:, :], func=AF.Exp, scale=scale))(),
                     [pr], [ptr])
                pt3 = pt.rearrange("p (a b) -> p a b", a=4)
                S.op("dve", (lambda pt3=pt3, mbc=mbc: lambda e: e.tensor_tensor(out=pt3, in0=pt3, in1=mbc, op=ALU.mult))(),
                     [ptr, mt_res], [ptr])
                return (blk, g, j, vtt, kvr, pt, ptr)

            def emit_pv(blk, g, j, vtt, kvr, pt, ptr):
                mm(po[g][0][:, :], vtt[:, j, g * 128:(g + 1) * 128], pt, blk == 0, blk == n - 1, [kvr, ptr], po[g][1])
                mm(pd[:, :], eqb[:, g, :], pt, blk == 0 and g == 0, blk == n - 1 and g == 3, [ptr, r_const], pd_res)

            pending = None
            for blk in range(n):
                for g in range(4):
                    cur = emit_qk(blk, g)
                    if pending is not None:
                        emit_pv(*pending)
                    pending = cur
            emit_pv(*pending)
            S.op("dve", lambda e: e.reciprocal(out=rden, in_=pd[:, :]), [pd_res], [rd_res])
            for g in range(4):
                ps, pr = ps_s.next()
                mm(ps[:, :], sel[:, g, :], rden, True, True, [rd_res, r_const], pr)
                S.op("act", (lambda ps=ps: lambda e: e.copy(out=rbc, in_=ps[:, :]))(), [pr], [rb_res])
                i = o_i[0] % 2
                o_i[0] += 1
                ot, orr = o_slots[i]
                S.op("dve", (lambda ot=ot, g=g: lambda e: e.tensor_tensor(out=ot, in0=po[g][0][:, :], in1=rbc, op=ALU.mult))(),
                     [po[g][1], rb_res], [orr])
                dst = OTs[:, 4 * g:4 * g + 4, t * 128:(t + 1) * 128]
                S.dma("sp", (lambda dst=dst, ot=ot: lambda e: e.dma_start(
                    out=dst, in_=ot.rearrange("p (a b) -> p a b", a=4)))(), reads=[orr], writes=[o_store[i]])

        for t in range(NTL):
            attn_tile(t)
        S.barrier()

        C3 = Carver(PBASE)
        hT = C3.f32(KC * 512).rearrange("p (a b) -> p a b", a=KC)
        uT = C3.bf(KC * 512).rearrange("p (a b) -> p a b", a=KC)
        h_res, u_res = S.res("hT3"), S.res("uT3")
        bigw = 32 * 512 // 2
        big_f = C3.f32(bigw)
        big_res = S.res("big")
        hid = big_f.bitcast(BF16).rearrange("p (a b) -> p a b", a=32)
        aT = big_f[:, 0:4096].bitcast(BF16).rearrange("p (a b) -> p a b", a=KC)
        peT = big_f.rearrange("p (a b) -> p a b", a=KC)
        zT = big_f.rearrange("p (a b) -> p a b", a=KC)
        sT = C3.bf(KC * 512).rearrange("p (a b) -> p a b", a=KC)
        s_res3 = S.res("sT")
        pT = C3.bf(2 * 512).rearrange("p (a b) -> p a b", a=2)
        p_res = S.res("pT")
        YW = 30 + 512
        yT = C3.bf(KC * YW).rearrange("p (a b) -> p a b", a=KC)
        y_res = S.res("yT")
        xslots = Rot([(C3.f32(2048), S.res("xs3_%d" % i)) for i in range(2)])
        wslots = Rot([(C3.bf(8192), S.res("ws3_%d" % i)) for i in range(2)])
        tmp1, tmp2 = C3.f32(512), C3.f32(512)
        t_res = S.res("ntmp3")
        e_slots = Rot([(C3.f32(512), S.res("et%d" % i)) for i in range(3)])
        sig4 = C3.f32(4 * 512).rearrange("p (a b) -> p a b", a=4)
        sig_res = S.res("sig4")
        mean_t, rstd_t = C3.f32(512), C3.f32(512)
        ln_res = S.res("ln")
        out_store = [S.res("outst%d" % i) for i in range(2)]
        dg_halves = [(C3.bf(16 * 128).rearrange("p (a b) -> p a b", a=16), S.res("dg%d" % i)) for i in range(2)]
        psrot = Rot([(psf[i], psf_res[i]) for i in range(7)])
        a_store = S.res("aload")

        S.op("dve", lambda e: e.memset(yT[:, :, 0:30], 0.0), [], [y_res])

        def add_res_evac(ntok, bias_name=None):
            def ev(oc, ps, pr):
                if bias_name is None:
                    S.op("dve", lambda e: e.tensor_tensor(out=hT[:, oc, 0:ntok], in0=ps[:, 0:ntok], in1=hT[:, oc, 0:ntok],
                         op=ALU.add), [pr, h_res], [h_res])
                else:
                    b = spc(bias_name, oc)
                    S.op("dve", lambda e: e.scalar_tensor_tensor(out=hT[:, oc, 0:ntok], in0=ps[:, 0:ntok], scalar=b,
                         in1=hT[:, oc, 0:ntok], op0=ALU.add, op1=ALU.add), [pr, h_res, r_const], [h_res])
            return ev

        def mlp(layer, ntok):
            rmsnorm(hT, h_res, "g_mlp%d" % layer, ntok, uT, u_res, tmp1, tmp2, t_res, psrot)
            for half in range(2):
                cnt = [0]

                def hid_evac(oc, ps, pr):
                    et, er = e_slots.next()
                    S.op("act", lambda e: e.activation(out=et[:, 0:ntok], in_=ps[:, 0:ntok], func=AF.Relu), [pr], [er])
                    eng = "dve" if cnt[0] % 2 == 0 else "pool"
                    cnt[0] += 1
                    S.op(eng, lambda e: e.tensor_tensor(out=hid[:, oc, 0:ntok], in0=et[:, 0:ntok], in1=et[:, 0:ntok],
                         op=ALU.mult), [er], [big_res])
                linear(lambda kc: uT[:, kc, 0:ntok], [u_res], mlp_w1[layer], KC, 4096, ntok, wslots, psrot, hid_evac,
                       col_base=half * 4096)
                linear(lambda kc: hid[:, kc, 0:ntok], [big_res], mlp_w2[layer][half * 4096:(half + 1) * 4096, :], 32, D,
                       ntok, wslots, psrot, add_res_evac(ntok))

        def pe_gate_block(layer, ntok, tok0):
            load_xT(pq[layer, tok0:tok0 + ntok, :], ntok, 2, pT, p_res, xslots, psrot)

            def pe_evac(oc, ps, pr):
                S.op("act", lambda e: e.copy(out=peT[:, oc, 0:ntok], in_=ps[:, 0:ntok]), [pr], [big_res])
            linear(lambda kc: pT[:, kc, 0:ntok], [p_res], pe_proj[layer], 2, D, ntok, wslots, psrot, pe_evac)
            rmsnorm(hT, h_res, "g_gate%d" % layer, ntok, uT, u_res, tmp1, tmp2, t_res, psrot)

            def gate_evac(oc, ps, pr):
                et, er = e_slots.next()
                S.op("act", lambda e: e.activation(out=et[:, 0:ntok], in_=ps[:, 0:ntok], func=AF.Sigmoid), [pr], [er])
                S.op("dve", lambda e: e.tensor_tensor(out=et[:, 0:ntok], in0=et[:, 0:ntok], in1=peT[:, oc, 0:ntok],
                     op=ALU.mult), [er, big_res], [er])
                S.op("dve", lambda e: e.tensor_tensor(out=hT[:, oc, 0:ntok], in0=et[:, 0:ntok], in1=hT[:, oc, 0:ntok],
                     op=ALU.add), [er, h_res], [h_res])
            linear(lambda kc: uT[:, kc, 0:ntok], [u_res], pe_gate[layer], KC, D, ntok, wslots, psrot, gate_evac)

        def conv_in_glu(ntok):
            rmsnorm(hT, h_res, "g_mix1", ntok, uT, u_res, tmp1, tmp2, t_res, psrot)
            for tq in range(4):
                def g_evac(oc, ps, pr, tq=tq):
                    j = oc - 4 * tq
                    b = spc("b_in", 16 + oc)
                    S.op("act", lambda e: e.activation(out=sig4[:, j, 0:ntok], in_=ps[:, 0:ntok], func=AF.Sigmoid, bias=b),
                         [pr, r_const], [sig_res])
                linear(lambda kc: uT[:, kc, 0:ntok], [u_res], cw_in, KC, 512, ntok, wslots, psrot,
                       lambda oc, ps, pr, tq=tq: g_evac(oc + 4 * tq, ps, pr), col_base=2048 + tq * 512)

                def a_evac(oc, ps, pr, tq=tq):
                    j = oc - 4 * tq
                    b = spc("b_in", oc)
                    S.op("dve", lambda e: e.scalar_tensor_tensor(out=yT[:, oc, 30:30 + ntok], in0=ps[:, 0:ntok], scalar=b,
                         in1=sig4[:, j, 0:ntok], op0=ALU.add, op1=ALU.mult), [pr, sig_res, r_const], [y_res])
                linear(lambda kc: uT[:, kc, 0:ntok], [u_res], cw_in, KC, 512, ntok, wslots, psrot,
                       lambda oc, ps, pr, tq=tq: a_evac(oc + 4 * tq, ps, pr), col_base=tq * 512)

        def layer0_dense(t0, ntl):
            ntok = ntl * 128
            tok0 = t0 * 128
            load_xT(xq[tok0:tok0 + ntok, :], ntok, KC, hT, h_res, xslots, psrot)
            S.dma("sp", lambda e: e.dma_start(out=aT[:, :, 0:ntok], in_=OTs[:, :, tok0:tok0 + ntok]), writes=[big_res])
            linear(lambda kc: aT[:, kc, 0:ntok], [big_res], w_out, KC, D, ntok, wslots, psrot, add_res_evac(ntok))
            mlp(0, ntok)
            pe_gate_block(0, ntok, tok0)

        otile_i = [0]

        def dense_group(t0, ntl):
            s, i0 = cfg.tiles[t0]
            ntok = ntl * 128
            tok0 = t0 * 128
            layer0_dense(t0, ntl)
            conv_in_glu(ntok)
            if i0 < 0:
                hs = spc("hscale", s)
                S.op("dve", lambda e, hs=hs: e.tensor_scalar(out=yT[:, :, 0:30], in0=yT[:, :, 128:158], scalar1=hs,
                     scalar2=None, op0=ALU.mult), [y_res, r_const], [y_res])
                return
            for c in range(KC):
                ps, pr = psrot.next()
                for hf in range(2):
                    dg, dgr = dg_halves[hf]
                    j0, j1 = (0, 16) if hf == 0 else (16, 31)
                    for j in range(j0, j1):
                        wj = spc("w_dw", c * 31 + j)
                        S.op("pool", lambda e, dg=dg, j=j, j0=j0, wj=wj: e.tensor_scalar(out=dg[:, j - j0, :], in0=idb, scalar1=wj,
                             scalar2=None, op0=ALU.mult), [r_const], [dgr])
                    for j in range(j0, j1):
                        mm(ps[:, 0:ntok], dg[:, j - j0, :], yT[:, c, j:j + ntok], j == 0, j == 30, [dgr, y_res], pr)
                bdw = spc("b_dw", c)
                S.op("dve", lambda e, c=c, ps=ps, bdw=bdw: e.tensor_scalar(out=zT[:, c, 0:ntok], in0=ps[:, 0:ntok], scalar1=bdw,
                     scalar2=None, op0=ALU.add), [pr, r_const, big_res], [big_res])
            S.op("dve", lambda e: e.tensor_copy(out=yT[:, :, 0:30], in_=yT[:, :, ntok:ntok + 30]), [y_res], [y_res])
            S.op("act", lambda e: e.copy(out=sT[:, :, 0:ntok], in_=zT[:, :, 0:ntok]), [big_res], [s_res3])
            S.op("act", lambda e: e.activation(out=uT[:, :, 0:ntok], in_=zT[:, :, 0:ntok], func=AF.Square), [big_res], [u_res])
            ps, pr = psrot.next()
            for kc in range(KC):
                mm(ps[:, 0:ntok], onesD, sT[:, kc, 0:ntok], kc == 0, kc == KC - 1, [s_res3, r_const], pr)
            ps2, pr2 = psrot.next()
            for kc in range(KC):
                mm(ps2[:, 0:ntok], onesD, uT[:, kc, 0:ntok], kc == 0, kc == KC - 1, [u_res, r_const], pr2)
            S.op("act", lambda e: e.copy(out=mean_t[:, 0:ntok], in_=ps[:, 0:ntok]), [pr], [ln_res])
            S.op("dve", lambda e: e.tensor_tensor(out=tmp1[:, 0:ntok], in0=mean_t[:, 0:ntok], in1=mean_t[:, 0:ntok], op=ALU.mult),
                 [ln_res], [t_res])
            S.op("dve", lambda e: e.tensor_tensor(out=tmp1[:, 0:ntok], in0=ps2[:, 0:ntok], in1=tmp1[:, 0:ntok], op=ALU.subtract),
                 [pr2, t_res], [t_res])
            S.op("dve", lambda e: e.tensor_scalar(out=tmp1[:, 0:ntok], in0=tmp1[:, 0:ntok], scalar1=0.0, scalar2=None,
                 op0=ALU.max), [t_res], [t_res])
            S.op("act", lambda e: e.activation(out=tmp2[:, 0:ntok], in_=tmp1[:, 0:ntok], func=AF.Sqrt, bias=EPS, scale=1.0),
                 [t_res], [t_res])
            S.op("dve", lambda e: e.reciprocal(out=rstd_t[:, 0:ntok], in_=tmp2[:, 0:ntok]), [t_res], [ln_res])
            for c in range(KC):
                et, er = e_slots.next()
                S.op("dve", lambda e, c=c, et=et: e.tensor_tensor(out=et[:, 0:ntok], in0=zT[:, c, 0:ntok], in1=mean_t[:, 0:ntok],
                     op=ALU.subtract), [big_res, ln_res], [er])
                S.op("dve", lambda e, et=et: e.tensor_tensor(out=et[:, 0:ntok], in0=et[:, 0:ntok], in1=rstd_t[:, 0:ntok],
                     op=ALU.mult), [er, ln_res], [er])
                lg, lb = spc("ln_g", c), spc("ln_b", c)
                S.op("act", lambda e, c=c, et=et, lg=lg, lb=lb: e.activation(out=sT[:, c, 0:ntok], in_=et[:, 0:ntok],
                     func=AF.Silu, bias=lb, scale=lg), [er, r_const], [s_res3])
            linear(lambda kc: sT[:, kc, 0:ntok], [s_res3], cw_out, KC, D, ntok, wslots, psrot, add_res_evac(ntok, "b_out"))
            mlp(1, ntok)
            pe_gate_block(1, ntok, tok0)
            outT = peT
            rmsnorm(hT, h_res, "g_final", ntok, uT, u_res, tmp1, tmp2, t_res, psrot, out_view=outT, out_res=big_res)
            seg_out_tile0 = s * cfg.seg_tiles + i0
            for sub in range(ntl):
                xt, xr = xslots.next()
                for d0 in range(0, KC, 4):
                    ps, pr = psrot.next()
                    for j in range(4):
                        tr(ps[:, j * 128:(j + 1) * 128], outT[:, d0 + j, sub * 128:(sub + 1) * 128], idf, [u_res, big_res, r_const], pr)
                    if (d0 // 4) % 2 == 0:
                        S.op("act", lambda e, xt=xt, ps=ps, d0=d0: e.copy(out=xt[:, d0 * 128:(d0 + 4) * 128], in_=ps[:, :]), [pr], [xr])
                    else:
                        S.op("dve", lambda e, xt=xt, ps=ps, d0=d0: e.tensor_copy(out=xt[:, d0 * 128:(d0 + 4) * 128], in_=ps[:, :]), [pr], [xr])
                row0 = (seg_out_tile0 + sub) * 128
                k = otile_i[0] % 2
                otile_i[0] += 1
                S.dma("sp", lambda e, xt=xt, row0=row0: e.dma_start(out=out[row0:row0 + 128, :], in_=xt), reads=[xr],
                      writes=[out_store[k]])

        for (t0, ntl) in groups:
            dense_group(t0, ntl)
        S.wait_all("sp", out_store)
        S.barrier()
        with nc.Block() as block:
            stats = S.finalize(block)
        build_nc.stats = stats
    return nc


def rope_tables(pos):
    half = 16
    inv = (np.float32(500000.0) ** (-np.arange(half, dtype=np.float32) * np.float32(2.0) / np.float32(32))).astype(np.float32)
    ang = pos.astype(np.float32)[None, :] * inv[:, None]
    cs = np.cos(ang).astype(np.float32)
    sn = np.sin(ang).astype(np.float32)
    return np.stack([np.concatenate([cs, cs], 0), np.concatenate([sn, sn], 0)], 0)


def make_inputs(cfg, inp):
    lay, NSP = small_param_layout()
    x = np.asarray(inp["x"], dtype=np.float32)
    p = np.asarray(inp["p"], dtype=np.float32)
    NKEY = cfg.nkg * 512
    ident = np.eye(128, dtype=np.float32)
    rot = np.zeros((32, 32), np.float32)
    for m in range(16):
        rot[m + 16, m] = -1.0
        rot[m, m + 16] = 1.0
    csel = np.zeros((128, 4, 128), np.float32)
    ceq = np.zeros((128, 4, 128), np.float32)
    for g in range(4):
        csel[32 * g, g, :] = 1.0
        ceq[:, g, 32 * g:32 * g + 32] = 1.0
    ropek = rope_tables(np.arange(NKEY))
    shared = {
        "c_ident": ident, "c_rot": rot, "c_sel": csel, "c_eq": ceq, "ropek": ropek,
        "dsa_w_in": np.ascontiguousarray(inp["dsa_w_in"][0]), "dsa_w_out": np.ascontiguousarray(inp["dsa_w_out"][0]),
        "mlp_w1": np.asarray(inp["mlp_w1"]), "mlp_w2": np.asarray(inp["mlp_w2"]),
        "pe_proj": np.asarray(inp["pe_proj"]), "pe_gate": np.asarray(inp["pe_gate"]),
        "conv_w_in": np.ascontiguousarray(inp["conv_w_in"][0]), "conv_w_out": np.ascontiguousarray(inp["conv_w_out"][0]),
    }
    sp_base = np.zeros((128, NSP), np.float32)

    def put(name, arr):
        o, n = lay[name]
        assert arr.shape == (128, n), (name, arr.shape)
        sp_base[:, o:o + n] = arr
    put("g_mix0", fm(inp["mix_norm"][0])); put("g_mlp0", fm(inp["mlp_norm"][0])); put("g_gate0", fm(inp["pe_gate_norm"][0]))
    put("g_mix1", fm(inp["mix_norm"][1])); put("g_mlp1", fm(inp["mlp_norm"][1])); put("g_gate1", fm(inp["pe_gate_norm"][1]))
    put("g_final", fm(inp["final_norm"]))
    put("b_in", fm(inp["conv_b_in"][0])); put("b_dw", fm(inp["conv_b_dw"][0])); put("ln_g", fm(inp["conv_ln_g"][0]))
    put("ln_b", fm(inp["conv_ln_b"][0])); put("b_out", fm(inp["conv_b_out"][0]))
    wdw = np.asarray(inp["conv_w_dw"][0], np.float32)
    wdw_fm = wdw.T.reshape(16, 128, 31).transpose(1, 0, 2).reshape(128, 16 * 31)
    put("w_dw", np.ascontiguousarray(wdw_fm))
    in_maps = []
    tile_blocks = []
    for c in range(8):
        b, half = c // 2, c % 2
        blocks = [cfg.block_of(half, t) for t in range(cfg.ntl)]
        tile_blocks.append(blocks)
        rows = np.concatenate([np.arange(j * 128, (j + 1) * 128) for j in blocks])
        m = dict(shared)
        m["xk"] = np.ascontiguousarray(x[b, 0:NKEY])
        m["xq"] = np.ascontiguousarray(x[b, rows])
        m["pq"] = np.ascontiguousarray(p[:, b][:, rows])
        m["ropeq"] = np.ascontiguousarray(rope_tables(rows))
        mb = np.zeros((2 * cfg.nseg, 128, NB_BIAS * 128), np.float32)
        for t in range(cfg.ntl):
            s, i = cfg.tiles[t]
            pat = 2 * s + (0 if i < 0 else 1)
            if i > 0:
                continue
            n = cfg.nkb[t]
            j = blocks[t]
            dpos = NB_BIAS - (n - j)
            assert 0 <= dpos < NB_BIAS, (c, t, n, j)
            pm = np.zeros((128, NB_BIAS, 128), np.float32)
            pm[:, dpos + 1:, :] = NEG
            pm[0:64, dpos, 64:128] = NEG
            mb[pat] = pm.reshape(128, NB_BIAS * 128)
        m["mbias"] = mb
        sp = sp_base.copy()
        o, _ = lay["hscale"]
        for s in range(cfg.nseg):
            sp[:, o + s] = cfg.halo_scale(half, s)
        m["smallp"] = sp
        in_maps.append(m)
    return in_maps, tile_blocks


_CFG = None


def kernel(**inputs):
    cfg = _CFG or Cfg()
    nc = build_nc(cfg)
    in_maps, tile_blocks = make_inputs(cfg, inputs)
    res = run_bass_kernel_spmd(nc, in_maps, core_ids=list(range(8)))
    B, S_, D_ = inputs["x"].shape
    outp = np.zeros((B, S_, D_), np.float32)
    for c in range(8):
        b = c // 2
        o = np.asarray(res.results[c]["out"])
        k = 0
        for t in range(cfg.ntl):
            s, i = cfg.tiles[t]
            if i < 0:
                continue
            j = tile_blocks[c][t]
            outp[b, j * 128:(j + 1) * 128, :] = o[k * 128:(k + 1) * 128]
            k += 1
    return outp
```

```python
import numpy as np
from contextlib import ExitStack
import concourse.bass as bass
import concourse.mybir as mybir
from concourse.bass_utils import run_bass_kernel_spmd

F32 = mybir.dt.float32
BF16 = mybir.dt.bfloat16
ALU = mybir.AluOpType
AF = mybir.ActivationFunctionType
AX = mybir.AxisListType

D = 2048
KC = 16
SEQ = 8192
NB_BIAS = 17
NEG = -1.0e30
EPS = 1e-6
TOPK = 256.0


class Res:
    __slots__ = ("name", "lw", "rd", "sem", "dcount")

    def __init__(self, name):
        self.name = name
        self.lw = None
        self.rd = []
        self.sem = None
        self.dcount = 0


class Op:
    __slots__ = ("eng", "fn", "deps", "inc", "seq", "dma_res", "dma_val", "idx")

    def __init__(self, eng, fn):
        self.eng = eng
        self.fn = fn
        self.idx = 0
        self.deps = {}
        self.inc = False
        self.seq = 0
        self.dma_res = None
        self.dma_val = 0


ENGS = ("pe", "act", "dve", "pool", "sp")


class Sched:
    def __init__(self, nc, stack):
        self.nc = nc
        self.stack = stack
        self.ops = {e: [] for e in ENGS}
        self.esem = {}
        for e in ("pe", "act", "dve", "pool"):
            self.esem[e] = stack.enter_context(nc.semaphore("sem_" + e))
        self.nres = 0
        self.dma_res = []

    def res(self, name=None):
        self.nres += 1
        return Res(name or ("r%d" % self.nres))

    @staticmethod
    def _add(deps, d):
        if d[0] == "op":
            key = d[1].eng
            cur = deps.get(key)
            if cur is None or cur[1].idx < d[1].idx:
                deps[key] = d
        else:
            key = d[1]
            cur = deps.get(key)
            if cur is None or cur[2] < d[2]:
                deps[key] = d

    def _collect(self, op, reads, writes):
        deps = op.deps
        for r in reads:
            if r.lw is not None:
                self._add(deps, r.lw)
        for w in writes:
            if w.lw is not None:
                self._add(deps, w.lw)
            for d in w.rd:
                self._add(deps, d)

    def op(self, eng, fn, reads=(), writes=()):
        o = Op(eng, fn)
        self._collect(o, reads, writes)
        me = ("op", o)
        for r in reads:
            r.rd = [d for d in r.rd if not (d[0] == "op" and d[1].eng == eng)]
            r.rd.append(me)
        for w in writes:
            w.lw = me
            w.rd = []
        o.idx = len(self.ops[eng])
        self.ops[eng].append(o)
        return o

    def dma(self, queue, fn, reads=(), writes=()):
        o = Op(queue, fn)
        self._collect(o, reads, writes)
        pr = writes[0]
        if pr.sem is None:
            pr.sem = self.stack.enter_context(self.nc.semaphore("d_" + pr.name))
            self.dma_res.append(pr)
        pr.dcount += 16
        o.dma_res = pr
        o.dma_val = pr.dcount
        me = ("dma", pr, pr.dcount)
        for r in reads:
            r.rd.append(me)
        for w in writes:
            w.lw = me
            w.rd = []
        o.idx = len(self.ops[queue])
        self.ops[queue].append(o)
        return o

    def wait_all(self, eng, reads):
        o = Op(eng, None)
        self._collect(o, reads, ())
        o.idx = len(self.ops[eng])
        self.ops[eng].append(o)
        return o

    def barrier(self):
        deps = []
        for e in ("pe", "act", "dve", "pool"):
            if self.ops[e]:
                for o in reversed(self.ops[e]):
                    if o.fn is not None and o.dma_res is None:
                        deps.append(("op", o))
                        break
        for r in self.dma_res:
            deps.append(("dma", r, r.dcount))
        for e in ENGS:
            o = Op(e, None)
            for d in deps:
                self._add(o.deps, d)
            o.idx = len(self.ops[e])
            self.ops[e].append(o)

    def finalize(self, block):
        for e in ENGS:
            for o in self.ops[e]:
                for d in o.deps.values():
                    if d[0] == "op":
                        p = d[1]
                        if p.eng == "pe" and o.eng == "pe":
                            continue
                        p.inc = True
        for e in ENGS:
            n = 0
            for o in self.ops[e]:
                if o.inc:
                    n += 1
                    o.seq = n
        stats = {}

        def emit(engname, engobj):
            seen = {}
            nw = 0
            for o in self.ops[engname]:
                for d in o.deps.values():
                    if d[0] == "op":
                        p = d[1]
                        if p.eng == "pe" and engname == "pe":
                            continue
                        sem = self.esem[p.eng]
                        val = p.seq
                        key = p.eng
                    else:
                        sem = d[1].sem
                        val = d[2]
                        key = d[1]
                    if seen.get(key, 0) >= val:
                        continue
                    seen[key] = val
                    engobj.wait_ge(sem, val)
                    nw += 1
                if o.fn is None:
                    continue
                ins = o.fn(engobj)
                if o.dma_res is not None:
                    ins.then_inc(o.dma_res.sem, 16)
                elif o.inc:
                    ins.then_inc(self.esem[engname], 1)
            stats[engname] = (len(self.ops[engname]), nw)

        @block.tensor
        def _(e):
            emit("pe", e)

        @block.scalar
        def _(e):
            emit("act", e)

        @block.vector
        def _(e):
            emit("dve", e)

        @block.gpsimd
        def _(e):
            emit("pool", e)

        @block.sync
        def _(e):
            emit("sp", e)

        return stats


class Cfg:
    def __init__(self, seg_starts=((0, 48), (16, 32)), seg_tiles=16, nkg=16, niter=18):
        self.seg_starts = seg_starts
        self.nseg = len(seg_starts[0])
        self.seg_tiles = seg_tiles
        self.nkg = nkg
        self.niter = niter
        self.tiles = []
        for s in range(self.nseg):
            self.tiles.append((s, -1))
            for i in range(seg_tiles):
                self.tiles.append((s, i))
        self.ntl = len(self.tiles)
        self.nkb = []
        for (s, i) in self.tiles:
            m = max(seg_starts[0][s], seg_starts[1][s])
            self.nkb.append(max(m + i + 1, 1) if i >= 0 else max(m, 1))
        assert max(self.nkb) <= nkg * 4

    def block_of(self, half, t):
        s, i = self.tiles[t]
        st = self.seg_starts[half][s]
        if i >= 0:
            return st + i
        return st - 1 if st > 0 else 0

    def halo_scale(self, half, s):
        return 1.0 if self.seg_starts[half][s] > 0 else 0.0


def small_param_layout():
    lay = {}
    off = 0
    for nm, n in (("g_mix0", 16), ("g_mlp0", 16), ("g_gate0", 16), ("g_mix1", 16), ("g_mlp1", 16),
                  ("g_gate1", 16), ("g_final", 16), ("b_in", 32), ("b_dw", 16), ("ln_g", 16),
                  ("ln_b", 16), ("b_out", 16), ("w_dw", 16 * 31), ("hscale", 2)):
        lay[nm] = (off, n)
        off += n
    return lay, off


def fm(v):
    v = np.asarray(v, dtype=np.float32)
    return np.ascontiguousarray(v.reshape(-1, 128).T)


def build_nc(cfg):
    nc = bass.Bass("TRN2", target_bir_lowering=False)
    NTL = cfg.ntl
    NTOK = NTL * 128
    NOUT = cfg.nseg * cfg.seg_tiles * 128
    NKEY = cfg.nkg * 512
    lay, NSP = small_param_layout()

    def din(name, shape):
        return nc.dram_tensor(name, list(shape), F32, kind="ExternalInput").ap()

    xk = din("xk", [NKEY, D])
    xq = din("xq", [NTOK, D])
    pq = din("pq", [2, NTOK, 256])
    ropek = din("ropek", [2, 32, NKEY])
    ropeq = din("ropeq", [2, 32, NTOK])
    mbias = din("mbias", [2 * cfg.nseg, 128, NB_BIAS * 128])
    c_ident = din("c_ident", [128, 128])
    c_rot = din("c_rot", [32, 32])
    c_sel = din("c_sel", [128, 4, 128])
    c_eq = din("c_eq", [128, 4, 128])
    smallp = din("smallp", [128, NSP])
    w_in = din("dsa_w_in", [D, 4232])
    w_out = din("dsa_w_out", [D, D])
    mlp_w1 = din("mlp_w1", [2, D, 4 * D])
    mlp_w2 = din("mlp_w2", [2, 4 * D, D])
    pe_proj = din("pe_proj", [2, 256, D])
    pe_gate = din("pe_gate", [2, D, D])
    cw_in = din("conv_w_in", [D, 2 * D])
    cw_out = din("conv_w_out", [D, D])
    out = nc.dram_tensor("out", [NOUT, D], F32, kind="ExternalOutput").ap()

    def dscr(name, shape, dt):
        return nc.dram_tensor(name, list(shape), dt, kind="Internal").ap()

    KTs = dscr("KTs", [4, 128, NKEY], BF16)
    kiTs = dscr("kiTs", [128, NKEY], BF16)
    Vs = dscr("Vs", [NKEY, 512], BF16)
    QTs = dscr("QTs", [128, 24, NTOK], BF16)
    WIs = dscr("WIs", [NTOK, 8], F32)
    OTs = dscr("OTs", [128, 16, NTOK], BF16)

    st = ExitStack()
    with st:
        S = Sched(nc, st)
        ARENA = 51500
        arena = st.enter_context(nc.sbuf_tensor("arena", [128, ARENA], F32))
        psf = [st.enter_context(nc.psum_tensor("psf%d" % i, [128, 512], F32)) for i in range(7)]
        psb = st.enter_context(nc.psum_tensor("psb", [128, 1024], BF16))
        psf_res = [S.res("psf%d" % i) for i in range(7)]
        psb_res = [S.res("psbA"), S.res("psbB")]

        class Carver:
            def __init__(self, base):
                self.pos = base

            def f32(self, n):
                a = arena[:, self.pos:self.pos + n]
                self.pos += n
                assert self.pos <= ARENA, self.pos
                return a

            def bf(self, n):
                w = (n + 1) // 2
                a = arena[:, self.pos:self.pos + w].bitcast(BF16)
                self.pos += w
                assert self.pos <= ARENA, self.pos
                return a

        C0 = Carver(0)
        idf = C0.f32(128)
        idb = C0.bf(128)
        rotb = C0.bf(32)
        sel = C0.f32(512).rearrange("p (a b) -> p a b", a=4)
        eqb = C0.bf(512).rearrange("p (a b) -> p a b", a=4)
        sp_t = C0.f32(NSP)
        onesD = C0.bf(128)
        r_const = S.res("const")
        S.dma("sp", lambda e: e.dma_start(out=idf, in_=c_ident[:, :]), writes=[r_const])
        S.dma("sp", lambda e: e.dma_start(out=sel, in_=c_sel[:, :, :]), writes=[r_const])
        S.dma("sp", lambda e: e.dma_start(out=sp_t, in_=smallp[:, :]), writes=[r_const])
        r_constp = S.res("constp")
        S.dma("pool", lambda e: e.dma_start(out=idb, in_=c_ident[:, :]), writes=[r_constp])
        S.dma("pool", lambda e: e.dma_start(out=rotb[0:32, :], in_=c_rot[:, :]), writes=[r_constp])
        S.dma("pool", lambda e: e.dma_start(out=eqb, in_=c_eq[:, :, :]), writes=[r_constp])
        S.op("dve", lambda e: e.memset(onesD, 1.0 / D), writes=[r_const])
        S.barrier()
        PBASE = C0.pos

        def spc(name, j=0, n=1):
            o, _ = lay[name]
            return sp_t[:, o + j:o + j + n]

        class Rot:
            def __init__(self, items):
                self.items = items
                self.i = 0

            def next(self):
                it = self.items[self.i % len(self.items)]
                self.i += 1
                return it

        def mm(ps_ap, lhsT, rhs, start, stop, reads, wres):
            S.op("pe", lambda e: e.matmul(ps_ap, lhsT=lhsT, rhs=rhs, start=start, stop=stop), reads, [wres])

        def tr(ps_ap, in_ap, ident, reads, wres):
            S.op("pe", lambda e: e.transpose(ps_ap, in_ap, ident), reads, [wres])

        def load_xT(src_rows, ntok, kcx, dst, dst_res, xslots, psrot, out_bf=False):
            nsub = ntok // 128
            cnt = 0
            for sub in range(nsub):
                xt, xr = xslots.next()
                xv = xt[:, 0:kcx * 128]
                rows = src_rows[sub * 128:(sub + 1) * 128, :]
                S.dma("sp", (lambda xv=xv, rows=rows: lambda e: e.dma_start(out=xv, in_=rows))(), writes=[xr])
                for k0 in range(0, kcx, 4):
                    nk = min(4, kcx - k0)
                    ps, pr = psrot.next()
                    for j in range(nk):
                        tr(ps[:, j * 128:(j + 1) * 128], xv[:, (k0 + j) * 128:(k0 + j + 1) * 128], idf, [xr, r_const], pr)
                    src = ps[:, 0:nk * 128].rearrange("p (a b) -> p a b", a=nk)
                    dv = dst[:, k0:k0 + nk, sub * 128:(sub + 1) * 128]
                    if cnt % 2 == 0:
                        S.op("act", (lambda dv=dv, src=src: lambda e: e.copy(out=dv, in_=src))(), [pr], [dst_res])
                    else:
                        S.op("dve", (lambda dv=dv, src=src: lambda e: e.tensor_copy(out=dv, in_=src))(), [pr], [dst_res])
                    cnt += 1

        def rmsnorm(hT, h_res, gname, ntok, uT, u_res, tmp1, tmp2, t_res, psrot, out_view=None, out_res=None):
            hv = hT[:, :, 0:ntok]
            uv = uT[:, :, 0:ntok]
            S.op("act", lambda e: e.activation(out=uv, in_=hv, func=AF.Square), [h_res], [u_res])
            ps, pr = psrot.next()
            for kc in range(KC):
                mm(ps[:, 0:ntok], onesD, uT[:, kc, 0:ntok], kc == 0, kc == KC - 1, [u_res, r_const], pr)
            t1 = tmp1[:, 0:ntok]
            t2 = tmp2[:, 0:ntok]
            S.op("act", lambda e: e.activation(out=t1, in_=ps[:, 0:ntok], func=AF.Sqrt, bias=EPS, scale=1.0), [pr], [t_res])
            S.op("dve", lambda e: e.reciprocal(out=t2, in_=t1), [t_res], [t_res])
            ov = out_view if out_view is not None else uT
            ores = out_res if out_res is not None else u_res
            for kc in range(KC):
                g = spc(gname, kc)
                S.op("dve", (lambda kc=kc, g=g: lambda e: e.scalar_tensor_tensor(
                    out=ov[:, kc, 0:ntok], in0=hT[:, kc, 0:ntok], scalar=g, in1=t2, op0=ALU.mult, op1=ALU.mult))(),
                    [h_res, t_res, r_const], [ores])

        conv_res = {}

        def convert(name, src_ap, shape, after=()):
            dstb = dscr(name + "_b", shape, BF16)
            r = S.res("cv_" + name)
            conv_res[name + "_b"] = r
            if len(shape) == 3:
                s2 = src_ap.rearrange("a k o -> (a k) o")
                d2 = dstb.rearrange("a k o -> (a k) o")
                rows = shape[0] * shape[1]
            else:
                s2, d2, rows = src_ap, dstb, shape[0]
            rb = max(128, (2 * 1024 * 1024) // shape[-1])
            for r0 in range(0, rows, rb):
                r1 = min(rows, r0 + rb)
                S.dma("pool", (lambda r0=r0, r1=r1: lambda e: e.dma_start(out=d2[r0:r1, :], in_=s2[r0:r1, :]))(),
                      reads=list(after), writes=[r])
            return dstb

        def load_w(wslots, Wsrc, kcw, col0, ncols):
            wt, wr = wslots.next()
            wv = wt[:, 0:kcw * ncols].rearrange("p (a b) -> p a b", a=kcw)
            src = Wsrc[:, col0:col0 + ncols].rearrange("(kc p) o -> p kc o", p=128)
            S.dma("sp", lambda e: e.dma_start(out=wv, in_=src), reads=[conv_res[Wsrc.name]], writes=[wr])
            return wv, wr

        def linear(in_fn, in_res, Wsrc, kcw, ncols_total, ntok, wslots, psrot, evac, tile_cols=None, col_base=0):
            if tile_cols is None:
                tile_cols = min(ncols_total, 8192 // kcw)
            for c0 in range(0, ncols_total, tile_cols):
                ncols = min(tile_cols, ncols_total - c0)
                wv, wr = load_w(wslots, Wsrc, kcw, col_base + c0, ncols)
                for o in range(ncols // 128):
                    ps, pr = psrot.next()
                    for kc in range(kcw):
                        mm(ps[:, 0:ntok], wv[:, kc, o * 128:(o + 1) * 128], in_fn(kc), kc == 0, kc == kcw - 1,
                           in_res + [wr], pr)
                    evac((c0 // 128) + o, ps, pr)

        def rope(t, t_res, cs, sn, tab_res, ntok, ps, pr, tmpa, tmpb, tmp_res):
            mm(ps[0:32, 0:ntok], rotb[0:32, 0:32], t[0:32, 0:ntok], True, True, [t_res, r_const], pr)
            a = tmpa[0:32, 0:ntok]
            b = tmpb[0:32, 0:ntok]
            S.op("dve", lambda e: e.tensor_tensor(out=a, in0=ps[0:32, 0:ntok], in1=sn[0:32, 0:ntok], op=ALU.mult),
                 [pr, tab_res], [tmp_res])
            S.op("dve", lambda e: e.tensor_tensor(out=b, in0=t[0:32, 0:ntok], in1=cs[0:32, 0:ntok], op=ALU.mult),
                 [t_res, tab_res], [tmp_res])
            S.op("dve", lambda e: e.tensor_tensor(out=t[0:32, 0:ntok], in0=a, in1=b, op=ALU.add), [tmp_res], [t_res])

        C1 = Carver(PBASE)
        hT = C1.f32(KC * 512).rearrange("p (a b) -> p a b", a=KC)
        uT = C1.bf(KC * 512).rearrange("p (a b) -> p a b", a=KC)
        h_res, u_res = S.res("hT"), S.res("uT")
        xslots = Rot([(C1.f32(2048), S.res("xs%d" % i)) for i in range(2)])
        wkv = C1.bf(KC * 1152).rearrange("p (a b) -> p a b", a=KC)
        wkv_res = S.res("wkv")
        wwi = C1.bf(KC * 8).rearrange("p (a b) -> p a b", a=KC)
        wslots = Rot([(C1.bf(8192), S.res("ws%d" % i)) for i in range(2)])
        tmp1, tmp2 = C1.f32(512), C1.f32(512)
        t_res = S.res("ntmp")
        rtA, rtB = C1.f32(512), C1.f32(512)
        rt_res = S.res("rtmp")
        tabs = Rot([(C1.f32(1024), S.res("tab%d" % i)) for i in range(2)])
        kt_slots = [(C1.bf(512), S.res("kt%d" % i)) for i in range(3)]
        vt_slots = [(C1.bf(512), S.res("vt%d" % i)) for i in range(2)]
        wi_slots = [(C1.f32(8), S.res("wit%d" % i)) for i in range(2)]
        kt_store = [S.res("kst%d" % i) for i in range(3)]
        vt_store = [S.res("vst%d" % i) for i in range(2)]
        wi_store = [S.res("wst%d" % i) for i in range(2)]
        psrot = Rot([(psf[i], psf_res[i]) for i in range(7)])

        for (c0, n, dcol) in ((2048, 1024, 0), (4096, 128, 1024)):
            src = w_in[:, c0:c0 + n].rearrange("(kc p) o -> p kc o", p=128)
            dv = wkv[:, :, dcol:dcol + n]
            S.dma("pool", (lambda dv=dv, src=src: lambda e: e.dma_start(out=dv, in_=src))(), writes=[wkv_res])
        wwi_src = w_in[:, 4224:4232].rearrange("(kc p) o -> p kc o", p=128)
        S.dma("pool", lambda e: e.dma_start(out=wwi, in_=wwi_src), writes=[wkv_res])

        w_in = convert("w_in", w_in, [D, 4232])

        kt_i = [0]
        vt_i = [0]
        wi_i = [0]

        def proj_store(ps, pr, ntok, cs, sn, tab_res, dst_ap, do_rope=True):
            i = kt_i[0] % 3
            kt_i[0] += 1
            kt, kr = kt_slots[i]
            ktv = kt[:, 0:ntok]
            S.op("act", lambda e: e.copy(out=ktv, in_=ps[:, 0:ntok]), [pr], [kr])
            if do_rope:
                ps2, pr2 = psrot.next()
                rope(kt, kr, cs, sn, tab_res, ntok, ps2, pr2, rtA, rtB, rt_res)
            S.dma("sp", lambda e: e.dma_start(out=dst_ap, in_=ktv), reads=[kr], writes=[kt_store[i]])

        def load_tabs(src, tok0, ntok):
            tb, tr_ = tabs.next()
            cs = tb[:, 0:512]
            sn = tb[:, 512:1024]
            S.dma("sp", lambda e: e.dma_start(out=cs[0:32, 0:ntok], in_=src[0, :, tok0:tok0 + ntok]), writes=[tr_])
            S.dma("sp", lambda e: e.dma_start(out=sn[0:32, 0:ntok], in_=src[1, :, tok0:tok0 + ntok]), writes=[tr_])
            return cs, sn, tr_

        for kg in range(cfg.nkg):
            tok0 = kg * 512
            load_xT(xk[tok0:tok0 + 512, :], 512, KC, hT, h_res, xslots, psrot)
            rmsnorm(hT, h_res, "g_mix0", 512, uT, u_res, tmp1, tmp2, t_res, psrot)
            cs, sn, tab_res = load_tabs(ropek, tok0, 512)
            for oc in range(5):
                ps, pr = psrot.next()
                col = oc * 128 if oc < 4 else 1024
                for kc in range(KC):
                    mm(ps[:, 0:512], wkv[:, kc, col:col + 128], uT[:, kc, 0:512], kc == 0, kc == KC - 1,
                       [u_res, wkv_res], pr)
                dst = KTs[oc, :, tok0:tok0 + 512] if oc < 4 else kiTs[:, tok0:tok0 + 512]
                proj_store(ps, pr, 512, cs, sn, tab_res, dst)
            for sub in range(4):
                ps, pr = psrot.next()
                for kc in range(KC):
                    mm(ps[:, 0:512], uT[:, kc, sub * 128:(sub + 1) * 128], wkv[:, kc, 512:1024], kc == 0, kc == KC - 1,
                       [u_res, wkv_res], pr)
                i = vt_i[0] % 2
                vt_i[0] += 1
                vt, vr = vt_slots[i]
                S.op("act", (lambda vt=vt, ps=ps: lambda e: e.copy(out=vt, in_=ps[:, 0:512]))(), [pr], [vr])
                dst = Vs[tok0 + sub * 128:tok0 + (sub + 1) * 128, :]
                S.dma("sp", (lambda dst=dst, vt=vt: lambda e: e.dma_start(out=dst, in_=vt))(), reads=[vr],
                      writes=[vt_store[i]])

        w_out = convert("w_out", w_out, [D, D], after=[h_res])
        mlp_w1 = convert("mlp_w1", mlp_w1, [2, D, 4 * D], after=[h_res])
        mlp_w2 = convert("mlp_w2", mlp_w2, [2, 4 * D, D], after=[h_res])
        pe_gate = convert("pe_gate", pe_gate, [2, D, D], after=[h_res])
        pe_proj = convert("pe_proj", pe_proj, [2, 256, D], after=[h_res])
        cw_in = convert("cw_in", cw_in, [D, 2 * D], after=[h_res])
        cw_out = convert("cw_out", cw_out, [D, D], after=[h_res])

        groups = []
        t = 0
        while t < NTL:
            s, i = cfg.tiles[t]
            if i < 0:
                groups.append((t, 1))
                t += 1
            else:
                groups.append((t, 4))
                t += 4
        for (t0, ntl) in groups:
            ntok = ntl * 128
            tok0 = t0 * 128
            load_xT(xq[tok0:tok0 + ntok, :], ntok, KC, hT, h_res, xslots, psrot)
            rmsnorm(hT, h_res, "g_mix0", ntok, uT, u_res, tmp1, tmp2, t_res, psrot)
            cs, sn, tab_res = load_tabs(ropeq, tok0, ntok)

            def q_evac(base):
                def ev(oc, ps, pr):
                    proj_store(ps, pr, ntok, cs, sn, tab_res, QTs[:, base + oc, tok0:tok0 + ntok])
                return ev
            inq = lambda kc: uT[:, kc, 0:ntok]
            linear(inq, [u_res], w_in, KC, 2048, ntok, wslots, psrot, q_evac(0), col_base=0)
            linear(inq, [u_res], w_in, KC, 1024, ntok, wslots, psrot, q_evac(16), col_base=3072)
            for sub in range(ntl):
                ps, pr = psrot.next()
                for kc in range(KC):
                    mm(ps[:, 0:8], uT[:, kc, sub * 128:(sub + 1) * 128], wwi[:, kc, 0:8], kc == 0, kc == KC - 1,
                       [u_res, wkv_res], pr)
                i = wi_i[0] % 2
                wi_i[0] += 1
                wt_, wr_ = wi_slots[i]
                S.op("act", (lambda wt_=wt_, ps=ps: lambda e: e.copy(out=wt_, in_=ps[:, 0:8]))(), [pr], [wr_])
                dst = WIs[tok0 + sub * 128:tok0 + (sub + 1) * 128, :]
                S.dma("sp", (lambda dst=dst, wt_=wt_: lambda e: e.dma_start(out=dst, in_=wt_))(), reads=[wr_],
                      writes=[wi_store[i]])
        S.barrier()

        C2 = Carver(PBASE)
        score = C2.f32(SEQ)
        maskq = C2.bf(SEQ)
        score_b = C2.bf(SEQ)
        sb_res = S.res("score_b")
        maskT = C2.bf(SEQ)
        mb_t = C2.f32(2 * cfg.nseg * NB_BIAS * 128).rearrange("p (a b) -> p a b", a=2 * cfg.nseg)
        s_res, mq_res, mt_res, mb_res = S.res("score"), S.res("maskq"), S.res("maskT"), S.res("mb")
        q_slots = Rot([(C2.bf(24 * 128).rearrange("p (a b) -> p a b", a=24), C2.f32(8), S.res("qs%d" % i)) for i in range(2)])
        ki_slots = Rot([(C2.bf(512), S.res("kis%d" % i)) for i in range(3)])
        kv_slots = Rot([(C2.bf(2048).rearrange("p (a b) -> p a b", a=4), C2.bf(2048).rearrange("p (a b) -> p a b", a=4),
                         S.res("kvs%d" % i)) for i in range(3)])
        r_slots = Rot([(C2.f32(512), S.res("rs%d" % i)) for i in range(3)])
        p_slots = Rot([(C2.bf(512), S.res("pt%d" % i)) for i in range(4)])
        sm = C2.f32(64)
        sm_res = S.res("sm")
        rden = C2.f32(512)
        rbc = C2.f32(512)
        rd_res, rb_res = S.res("rden"), S.res("rbc")
        o_slots = [(C2.bf(512), S.res("ot%d" % i)) for i in range(2)]
        o_store = [S.res("ost%d" % i) for i in range(2)]
        o_i = [0]
        WABS, WSGN, AMAX, RNG, LO, MID, CNT, DD, ZERO, KTH = 0, 8, 16, 17, 18, 19, 20, 21, 22, 23
        S.op("dve", lambda e: e.memset(sm[:, ZERO:ZERO + 1], 0.0), [], [sm_res])
        S.op("dve", lambda e: e.memset(sm[:, KTH:KTH + 1], TOPK - 0.5), [sm_res], [sm_res])

        for pi in range(2 * cfg.nseg):
            S.dma("sp", (lambda pi=pi: lambda e: e.dma_start(out=mb_t[:, pi, :], in_=mbias[pi, :, :]))(), writes=[mb_res])

        po = [(psf[i], psf_res[i]) for i in range(4)]
        pd, pd_res = psf[4], psf_res[4]
        ps_s = Rot([(psf[5], psf_res[5]), (psf[6], psf_res[6])])
        ps_all = Rot([(psf[i], psf_res[i]) for i in range(7)])
        scale = 128.0 ** -0.5

        tstate = {}

        def prep_a(t):
            s, ti = cfg.tiles[t]
            n = cfg.nkb[t]
            ncol = n * 128
            pat = 2 * s + (0 if ti < 0 else 1)
            qT, wi_t, q_res = q_slots.next()
            S.dma("sp", (lambda qT=qT, t=t: lambda e: e.dma_start(out=qT, in_=QTs[:, :, t * 128:(t + 1) * 128]))(),
                  writes=[q_res])
            S.dma("sp", (lambda wi_t=wi_t, t=t: lambda e: e.dma_start(out=wi_t, in_=WIs[t * 128:(t + 1) * 128, :]))(),
                  writes=[q_res])
            S.op("dve", (lambda wi_t=wi_t: lambda e: e.tensor_scalar(out=sm[:, 24:32], in0=wi_t, scalar1=-1.0,
                 scalar2=None, op0=ALU.mult))(), [q_res], [sm_res])
            S.op("dve", (lambda wi_t=wi_t: lambda e: e.tensor_tensor(out=sm[:, WABS:WABS + 8], in0=wi_t, in1=sm[:, 24:32],
                 op=ALU.max))(), [q_res, sm_res], [sm_res])
            S.op("dve", (lambda wi_t=wi_t: lambda e: e.tensor_scalar(out=sm[:, WSGN:WSGN + 8], in0=wi_t, scalar1=0.0,
                 scalar2=2.0, op0=ALU.is_ge, op1=ALU.mult))(), [q_res], [sm_res])
            S.op("dve", lambda e: e.tensor_scalar(out=sm[:, WSGN:WSGN + 8], in0=sm[:, WSGN:WSGN + 8], scalar1=-1.0,
                 scalar2=None, op0=ALU.add), [sm_res], [sm_res])
            ngrp = (n + 3) // 4
            for kg in range(ngrp):
                nb = min(4, n - kg * 4)
                cols = nb * 128
                kit, kir = ki_slots.next()
                S.dma("sp", (lambda kit=kit, kg=kg, cols=cols: lambda e: e.dma_start(
                    out=kit[:, 0:cols], in_=kiTs[:, kg * 512:kg * 512 + cols]))(), writes=[kir])
                sv = score[:, kg * 512:kg * 512 + cols]
                for h in range(8):
                    ps, pr = ps_all.next()
                    mm(ps[:, 0:cols], qT[:, 16 + h, :], kit[:, 0:cols], True, True, [q_res, kir], pr)
                    rt, rr = r_slots.next()
                    S.op("act", (lambda rt=rt, ps=ps, cols=cols, h=h: lambda e: e.activation(
                        out=rt[:, 0:cols], in_=ps[:, 0:cols], func=AF.Relu, scale=sm[:, WABS + h:WABS + h + 1]))(),
                        [pr, sm_res], [rr])
                    if h == 0:
                        S.op("dve", (lambda rt=rt, sv=sv, cols=cols: lambda e: e.tensor_scalar(
                            out=sv, in0=rt[:, 0:cols], scalar1=sm[:, WSGN:WSGN + 1], scalar2=None, op0=ALU.mult))(),
                            [rr, sm_res], [s_res])
                    else:
                        S.op("dve", (lambda rt=rt, sv=sv, cols=cols, h=h: lambda e: e.scalar_tensor_tensor(
                            out=sv, in0=rt[:, 0:cols], scalar=sm[:, WSGN + h:WSGN + h + 1], in1=sv,
                            op0=ALU.mult, op1=ALU.add))(), [rr, sm_res, s_res], [s_res])
            sc_v = score[:, 0:ncol]
            S.op("dve", lambda e, sc_v=sc_v: e.tensor_reduce(out=sm[:, AMAX:AMAX + 1], in_=sc_v, axis=AX.X, op=ALU.max,
                 apply_absolute_value=True), [s_res], [sm_res])
            S.op("dve", lambda e: e.tensor_scalar(out=sm[:, RNG:RNG + 1], in0=sm[:, AMAX:AMAX + 1], scalar1=2.0,
                 scalar2=None, op0=ALU.mult), [sm_res], [sm_res])
            S.op("dve", lambda e: e.tensor_scalar(out=sm[:, LO:LO + 1], in0=sm[:, AMAX:AMAX + 1], scalar1=-1.0,
                 scalar2=None, op0=ALU.mult), [sm_res], [sm_res])
            nbb = min(n, NB_BIAS)
            bv = score[:, (n - nbb) * 128:ncol]
            mv = mb_t[:, pat, (NB_BIAS - nbb) * 128:NB_BIAS * 128]
            S.op("dve", (lambda bv=bv, mv=mv: lambda e: e.tensor_tensor(out=bv, in0=bv, in1=mv, op=ALU.add))(),
                 [s_res, mb_res], [s_res])
            mq_v = maskq[:, 0:ncol]
            sc_f = sc_v
            sc_v = score_b[:, 0:ncol]
            S.op("act", (lambda sc_f=sc_f, sc_v=sc_v: lambda e: e.copy(out=sc_v, in_=sc_f))(), [s_res], [sb_res])
            for it in range(cfg.niter):
                ci = 0.5 ** (it + 1)
                S.op("dve", (lambda ci=ci: lambda e: e.scalar_tensor_tensor(
                    out=sm[:, MID:MID + 1], in0=sm[:, RNG:RNG + 1], scalar=ci, in1=sm[:, LO:LO + 1],
                    op0=ALU.mult, op1=ALU.add))(), [sm_res], [sm_res])
                S.op("dve", lambda e: e.memset(sm[:, CNT:CNT + 1], 0.0), [sm_res], [sm_res])
                S.op("dve", (lambda mq_v=mq_v, sc_v=sc_v: lambda e: e.tensor_scalar(
                    out=mq_v, in0=sc_v, scalar1=sm[:, MID:MID + 1], scalar2=sm[:, ZERO:ZERO + 1], op0=ALU.is_ge, op1=ALU.add,
                    accum_out=sm[:, CNT:CNT + 1]))(), [sb_res, sm_res, mq_res], [sm_res, mq_res])
                S.op("dve", lambda e: e.tensor_scalar(out=sm[:, DD:DD + 1], in0=sm[:, CNT:CNT + 1], scalar1=sm[:, KTH:KTH + 1],
                     scalar2=sm[:, RNG:RNG + 1], op0=ALU.is_ge, op1=ALU.mult), [sm_res], [sm_res])
                S.op("dve", (lambda ci=ci: lambda e: e.scalar_tensor_tensor(
                    out=sm[:, LO:LO + 1], in0=sm[:, DD:DD + 1], scalar=ci, in1=sm[:, LO:LO + 1],
                    op0=ALU.mult, op1=ALU.add))(), [sm_res], [sm_res])
            S.op("dve", (lambda mq_v=mq_v, sc_v=sc_v: lambda e: e.tensor_scalar(
                out=mq_v, in0=sc_v, scalar1=sm[:, LO:LO + 1], scalar2=None, op0=ALU.is_ge))(),
                [sb_res, sm_res], [mq_res])
            tstate[t] = (qT, q_res, n)

        def prep_b(t):
            qT, q_res, n = tstate[t]
            for b0 in range(0, n, 8):
                nb = min(8, n - b0)
                pbv = psb[:, 0:nb * 128]
                pbr = psb_res[0]
                for j in range(nb):
                    tr(psb[:, j * 128:(j + 1) * 128], maskq[:, (b0 + j) * 128:(b0 + j + 1) * 128],
                       idb, [mq_res, r_const], pbr)
                mtv = maskT[:, b0 * 128:(b0 + nb) * 128]
                S.op("dve", (lambda mtv=mtv, pbv=pbv: lambda e: e.tensor_scalar(out=mtv, in0=pbv, scalar1=-1.0, scalar2=30000.0,
                     op0=ALU.add, op1=ALU.mult))(), [pbr], [mt_res])

        def attend(t):
            qT, q_res, n = tstate[t]
            state = {"kv": None}

            def emit_qk(blk, g):
                kg, j = blk // 4, blk % 4
                if j == 0 and g == 0:
                    nb = min(4, n - blk)
                    ktt, vtt, kvr = kv_slots.next()
                    S.dma("sp", (lambda ktt=ktt, kg=kg, nb=nb: lambda e: e.dma_start(
                        out=ktt[:, :, 0:nb * 128], in_=KTs[:, :, kg * 512:kg * 512 + nb * 128].rearrange("g d k -> d g k")))(),
                        writes=[kvr])
                    S.dma("sp", (lambda vtt=vtt, kg=kg, nb=nb: lambda e: e.dma_start(
                        out=vtt[:, 0:nb, :], in_=Vs[kg * 512:kg * 512 + nb * 128, :].rearrange("(b p) c -> p b c", p=128)))(),
                        writes=[kvr])
                    state["kv"] = (ktt, vtt, kvr)
                ktt, vtt, kvr = state["kv"]
                mbc = maskT[:, blk * 128:(blk + 1) * 128].unsqueeze(1).to_broadcast([128, 4, 128])
                ps, pr = ps_s.next()
                mm(ps[:, :], ktt[:, g, j * 128:(j + 1) * 128], qT[:, 4 * g:4 * g + 4, :], True, False, [kvr, q_res], pr)
                mm(ps[:, :], idb, mbc, False, True, [mt_res, r_const], pr)
                pt, ptr = p_slots.next()
                S.op("act", (lambda pt=pt, ps=ps: lambda e: e.activation(out=pt, in_=ps[:, :], func=AF.Exp, scale=scale))(),
                     [pr], [ptr])
                return (blk, g, j, vtt, kvr, pt, ptr)

            def emit_pv(blk, g, j, vtt, kvr, pt, ptr):
                mm(po[g][0][:, :], vtt[:, j, g * 128:(g + 1) * 128], pt, blk == 0, blk == n - 1, [kvr, ptr], po[g][1])
                mm(pd[:, :], eqb[:, g, :], pt, blk == 0 and g == 0, blk == n - 1 and g == 3, [ptr, r_const], pd_res)

            pending = None
            for blk in range(n):
                for g in range(4):
                    cur = emit_qk(blk, g)
                    if pending is not None:
                        emit_pv(*pending)
                    pending = cur
            emit_pv(*pending)
            S.op("dve", lambda e: e.reciprocal(out=rden, in_=pd[:, :]), [pd_res], [rd_res])
            for g in range(4):
                ps, pr = ps_s.next()
                mm(ps[:, :], sel[:, g, :], rden, True, True, [rd_res, r_const], pr)
                S.op("act", (lambda ps=ps: lambda e: e.copy(out=rbc, in_=ps[:, :]))(), [pr], [rb_res])
                i = o_i[0] % 2
                o_i[0] += 1
                ot, orr = o_slots[i]
                S.op("dve", (lambda ot=ot, g=g: lambda e: e.tensor_tensor(out=ot, in0=po[g][0][:, :], in1=rbc, op=ALU.mult))(),
                     [po[g][1], rb_res], [orr])
                dst = OTs[:, 4 * g:4 * g + 4, t * 128:(t + 1) * 128]
                S.dma("sp", (lambda dst=dst, ot=ot: lambda e: e.dma_start(
                    out=dst, in_=ot.rearrange("p (a b) -> p a b", a=4)))(), reads=[orr], writes=[o_store[i]])

        prep_a(0)
        prep_b(0)
        for t in range(NTL):
            if t + 1 < NTL:
                prep_a(t + 1)
            attend(t)
            if t + 1 < NTL:
                prep_b(t + 1)
        S.barrier()

        C3 = Carver(PBASE)
        hT = C3.f32(KC * 512).rearrange("p (a b) -> p a b", a=KC)
        uT = C3.bf(KC * 512).rearrange("p (a b) -> p a b", a=KC)
        h_res, u_res = S.res("hT3"), S.res("uT3")
        bigw = 32 * 512 // 2
        big_f = C3.f32(bigw)
        big_res = S.res("big")
        hid = big_f.bitcast(BF16).rearrange("p (a b) -> p a b", a=32)
        aT = big_f[:, 0:4096].bitcast(BF16).rearrange("p (a b) -> p a b", a=KC)
        peT = big_f.rearrange("p (a b) -> p a b", a=KC)
        zT = big_f.rearrange("p (a b) -> p a b", a=KC)
        sT = C3.bf(KC * 512).rearrange("p (a b) -> p a b", a=KC)
        s_res3 = S.res("sT")
        pT = C3.bf(2 * 512).rearrange("p (a b) -> p a b", a=2)
        p_res = S.res("pT")
        YW = 30 + 512
        yT = C3.bf(KC * YW).rearrange("p (a b) -> p a b", a=KC)
        y_res = S.res("yT")
        xslots = Rot([(C3.f32(2048), S.res("xs3_%d" % i)) for i in range(2)])
        wslots = Rot([(C3.bf(8192), S.res("ws3_%d" % i)) for i in range(2)])
        tmp1, tmp2 = C3.f32(512), C3.f32(512)
        t_res = S.res("ntmp3")
        e_slots = Rot([(C3.f32(512), S.res("et%d" % i)) for i in range(3)])
        sig4 = C3.f32(4 * 512).rearrange("p (a b) -> p a b", a=4)
        sig_res = S.res("sig4")
        mean_t, rstd_t = C3.f32(512), C3.f32(512)
        ln_res = S.res("ln")
        out_store = [S.res("outst%d" % i) for i in range(2)]
        dg_halves = [(C3.bf(16 * 128).rearrange("p (a b) -> p a b", a=16), S.res("dg%d" % i)) for i in range(2)]
        psrot = Rot([(psf[i], psf_res[i]) for i in range(7)])
        a_store = S.res("aload")

        S.op("dve", lambda e: e.memset(yT[:, :, 0:30], 0.0), [], [y_res])

        def add_res_evac(ntok, bias_name=None):
            def ev(oc, ps, pr):
                if bias_name is None:
                    S.op("dve", lambda e: e.tensor_tensor(out=hT[:, oc, 0:ntok], in0=ps[:, 0:ntok], in1=hT[:, oc, 0:ntok],
                         op=ALU.add), [pr, h_res], [h_res])
                else:
                    b = spc(bias_name, oc)
                    S.op("dve", lambda e: e.scalar_tensor_tensor(out=hT[:, oc, 0:ntok], in0=ps[:, 0:ntok], scalar=b,
                         in1=hT[:, oc, 0:ntok], op0=ALU.add, op1=ALU.add), [pr, h_res, r_const], [h_res])
            return ev

        def mlp(layer, ntok):
            rmsnorm(hT, h_res, "g_mlp%d" % layer, ntok, uT, u_res, tmp1, tmp2, t_res, psrot)
            for half in range(2):
                cnt = [0]

                def hid_evac(oc, ps, pr):
                    et, er = e_slots.next()
                    S.op("act", lambda e: e.activation(out=et[:, 0:ntok], in_=ps[:, 0:ntok], func=AF.Relu), [pr], [er])
                    eng = "dve" if cnt[0] % 2 == 0 else "pool"
                    cnt[0] += 1
                    S.op(eng, lambda e: e.tensor_tensor(out=hid[:, oc, 0:ntok], in0=et[:, 0:ntok], in1=et[:, 0:ntok],
                         op=ALU.mult), [er], [big_res])
                linear(lambda kc: uT[:, kc, 0:ntok], [u_res], mlp_w1[layer], KC, 4096, ntok, wslots, psrot, hid_evac,
                       col_base=half * 4096)
                linear(lambda kc: hid[:, kc, 0:ntok], [big_res], mlp_w2[layer][half * 4096:(half + 1) * 4096, :], 32, D,
                       ntok, wslots, psrot, add_res_evac(ntok))

        def pe_gate_block(layer, ntok, tok0):
            load_xT(pq[layer, tok0:tok0 + ntok, :], ntok, 2, pT, p_res, xslots, psrot)

            def pe_evac(oc, ps, pr):
                S.op("act", lambda e: e.copy(out=peT[:, oc, 0:ntok], in_=ps[:, 0:ntok]), [pr], [big_res])
            linear(lambda kc: pT[:, kc, 0:ntok], [p_res], pe_proj[layer], 2, D, ntok, wslots, psrot, pe_evac)
            rmsnorm(hT, h_res, "g_gate%d" % layer, ntok, uT, u_res, tmp1, tmp2, t_res, psrot)

            def gate_evac(oc, ps, pr):
                et, er = e_slots.next()
                S.op("act", lambda e: e.activation(out=et[:, 0:ntok], in_=ps[:, 0:ntok], func=AF.Sigmoid), [pr], [er])
                S.op("dve", lambda e: e.tensor_tensor(out=et[:, 0:ntok], in0=et[:, 0:ntok], in1=peT[:, oc, 0:ntok],
                     op=ALU.mult), [er, big_res], [er])
                S.op("dve", lambda e: e.tensor_tensor(out=hT[:, oc, 0:ntok], in0=et[:, 0:ntok], in1=hT[:, oc, 0:ntok],
                     op=ALU.add), [er, h_res], [h_res])
            linear(lambda kc: uT[:, kc, 0:ntok], [u_res], pe_gate[layer], KC, D, ntok, wslots, psrot, gate_evac)

        def conv_in_glu(ntok):
            rmsnorm(hT, h_res, "g_mix1", ntok, uT, u_res, tmp1, tmp2, t_res, psrot)
            for tq in range(4):
                def g_evac(oc, ps, pr, tq=tq):
                    j = oc - 4 * tq
                    b = spc("b_in", 16 + oc)
                    S.op("act", lambda e: e.activation(out=sig4[:, j, 0:ntok], in_=ps[:, 0:ntok], func=AF.Sigmoid, bias=b),
                         [pr, r_const], [sig_res])
                linear(lambda kc: uT[:, kc, 0:ntok], [u_res], cw_in, KC, 512, ntok, wslots, psrot,
                       lambda oc, ps, pr, tq=tq: g_evac(oc + 4 * tq, ps, pr), col_base=2048 + tq * 512)

                def a_evac(oc, ps, pr, tq=tq):
                    j = oc - 4 * tq
                    b = spc("b_in", oc)
                    S.op("dve", lambda e: e.scalar_tensor_tensor(out=yT[:, oc, 30:30 + ntok], in0=ps[:, 0:ntok], scalar=b,
                         in1=sig4[:, j, 0:ntok], op0=ALU.add, op1=ALU.mult), [pr, sig_res, r_const], [y_res])
                linear(lambda kc: uT[:, kc, 0:ntok], [u_res], cw_in, KC, 512, ntok, wslots, psrot,
                       lambda oc, ps, pr, tq=tq: a_evac(oc + 4 * tq, ps, pr), col_base=tq * 512)

        def layer0_dense(t0, ntl):
            ntok = ntl * 128
            tok0 = t0 * 128
            load_xT(xq[tok0:tok0 + ntok, :], ntok, KC, hT, h_res, xslots, psrot)
            S.dma("sp", lambda e: e.dma_start(out=aT[:, :, 0:ntok], in_=OTs[:, :, tok0:tok0 + ntok]), writes=[big_res])
            linear(lambda kc: aT[:, kc, 0:ntok], [big_res], w_out, KC, D, ntok, wslots, psrot, add_res_evac(ntok))
            mlp(0, ntok)
            pe_gate_block(0, ntok, tok0)

        otile_i = [0]

        def dense_group(t0, ntl):
            s, i0 = cfg.tiles[t0]
            ntok = ntl * 128
            tok0 = t0 * 128
            layer0_dense(t0, ntl)
            conv_in_glu(ntok)
            if i0 < 0:
                hs = spc("hscale", s)
                S.op("dve", lambda e, hs=hs: e.tensor_scalar(out=yT[:, :, 0:30], in0=yT[:, :, 128:158], scalar1=hs,
                     scalar2=None, op0=ALU.mult), [y_res, r_const], [y_res])
                return
            for c in range(KC):
                ps, pr = psrot.next()
                for hf in range(2):
                    dg, dgr = dg_halves[hf]
                    j0, j1 = (0, 16) if hf == 0 else (16, 31)
                    for j in range(j0, j1):
                        wj = spc("w_dw", c * 31 + j)
                        S.op("dve", lambda e, dg=dg, j=j, j0=j0, wj=wj: e.tensor_scalar(out=dg[:, j - j0, :], in0=idb, scalar1=wj,
                             scalar2=None, op0=ALU.mult), [r_const], [dgr])
                    for j in range(j0, j1):
                        mm(ps[:, 0:ntok], dg[:, j - j0, :], yT[:, c, j:j + ntok], j == 0, j == 30, [dgr, y_res], pr)
                bdw = spc("b_dw", c)
                S.op("dve", lambda e, c=c, ps=ps, bdw=bdw: e.tensor_scalar(out=zT[:, c, 0:ntok], in0=ps[:, 0:ntok], scalar1=bdw,
                     scalar2=None, op0=ALU.add), [pr, r_const, big_res], [big_res])
            S.op("dve", lambda e: e.tensor_copy(out=yT[:, :, 0:30], in_=yT[:, :, ntok:ntok + 30]), [y_res], [y_res])
            S.op("act", lambda e: e.copy(out=sT[:, :, 0:ntok], in_=zT[:, :, 0:ntok]), [big_res], [s_res3])
            S.op("act", lambda e: e.activation(out=uT[:, :, 0:ntok], in_=zT[:, :, 0:ntok], func=AF.Square), [big_res], [u_res])
            ps, pr = psrot.next()
            for kc in range(KC):
                mm(ps[:, 0:ntok], onesD, sT[:, kc, 0:ntok], kc == 0, kc == KC - 1, [s_res3, r_const], pr)
            ps2, pr2 = psrot.next()
            for kc in range(KC):
                mm(ps2[:, 0:ntok], onesD, uT[:, kc, 0:ntok], kc == 0, kc == KC - 1, [u_res, r_const], pr2)
            S.op("act", lambda e: e.copy(out=mean_t[:, 0:ntok], in_=ps[:, 0:ntok]), [pr], [ln_res])
            S.op("dve", lambda e: e.tensor_tensor(out=tmp1[:, 0:ntok], in0=mean_t[:, 0:ntok], in1=mean_t[:, 0:ntok], op=ALU.mult),
                 [ln_res], [t_res])
            S.op("dve", lambda e: e.tensor_tensor(out=tmp1[:, 0:ntok], in0=ps2[:, 0:ntok], in1=tmp1[:, 0:ntok], op=ALU.subtract),
                 [pr2, t_res], [t_res])
            S.op("dve", lambda e: e.tensor_scalar(out=tmp1[:, 0:ntok], in0=tmp1[:, 0:ntok], scalar1=0.0, scalar2=None,
                 op0=ALU.max), [t_res], [t_res])
            S.op("act", lambda e: e.activation(out=tmp2[:, 0:ntok], in_=tmp1[:, 0:ntok], func=AF.Sqrt, bias=EPS, scale=1.0),
                 [t_res], [t_res])
            S.op("dve", lambda e: e.reciprocal(out=rstd_t[:, 0:ntok], in_=tmp2[:, 0:ntok]), [t_res], [ln_res])
            for c in range(KC):
                et, er = e_slots.next()
                S.op("dve", lambda e, c=c, et=et: e.tensor_tensor(out=et[:, 0:ntok], in0=zT[:, c, 0:ntok], in1=mean_t[:, 0:ntok],
                     op=ALU.subtract), [big_res, ln_res], [er])
                S.op("dve", lambda e, et=et: e.tensor_tensor(out=et[:, 0:ntok], in0=et[:, 0:ntok], in1=rstd_t[:, 0:ntok],
                     op=ALU.mult), [er, ln_res], [er])
                lg, lb = spc("ln_g", c), spc("ln_b", c)
                S.op("act", lambda e, c=c, et=et, lg=lg, lb=lb: e.activation(out=sT[:, c, 0:ntok], in_=et[:, 0:ntok],
                     func=AF.Silu, bias=lb, scale=lg), [er, r_const], [s_res3])
            linear(lambda kc: sT[:, kc, 0:ntok], [s_res3], cw_out, KC, D, ntok, wslots, psrot, add_res_evac(ntok, "b_out"))
            mlp(1, ntok)
            pe_gate_block(1, ntok, tok0)
            outT = peT
            rmsnorm(hT, h_res, "g_final", ntok, uT, u_res, tmp1, tmp2, t_res, psrot, out_view=outT, out_res=big_res)
            seg_out_tile0 = s * cfg.seg_tiles + i0
            for sub in range(ntl):
                xt, xr = xslots.next()
                for d0 in range(0, KC, 4):
                    ps, pr = psrot.next()
                    for j in range(4):
                        tr(ps[:, j * 128:(j + 1) * 128], outT[:, d0 + j, sub * 128:(sub + 1) * 128], idf, [u_res, big_res, r_const], pr)
                    if (d0 // 4) % 2 == 0:
                        S.op("act", lambda e, xt=xt, ps=ps, d0=d0: e.copy(out=xt[:, d0 * 128:(d0 + 4) * 128], in_=ps[:, :]), [pr], [xr])
                    else:
                        S.op("dve", lambda e, xt=xt, ps=ps, d0=d0: e.tensor_copy(out=xt[:, d0 * 128:(d0 + 4) * 128], in_=ps[:, :]), [pr], [xr])
                row0 = (seg_out_tile0 + sub) * 128
                k = otile_i[0] % 2
                otile_i[0] += 1
                S.dma("sp", lambda e, xt=xt, row0=row0: e.dma_start(out=out[row0:row0 + 128, :], in_=xt), reads=[xr],
                      writes=[out_store[k]])

        for (t0, ntl) in groups:
            dense_group(t0, ntl)
        S.wait_all("sp", out_store)
        S.barrier()
        with nc.Block() as block:
            stats = S.finalize(block)
        build_nc.stats = stats
    return nc


def rope_tables(pos):
    half = 16
    inv = (np.float32(500000.0) ** (-np.arange(half, dtype=np.float32) * np.float32(2.0) / np.float32(32))).astype(np.float32)
    ang = pos.astype(np.float32)[None, :] * inv[:, None]
    cs = np.cos(ang).astype(np.float32)
    sn = np.sin(ang).astype(np.float32)
    return np.stack([np.concatenate([cs, cs], 0), np.concatenate([sn, sn], 0)], 0)


def make_inputs(cfg, inp):
    lay, NSP = small_param_layout()
    x = np.asarray(inp["x"], dtype=np.float32)
    p = np.asarray(inp["p"], dtype=np.float32)
    NKEY = cfg.nkg * 512
    ident = np.eye(128, dtype=np.float32)
    rot = np.zeros((32, 32), np.float32)
    for m in range(16):
        rot[m + 16, m] = -1.0
        rot[m, m + 16] = 1.0
    csel = np.zeros((128, 4, 128), np.float32)
    ceq = np.zeros((128, 4, 128), np.float32)
    for g in range(4):
        csel[32 * g, g, :] = 1.0
        ceq[:, g, 32 * g:32 * g + 32] = 1.0
    ropek = rope_tables(np.arange(NKEY))
    shared = {
        "c_ident": ident, "c_rot": rot, "c_sel": csel, "c_eq": ceq, "ropek": ropek,
        "dsa_w_in": np.ascontiguousarray(inp["dsa_w_in"][0]), "dsa_w_out": np.ascontiguousarray(inp["dsa_w_out"][0]),
        "mlp_w1": np.asarray(inp["mlp_w1"]), "mlp_w2": np.asarray(inp["mlp_w2"]),
        "pe_proj": np.asarray(inp["pe_proj"]), "pe_gate": np.asarray(inp["pe_gate"]),
        "conv_w_in": np.ascontiguousarray(inp["conv_w_in"][0]), "conv_w_out": np.ascontiguousarray(inp["conv_w_out"][0]),
    }
    sp_base = np.zeros((128, NSP), np.float32)

    def put(name, arr):
        o, n = lay[name]
        assert arr.shape == (128, n), (name, arr.shape)
        sp_base[:, o:o + n] = arr
    put("g_mix0", fm(inp["mix_norm"][0])); put("g_mlp0", fm(inp["mlp_norm"][0])); put("g_gate0", fm(inp["pe_gate_norm"][0]))
    put("g_mix1", fm(inp["mix_norm"][1])); put("g_mlp1", fm(inp["mlp_norm"][1])); put("g_gate1", fm(inp["pe_gate_norm"][1]))
    put("g_final", fm(inp["final_norm"]))
    put("b_in", fm(inp["conv_b_in"][0])); put("b_dw", fm(inp["conv_b_dw"][0])); put("ln_g", fm(inp["conv_ln_g"][0]))
    put("ln_b", fm(inp["conv_ln_b"][0])); put("b_out", fm(inp["conv_b_out"][0]))
    wdw = np.asarray(inp["conv_w_dw"][0], np.float32)
    wdw_fm = wdw.T.reshape(16, 128, 31).transpose(1, 0, 2).reshape(128, 16 * 31)
    put("w_dw", np.ascontiguousarray(wdw_fm))
    in_maps = []
    tile_blocks = []
    for c in range(8):
        b, half = c // 2, c % 2
        blocks = [cfg.block_of(half, t) for t in range(cfg.ntl)]
        tile_blocks.append(blocks)
        rows = np.concatenate([np.arange(j * 128, (j + 1) * 128) for j in blocks])
        m = dict(shared)
        m["xk"] = np.ascontiguousarray(x[b, 0:NKEY])
        m["xq"] = np.ascontiguousarray(x[b, rows])
        m["pq"] = np.ascontiguousarray(p[:, b][:, rows])
        m["ropeq"] = np.ascontiguousarray(rope_tables(rows))
        mb = np.zeros((2 * cfg.nseg, 128, NB_BIAS * 128), np.float32)
        for t in range(cfg.ntl):
            s, i = cfg.tiles[t]
            pat = 2 * s + (0 if i < 0 else 1)
            if i > 0:
                continue
            n = cfg.nkb[t]
            j = blocks[t]
            dpos = NB_BIAS - (n - j)
            assert 0 <= dpos < NB_BIAS, (c, t, n, j)
            pm = np.zeros((128, NB_BIAS, 128), np.float32)
            pm[:, dpos + 1:, :] = NEG
            pm[0:64, dpos, 64:128] = NEG
            mb[pat] = pm.reshape(128, NB_BIAS * 128)
        m["mbias"] = mb
        sp = sp_base.copy()
        o, _ = lay["hscale"]
        for s in range(cfg.nseg):
            sp[:, o + s] = cfg.halo_scale(half, s)
        m["smallp"] = sp
        in_maps.append(m)
    return in_maps, tile_blocks


_CFG = None


def kernel(**inputs):
    cfg = _CFG or Cfg()
    nc = build_nc(cfg)
    in_maps, tile_blocks = make_inputs(cfg, inputs)
    res = run_bass_kernel_spmd(nc, in_maps, core_ids=list(range(8)))
    B, S_, D_ = inputs["x"].shape
    outp = np.zeros((B, S_, D_), np.float32)
    for c in range(8):
        b = c // 2
        o = np.asarray(res.results[c]["out"])
        k = 0
        for t in range(cfg.ntl):
            s, i = cfg.tiles[t]
            if i < 0:
                continue
            j = tile_blocks[c][t]
            outp[b, j * 128:(j + 1) * 128, :] = o[k * 128:(k + 1) * 128]
            k += 1
    return outp
```

```python
import numpy as np
from contextlib import ExitStack
import concourse.bass as bass
import concourse.mybir as mybir
from concourse.bass_utils import run_bass_kernel_spmd

F32 = mybir.dt.float32
BF16 = mybir.dt.bfloat16
ALU = mybir.AluOpType
AF = mybir.ActivationFunctionType
AX = mybir.AxisListType

D = 2048
KC = 16
SEQ = 8192
NB_BIAS = 17
NEG = -1.0e30
EPS = 1e-6
TOPK = 256.0


class Res:
    __slots__ = ("name", "lw", "rd", "sem", "dcount")

    def __init__(self, name):
        self.name = name
        self.lw = None
        self.rd = []
        self.sem = None
        self.dcount = 0


class Op:
    __slots__ = ("eng", "fn", "deps", "inc", "seq", "dma_res", "dma_val", "idx")

    def __init__(self, eng, fn):
        self.eng = eng
        self.fn = fn
        self.idx = 0
        self.deps = {}
        self.inc = False
        self.seq = 0
        self.dma_res = None
        self.dma_val = 0


ENGS = ("pe", "act", "dve", "pool", "sp")


class Sched:
    def __init__(self, nc, stack):
        self.nc = nc
        self.stack = stack
        self.ops = {e: [] for e in ENGS}
        self.esem = {}
        for e in ("pe", "act", "dve", "pool"):
            self.esem[e] = stack.enter_context(nc.semaphore("sem_" + e))
        self.nres = 0
        self.dma_res = []

    def res(self, name=None):
        self.nres += 1
        return Res(name or ("r%d" % self.nres))

    @staticmethod
    def _add(deps, d):
        if d[0] == "op":
            key = d[1].eng
            cur = deps.get(key)
            if cur is None or cur[1].idx < d[1].idx:
                deps[key] = d
        else:
            key = d[1]
            cur = deps.get(key)
            if cur is None or cur[2] < d[2]:
                deps[key] = d

    def _collect(self, op, reads, writes):
        deps = op.deps
        for r in reads:
            if r.lw is not None:
                self._add(deps, r.lw)
        for w in writes:
            if w.lw is not None:
                self._add(deps, w.lw)
            for d in w.rd:
                self._add(deps, d)

    def op(self, eng, fn, reads=(), writes=()):
        o = Op(eng, fn)
        self._collect(o, reads, writes)
        me = ("op", o)
        for r in reads:
            r.rd = [d for d in r.rd if not (d[0] == "op" and d[1].eng == eng)]
            r.rd.append(me)
        for w in writes:
            w.lw = me
            w.rd = []
        o.idx = len(self.ops[eng])
        self.ops[eng].append(o)
        return o

    def dma(self, queue, fn, reads=(), writes=()):
        o = Op(queue, fn)
        self._collect(o, reads, writes)
        pr = writes[0]
        if pr.sem is None:
            pr.sem = self.stack.enter_context(self.nc.semaphore("d_" + pr.name))
            self.dma_res.append(pr)
        pr.dcount += 16
        o.dma_res = pr
        o.dma_val = pr.dcount
        me = ("dma", pr, pr.dcount)
        for r in reads:
            r.rd.append(me)
        for w in writes:
            w.lw = me
            w.rd = []
        o.idx = len(self.ops[queue])
        self.ops[queue].append(o)
        return o

    def wait_all(self, eng, reads):
        o = Op(eng, None)
        self._collect(o, reads, ())
        o.idx = len(self.ops[eng])
        self.ops[eng].append(o)
        return o

    def barrier(self):
        deps = []
        for e in ("pe", "act", "dve", "pool"):
            if self.ops[e]:
                for o in reversed(self.ops[e]):
                    if o.fn is not None and o.dma_res is None:
                        deps.append(("op", o))
                        break
        for r in self.dma_res:
            deps.append(("dma", r, r.dcount))
        for e in ENGS:
            o = Op(e, None)
            for d in deps:
                self._add(o.deps, d)
            o.idx = len(self.ops[e])
            self.ops[e].append(o)

    def finalize(self, block):
        for e in ENGS:
            for o in self.ops[e]:
                for d in o.deps.values():
                    if d[0] == "op":
                        p = d[1]
                        if p.eng == "pe" and o.eng == "pe":
                            continue
                        p.inc = True
        for e in ENGS:
            n = 0
            for o in self.ops[e]:
                if o.inc:
                    n += 1
                    o.seq = n
        stats = {}

        def emit(engname, engobj):
            seen = {}
            nw = 0
            for o in self.ops[engname]:
                for d in o.deps.values():
                    if d[0] == "op":
                        p = d[1]
                        if p.eng == "pe" and engname == "pe":
                            continue
                        sem = self.esem[p.eng]
                        val = p.seq
                        key = p.eng
                    else:
                        sem = d[1].sem
                        val = d[2]
                        key = d[1]
                    if seen.get(key, 0) >= val:
                        continue
                    seen[key] = val
                    engobj.wait_ge(sem, val)
                    nw += 1
                if o.fn is None:
                    continue
                ins = o.fn(engobj)
                if o.dma_res is not None:
                    ins.then_inc(o.dma_res.sem, 16)
                elif o.inc:
                    ins.then_inc(self.esem[engname], 1)
            stats[engname] = (len(self.ops[engname]), nw)

        @block.tensor
        def _(e):
            emit("pe", e)

        @block.scalar
        def _(e):
            emit("act", e)

        @block.vector
        def _(e):
            emit("dve", e)

        @block.gpsimd
        def _(e):
            emit("pool", e)

        @block.sync
        def _(e):
            emit("sp", e)

        return stats


class Cfg:
    def __init__(self, seg_starts=((0, 48), (16, 32)), seg_tiles=16, nkg=16, niter=16):
        self.seg_starts = seg_starts
        self.nseg = len(seg_starts[0])
        self.seg_tiles = seg_tiles
        self.nkg = nkg
        self.niter = niter
        self.tiles = []
        for s in range(self.nseg):
            self.tiles.append((s, -1))
            for i in range(seg_tiles):
                self.tiles.append((s, i))
        self.ntl = len(self.tiles)
        self.nkb = []
        for (s, i) in self.tiles:
            m = max(seg_starts[0][s], seg_starts[1][s])
            self.nkb.append(max(m + i + 1, 1) if i >= 0 else max(m, 1))
        assert max(self.nkb) <= nkg * 4

    def block_of(self, half, t):
        s, i = self.tiles[t]
        st = self.seg_starts[half][s]
        if i >= 0:
            return st + i
        return st - 1 if st > 0 else 0

    def halo_scale(self, half, s):
        return 1.0 if self.seg_starts[half][s] > 0 else 0.0


def small_param_layout():
    lay = {}
    off = 0
    for nm, n in (("g_mix0", 16), ("g_mlp0", 16), ("g_gate0", 16), ("g_mix1", 16), ("g_mlp1", 16),
                  ("g_gate1", 16), ("g_final", 16), ("b_in", 32), ("b_dw", 16), ("ln_g", 16),
                  ("ln_b", 16), ("b_out", 16), ("w_dw", 16 * 31), ("hscale", 2)):
        lay[nm] = (off, n)
        off += n
    return lay, off


def fm(v):
    v = np.asarray(v, dtype=np.float32)
    return np.ascontiguousarray(v.reshape(-1, 128).T)


def build_nc(cfg):
    nc = bass.Bass("TRN2", target_bir_lowering=False)
    NTL = cfg.ntl
    NTOK = NTL * 128
    NOUT = cfg.nseg * cfg.seg_tiles * 128
    NKEY = cfg.nkg * 512
    lay, NSP = small_param_layout()

    def din(name, shape):
        return nc.dram_tensor(name, list(shape), F32, kind="ExternalInput").ap()

    xk = din("xk", [NKEY, D])
    xq = din("xq", [NTOK, D])
    pq = din("pq", [2, NTOK, 256])
    ropek = din("ropek", [2, 32, NKEY])
    ropeq = din("ropeq", [2, 32, NTOK])
    mbias = din("mbias", [2 * cfg.nseg, 128, NB_BIAS * 128])
    c_ident = din("c_ident", [128, 128])
    c_rot = din("c_rot", [32, 32])
    c_sel = din("c_sel", [128, 4, 128])
    c_eq = din("c_eq", [128, 4, 128])
    smallp = din("smallp", [128, NSP])
    w_in = din("dsa_w_in", [D, 4232])
    w_out = din("dsa_w_out", [D, D])
    mlp_w1 = din("mlp_w1", [2, D, 4 * D])
    mlp_w2 = din("mlp_w2", [2, 4 * D, D])
    pe_proj = din("pe_proj", [2, 256, D])
    pe_gate = din("pe_gate", [2, D, D])
    cw_in = din("conv_w_in", [D, 2 * D])
    cw_out = din("conv_w_out", [D, D])
    out = nc.dram_tensor("out", [NOUT, D], F32, kind="ExternalOutput").ap()

    def dscr(name, shape, dt):
        return nc.dram_tensor(name, list(shape), dt, kind="Internal").ap()

    KTs = dscr("KTs", [4, 128, NKEY], BF16)
    kiTs = dscr("kiTs", [128, NKEY], BF16)
    Vs = dscr("Vs", [NKEY, 512], BF16)
    QTs = dscr("QTs", [128, 24, NTOK], BF16)
    WIs = dscr("WIs", [NTOK, 8], F32)
    OTs = dscr("OTs", [128, 16, NTOK], BF16)

    st = ExitStack()
    with st:
        S = Sched(nc, st)
        ARENA = 51500
        arena = st.enter_context(nc.sbuf_tensor("arena", [128, ARENA], F32))
        psf = [st.enter_context(nc.psum_tensor("psf%d" % i, [128, 512], F32)) for i in range(7)]
        psb = st.enter_context(nc.psum_tensor("psb", [128, 1024], BF16))
        psf_res = [S.res("psf%d" % i) for i in range(7)]
        psb_res = [S.res("psbA"), S.res("psbB")]

        class Carver:
            def __init__(self, base):
                self.pos = base

            def f32(self, n):
                a = arena[:, self.pos:self.pos + n]
                self.pos += n
                assert self.pos <= ARENA, self.pos
                return a

            def bf(self, n):
                w = (n + 1) // 2
                a = arena[:, self.pos:self.pos + w].bitcast(BF16)
                self.pos += w
                assert self.pos <= ARENA, self.pos
                return a

        C0 = Carver(0)
        idf = C0.f32(128)
        idb = C0.bf(128)
        rotb = C0.bf(32)
        sel = C0.f32(512).rearrange("p (a b) -> p a b", a=4)
        eqb = C0.bf(512).rearrange("p (a b) -> p a b", a=4)
        sp_t = C0.f32(NSP)
        onesD = C0.bf(128)
        r_const = S.res("const")
        S.dma("sp", lambda e: e.dma_start(out=idf, in_=c_ident[:, :]), writes=[r_const])
        S.dma("sp", lambda e: e.dma_start(out=sel, in_=c_sel[:, :, :]), writes=[r_const])
        S.dma("sp", lambda e: e.dma_start(out=sp_t, in_=smallp[:, :]), writes=[r_const])
        r_constp = S.res("constp")
        S.dma("pool", lambda e: e.dma_start(out=idb, in_=c_ident[:, :]), writes=[r_constp])
        S.dma("pool", lambda e: e.dma_start(out=rotb[0:32, :], in_=c_rot[:, :]), writes=[r_constp])
        S.dma("pool", lambda e: e.dma_start(out=eqb, in_=c_eq[:, :, :]), writes=[r_constp])
        S.op("dve", lambda e: e.memset(onesD, 1.0 / D), writes=[r_const])
        S.barrier()
        PBASE = C0.pos

        def spc(name, j=0, n=1):
            o, _ = lay[name]
            return sp_t[:, o + j:o + j + n]

        class Rot:
            def __init__(self, items):
                self.items = items
                self.i = 0

            def next(self):
                it = self.items[self.i % len(self.items)]
                self.i += 1
                return it

        def mm(ps_ap, lhsT, rhs, start, stop, reads, wres):
            S.op("pe", lambda e: e.matmul(ps_ap, lhsT=lhsT, rhs=rhs, start=start, stop=stop), reads, [wres])

        def tr(ps_ap, in_ap, ident, reads, wres):
            S.op("pe", lambda e: e.transpose(ps_ap, in_ap, ident), reads, [wres])

        def load_xT(src_rows, ntok, kcx, dst, dst_res, xslots, psrot, out_bf=False):
            nsub = ntok // 128
            cnt = 0
            for sub in range(nsub):
                xt, xr = xslots.next()
                xv = xt[:, 0:kcx * 128]
                rows = src_rows[sub * 128:(sub + 1) * 128, :]
                S.dma("sp", (lambda xv=xv, rows=rows: lambda e: e.dma_start(out=xv, in_=rows))(), writes=[xr])
                for k0 in range(0, kcx, 4):
                    nk = min(4, kcx - k0)
                    ps, pr = psrot.next()
                    for j in range(nk):
                        tr(ps[:, j * 128:(j + 1) * 128], xv[:, (k0 + j) * 128:(k0 + j + 1) * 128], idf, [xr, r_const], pr)
                    src = ps[:, 0:nk * 128].rearrange("p (a b) -> p a b", a=nk)
                    dv = dst[:, k0:k0 + nk, sub * 128:(sub + 1) * 128]
                    if cnt % 2 == 0:
                        S.op("act", (lambda dv=dv, src=src: lambda e: e.copy(out=dv, in_=src))(), [pr], [dst_res])
                    else:
                        S.op("dve", (lambda dv=dv, src=src: lambda e: e.tensor_copy(out=dv, in_=src))(), [pr], [dst_res])
                    cnt += 1

        def rmsnorm(hT, h_res, gname, ntok, uT, u_res, tmp1, tmp2, t_res, psrot, out_view=None, out_res=None):
            hv = hT[:, :, 0:ntok]
            uv = uT[:, :, 0:ntok]
            S.op("act", lambda e: e.activation(out=uv, in_=hv, func=AF.Square), [h_res], [u_res])
            ps, pr = psrot.next()
            for kc in range(KC):
                mm(ps[:, 0:ntok], onesD, uT[:, kc, 0:ntok], kc == 0, kc == KC - 1, [u_res, r_const], pr)
            t1 = tmp1[:, 0:ntok]
            t2 = tmp2[:, 0:ntok]
            S.op("act", lambda e: e.activation(out=t1, in_=ps[:, 0:ntok], func=AF.Sqrt, bias=EPS, scale=1.0), [pr], [t_res])
            S.op("dve", lambda e: e.reciprocal(out=t2, in_=t1), [t_res], [t_res])
            ov = out_view if out_view is not None else uT
            ores = out_res if out_res is not None else u_res
            for kc in range(KC):
                g = spc(gname, kc)
                S.op("dve", (lambda kc=kc, g=g: lambda e: e.scalar_tensor_tensor(
                    out=ov[:, kc, 0:ntok], in0=hT[:, kc, 0:ntok], scalar=g, in1=t2, op0=ALU.mult, op1=ALU.mult))(),
                    [h_res, t_res, r_const], [ores])

        conv_res = {}

        def convert(name, src_ap, shape, after=()):
            dstb = dscr(name + "_b", shape, BF16)
            r = S.res("cv_" + name)
            conv_res[name + "_b"] = r
            if len(shape) == 3:
                s2 = src_ap.rearrange("a k o -> (a k) o")
                d2 = dstb.rearrange("a k o -> (a k) o")
                rows = shape[0] * shape[1]
            else:
                s2, d2, rows = src_ap, dstb, shape[0]
            rb = max(128, (2 * 1024 * 1024) // shape[-1])
            for r0 in range(0, rows, rb):
                r1 = min(rows, r0 + rb)
                S.dma("pool", (lambda r0=r0, r1=r1: lambda e: e.dma_start(out=d2[r0:r1, :], in_=s2[r0:r1, :]))(),
                      reads=list(after), writes=[r])
            return dstb

        def load_w(wslots, Wsrc, kcw, col0, ncols):
            wt, wr = wslots.next()
            wv = wt[:, 0:kcw * ncols].rearrange("p (a b) -> p a b", a=kcw)
            src = Wsrc[:, col0:col0 + ncols].rearrange("(kc p) o -> p kc o", p=128)
            S.dma("sp", lambda e: e.dma_start(out=wv, in_=src), reads=[conv_res[Wsrc.name]], writes=[wr])
            return wv, wr

        def linear(in_fn, in_res, Wsrc, kcw, ncols_total, ntok, wslots, psrot, evac, tile_cols=None, col_base=0):
            if tile_cols is None:
                tile_cols = min(ncols_total, 8192 // kcw)
            for c0 in range(0, ncols_total, tile_cols):
                ncols = min(tile_cols, ncols_total - c0)
                wv, wr = load_w(wslots, Wsrc, kcw, col_base + c0, ncols)
                for o in range(ncols // 128):
                    ps, pr = psrot.next()
                    for kc in range(kcw):
                        mm(ps[:, 0:ntok], wv[:, kc, o * 128:(o + 1) * 128], in_fn(kc), kc == 0, kc == kcw - 1,
                           in_res + [wr], pr)
                    evac((c0 // 128) + o, ps, pr)

        def rope(t, t_res, cs, sn, tab_res, ntok, ps, pr, tmpa, tmpb, tmp_res):
            mm(ps[0:32, 0:ntok], rotb[0:32, 0:32], t[0:32, 0:ntok], True, True, [t_res, r_const], pr)
            a = tmpa[0:32, 0:ntok]
            b = tmpb[0:32, 0:ntok]
            S.op("dve", lambda e: e.tensor_tensor(out=a, in0=ps[0:32, 0:ntok], in1=sn[0:32, 0:ntok], op=ALU.mult),
                 [pr, tab_res], [tmp_res])
            S.op("dve", lambda e: e.tensor_tensor(out=b, in0=t[0:32, 0:ntok], in1=cs[0:32, 0:ntok], op=ALU.mult),
                 [t_res, tab_res], [tmp_res])
            S.op("dve", lambda e: e.tensor_tensor(out=t[0:32, 0:ntok], in0=a, in1=b, op=ALU.add), [tmp_res], [t_res])

        C1 = Carver(PBASE)
        hT = C1.f32(KC * 512).rearrange("p (a b) -> p a b", a=KC)
        uT = C1.bf(KC * 512).rearrange("p (a b) -> p a b", a=KC)
        h_res, u_res = S.res("hT"), S.res("uT")
        xslots = Rot([(C1.f32(2048), S.res("xs%d" % i)) for i in range(4)])
        wkv = C1.bf(KC * 1152).rearrange("p (a b) -> p a b", a=KC)
        wkv_res = S.res("wkv")
        wwi = C1.bf(KC * 8).rearrange("p (a b) -> p a b", a=KC)
        wslots = Rot([(C1.bf(8192), S.res("ws%d" % i)) for i in range(2)])
        tmp1, tmp2 = C1.f32(512), C1.f32(512)
        t_res = S.res("ntmp")
        rtA, rtB = C1.f32(512), C1.f32(512)
        rt_res = S.res("rtmp")
        tabs = Rot([(C1.f32(1024), S.res("tab%d" % i)) for i in range(2)])
        kt_slots = [(C1.bf(512), S.res("kt%d" % i)) for i in range(3)]
        vt_slots = [(C1.bf(512), S.res("vt%d" % i)) for i in range(2)]
        wi_slots = [(C1.f32(8), S.res("wit%d" % i)) for i in range(2)]
        kt_store = [S.res("kst%d" % i) for i in range(3)]
        vt_store = [S.res("vst%d" % i) for i in range(2)]
        wi_store = [S.res("wst%d" % i) for i in range(2)]
        psrot = Rot([(psf[i], psf_res[i]) for i in range(7)])

        for (c0, n, dcol) in ((2048, 1024, 0), (4096, 128, 1024)):
            src = w_in[:, c0:c0 + n].rearrange("(kc p) o -> p kc o", p=128)
            dv = wkv[:, :, dcol:dcol + n]
            S.dma("pool", (lambda dv=dv, src=src: lambda e: e.dma_start(out=dv, in_=src))(), writes=[wkv_res])
        wwi_src = w_in[:, 4224:4232].rearrange("(kc p) o -> p kc o", p=128)
        S.dma("pool", lambda e: e.dma_start(out=wwi, in_=wwi_src), writes=[wkv_res])

        w_in = convert("w_in", w_in, [D, 4232])

        kt_i = [0]
        vt_i = [0]
        wi_i = [0]

        def proj_store(ps, pr, ntok, cs, sn, tab_res, dst_ap, do_rope=True):
            i = kt_i[0] % 3
            kt_i[0] += 1
            kt, kr = kt_slots[i]
            ktv = kt[:, 0:ntok]
            S.op("act", lambda e: e.copy(out=ktv, in_=ps[:, 0:ntok]), [pr], [kr])
            if do_rope:
                ps2, pr2 = psrot.next()
                rope(kt, kr, cs, sn, tab_res, ntok, ps2, pr2, rtA, rtB, rt_res)
            S.dma("pool", lambda e: e.dma_start(out=dst_ap, in_=ktv), reads=[kr], writes=[kt_store[i]])

        def load_tabs(src, tok0, ntok):
            tb, tr_ = tabs.next()
            cs = tb[:, 0:512]
            sn = tb[:, 512:1024]
            S.dma("sp", lambda e: e.dma_start(out=cs[0:32, 0:ntok], in_=src[0, :, tok0:tok0 + ntok]), writes=[tr_])
            S.dma("sp", lambda e: e.dma_start(out=sn[0:32, 0:ntok], in_=src[1, :, tok0:tok0 + ntok]), writes=[tr_])
            return cs, sn, tr_

        for kg in range(cfg.nkg):
            tok0 = kg * 512
            load_xT(xk[tok0:tok0 + 512, :], 512, KC, hT, h_res, xslots, psrot)
            rmsnorm(hT, h_res, "g_mix0", 512, uT, u_res, tmp1, tmp2, t_res, psrot)
            cs, sn, tab_res = load_tabs(ropek, tok0, 512)
            for oc in range(5):
                ps, pr = psrot.next()
                col = oc * 128 if oc < 4 else 1024
                for kc in range(KC):
                    mm(ps[:, 0:512], wkv[:, kc, col:col + 128], uT[:, kc, 0:512], kc == 0, kc == KC - 1,
                       [u_res, wkv_res], pr)
                dst = KTs[oc, :, tok0:tok0 + 512] if oc < 4 else kiTs[:, tok0:tok0 + 512]
                proj_store(ps, pr, 512, cs, sn, tab_res, dst)
            for sub in range(4):
                ps, pr = psrot.next()
                for kc in range(KC):
                    mm(ps[:, 0:512], uT[:, kc, sub * 128:(sub + 1) * 128], wkv[:, kc, 512:1024], kc == 0, kc == KC - 1,
                       [u_res, wkv_res], pr)
                i = vt_i[0] % 2
                vt_i[0] += 1
                vt, vr = vt_slots[i]
                S.op("act", (lambda vt=vt, ps=ps: lambda e: e.copy(out=vt, in_=ps[:, 0:512]))(), [pr], [vr])
                dst = Vs[tok0 + sub * 128:tok0 + (sub + 1) * 128, :]
                S.dma("pool", (lambda dst=dst, vt=vt: lambda e: e.dma_start(out=dst, in_=vt))(), reads=[vr],
                      writes=[vt_store[i]])

        w_out = convert("w_out", w_out, [D, D], after=[h_res])
        mlp_w1 = convert("mlp_w1", mlp_w1, [2, D, 4 * D], after=[h_res])
        mlp_w2 = convert("mlp_w2", mlp_w2, [2, 4 * D, D], after=[h_res])
        pe_gate = convert("pe_gate", pe_gate, [2, D, D], after=[h_res])
        pe_proj = convert("pe_proj", pe_proj, [2, 256, D], after=[h_res])
        cw_in = convert("cw_in", cw_in, [D, 2 * D], after=[h_res])
        cw_out = convert("cw_out", cw_out, [D, D], after=[h_res])

        groups = []
        t = 0
        while t < NTL:
            s, i = cfg.tiles[t]
            if i < 0:
                groups.append((t, 1))
                t += 1
            else:
                groups.append((t, 4))
                t += 4
        for (t0, ntl) in groups:
            ntok = ntl * 128
            tok0 = t0 * 128
            load_xT(xq[tok0:tok0 + ntok, :], ntok, KC, hT, h_res, xslots, psrot)
            rmsnorm(hT, h_res, "g_mix0", ntok, uT, u_res, tmp1, tmp2, t_res, psrot)
            cs, sn, tab_res = load_tabs(ropeq, tok0, ntok)

            def q_evac(base):
                def ev(oc, ps, pr):
                    proj_store(ps, pr, ntok, cs, sn, tab_res, QTs[:, base + oc, tok0:tok0 + ntok])
                return ev
            inq = lambda kc: uT[:, kc, 0:ntok]
            linear(inq, [u_res], w_in, KC, 2048, ntok, wslots, psrot, q_evac(0), col_base=0)
            linear(inq, [u_res], w_in, KC, 1024, ntok, wslots, psrot, q_evac(16), col_base=3072)
            for sub in range(ntl):
                ps, pr = psrot.next()
                for kc in range(KC):
                    mm(ps[:, 0:8], uT[:, kc, sub * 128:(sub + 1) * 128], wwi[:, kc, 0:8], kc == 0, kc == KC - 1,
                       [u_res, wkv_res], pr)
                i = wi_i[0] % 2
                wi_i[0] += 1
                wt_, wr_ = wi_slots[i]
                S.op("act", (lambda wt_=wt_, ps=ps: lambda e: e.copy(out=wt_, in_=ps[:, 0:8]))(), [pr], [wr_])
                dst = WIs[tok0 + sub * 128:tok0 + (sub + 1) * 128, :]
                S.dma("pool", (lambda dst=dst, wt_=wt_: lambda e: e.dma_start(out=dst, in_=wt_))(), reads=[wr_],
                      writes=[wi_store[i]])
        S.barrier()

        C2 = Carver(PBASE)
        score = C2.f32(SEQ)
        maskq = C2.bf(SEQ)
        score_b = C2.bf(SEQ)
        sb_res = S.res("score_b")
        maskT = C2.bf(SEQ)
        mb_t = C2.f32(2 * cfg.nseg * NB_BIAS * 128).rearrange("p (a b) -> p a b", a=2 * cfg.nseg)
        s_res, mq_res, mt_res, mb_res = S.res("score"), S.res("maskq"), S.res("maskT"), S.res("mb")
        q_slots = Rot([(C2.bf(24 * 128).rearrange("p (a b) -> p a b", a=24), C2.f32(8), S.res("qs%d" % i)) for i in range(2)])
        ki_slots = Rot([(C2.bf(512), S.res("kis%d" % i)) for i in range(3)])
        kv_slots = Rot([(C2.bf(2048).rearrange("p (a b) -> p a b", a=4), C2.bf(2048).rearrange("p (a b) -> p a b", a=4),
                         S.res("kvs%d" % i)) for i in range(3)])
        r_slots = Rot([(C2.f32(512), S.res("rs%d" % i)) for i in range(3)])
        p_slots = Rot([(C2.bf(512), S.res("pt%d" % i)) for i in range(4)])
        sm = C2.f32(64)
        sm_res = S.res("sm")
        rden = C2.f32(512)
        rbc = C2.f32(512)
        rd_res, rb_res = S.res("rden"), S.res("rbc")
        o_slots = [(C2.bf(512), S.res("ot%d" % i)) for i in range(2)]
        o_store = [S.res("ost%d" % i) for i in range(2)]
        o_i = [0]
        WABS, WSGN, AMAX, RNG, LO, MID, CNT, DD, ZERO, KTH = 0, 8, 16, 17, 18, 19, 20, 21, 22, 23
        S.op("dve", lambda e: e.memset(sm[:, ZERO:ZERO + 1], 0.0), [], [sm_res])
        S.op("dve", lambda e: e.memset(sm[:, KTH:KTH + 1], TOPK - 0.5), [sm_res], [sm_res])

        for pi in range(2 * cfg.nseg):
            S.dma("sp", (lambda pi=pi: lambda e: e.dma_start(out=mb_t[:, pi, :], in_=mbias[pi, :, :]))(), writes=[mb_res])

        po = [(psf[i], psf_res[i]) for i in range(4)]
        pd, pd_res = psf[4], psf_res[4]
        ps_s = Rot([(psf[5], psf_res[5]), (psf[6], psf_res[6])])
        ps_all = Rot([(psf[i], psf_res[i]) for i in range(7)])
        scale = 128.0 ** -0.5

        tstate = {}

        def prep_a(t):
            s, ti = cfg.tiles[t]
            n = cfg.nkb[t]
            ncol = n * 128
            pat = 2 * s + (0 if ti < 0 else 1)
            qT, wi_t, q_res = q_slots.next()
            S.dma("sp", (lambda qT=qT, t=t: lambda e: e.dma_start(out=qT, in_=QTs[:, :, t * 128:(t + 1) * 128]))(),
                  writes=[q_res])
            S.dma("sp", (lambda wi_t=wi_t, t=t: lambda e: e.dma_start(out=wi_t, in_=WIs[t * 128:(t + 1) * 128, :]))(),
                  writes=[q_res])
            S.op("dve", (lambda wi_t=wi_t: lambda e: e.tensor_scalar(out=sm[:, 24:32], in0=wi_t, scalar1=-1.0,
                 scalar2=None, op0=ALU.mult))(), [q_res], [sm_res])
            S.op("dve", (lambda wi_t=wi_t: lambda e: e.tensor_tensor(out=sm[:, WABS:WABS + 8], in0=wi_t, in1=sm[:, 24:32],
                 op=ALU.max))(), [q_res, sm_res], [sm_res])
            S.op("dve", (lambda wi_t=wi_t: lambda e: e.tensor_scalar(out=sm[:, WSGN:WSGN + 8], in0=wi_t, scalar1=0.0,
                 scalar2=2.0, op0=ALU.is_ge, op1=ALU.mult))(), [q_res], [sm_res])
            S.op("dve", lambda e: e.tensor_scalar(out=sm[:, WSGN:WSGN + 8], in0=sm[:, WSGN:WSGN + 8], scalar1=-1.0,
                 scalar2=None, op0=ALU.add), [sm_res], [sm_res])
            ngrp = (n + 3) // 4
            for kg in range(ngrp):
                nb = min(4, n - kg * 4)
                cols = nb * 128
                kit, kir = ki_slots.next()
                S.dma("sp", (lambda kit=kit, kg=kg, cols=cols: lambda e: e.dma_start(
                    out=kit[:, 0:cols], in_=kiTs[:, kg * 512:kg * 512 + cols]))(), writes=[kir])
                sv = score[:, kg * 512:kg * 512 + cols]
                for h in range(8):
                    ps, pr = ps_all.next()
                    mm(ps[:, 0:cols], qT[:, 16 + h, :], kit[:, 0:cols], True, True, [q_res, kir], pr)
                    rt, rr = r_slots.next()
                    S.op("act", (lambda rt=rt, ps=ps, cols=cols, h=h: lambda e: e.activation(
                        out=rt[:, 0:cols], in_=ps[:, 0:cols], func=AF.Relu, scale=sm[:, WABS + h:WABS + h + 1]))(),
                        [pr, sm_res], [rr])
                    if h == 0:
                        S.op("dve", (lambda rt=rt, sv=sv, cols=cols: lambda e: e.tensor_scalar(
                            out=sv, in0=rt[:, 0:cols], scalar1=sm[:, WSGN:WSGN + 1], scalar2=None, op0=ALU.mult))(),
                            [rr, sm_res], [s_res])
                    else:
                        S.op("dve", (lambda rt=rt, sv=sv, cols=cols, h=h: lambda e: e.scalar_tensor_tensor(
                            out=sv, in0=rt[:, 0:cols], scalar=sm[:, WSGN + h:WSGN + h + 1], in1=sv,
                            op0=ALU.mult, op1=ALU.add))(), [rr, sm_res, s_res], [s_res])
            sc_v = score[:, 0:ncol]
            S.op("dve", lambda e, sc_v=sc_v: e.tensor_reduce(out=sm[:, AMAX:AMAX + 1], in_=sc_v, axis=AX.X, op=ALU.max,
                 apply_absolute_value=True), [s_res], [sm_res])
            S.op("dve", lambda e: e.tensor_scalar(out=sm[:, RNG:RNG + 1], in0=sm[:, AMAX:AMAX + 1], scalar1=2.0,
                 scalar2=None, op0=ALU.mult), [sm_res], [sm_res])
            S.op("dve", lambda e: e.tensor_scalar(out=sm[:, LO:LO + 1], in0=sm[:, AMAX:AMAX + 1], scalar1=-1.0,
                 scalar2=None, op0=ALU.mult), [sm_res], [sm_res])
            nbb = min(n, NB_BIAS)
            bv = score[:, (n - nbb) * 128:ncol]
            mv = mb_t[:, pat, (NB_BIAS - nbb) * 128:NB_BIAS * 128]
            S.op("dve", (lambda bv=bv, mv=mv: lambda e: e.tensor_tensor(out=bv, in0=bv, in1=mv, op=ALU.add))(),
                 [s_res, mb_res], [s_res])
            mq_v = maskq[:, 0:ncol]
            sc_f = sc_v
            sc_v = score_b[:, 0:ncol]
            S.op("act", (lambda sc_f=sc_f, sc_v=sc_v: lambda e: e.copy(out=sc_v, in_=sc_f))(), [s_res], [sb_res])
            for it in range(cfg.niter):
                ci = 0.5 ** (it + 1)
                S.op("dve", (lambda ci=ci: lambda e: e.scalar_tensor_tensor(
                    out=sm[:, MID:MID + 1], in0=sm[:, RNG:RNG + 1], scalar=ci, in1=sm[:, LO:LO + 1],
                    op0=ALU.mult, op1=ALU.add))(), [sm_res], [sm_res])
                S.op("dve", lambda e: e.memset(sm[:, CNT:CNT + 1], 0.0), [sm_res], [sm_res])
                S.op("dve", (lambda mq_v=mq_v, sc_v=sc_v: lambda e: e.tensor_scalar(
                    out=mq_v, in0=sc_v, scalar1=sm[:, MID:MID + 1], scalar2=sm[:, ZERO:ZERO + 1], op0=ALU.is_ge, op1=ALU.add,
                    accum_out=sm[:, CNT:CNT + 1]))(), [sb_res, sm_res, mq_res], [sm_res, mq_res])
                S.op("dve", lambda e: e.tensor_scalar(out=sm[:, DD:DD + 1], in0=sm[:, CNT:CNT + 1], scalar1=sm[:, KTH:KTH + 1],
                     scalar2=sm[:, RNG:RNG + 1], op0=ALU.is_ge, op1=ALU.mult), [sm_res], [sm_res])
                S.op("dve", (lambda ci=ci: lambda e: e.scalar_tensor_tensor(
                    out=sm[:, LO:LO + 1], in0=sm[:, DD:DD + 1], scalar=ci, in1=sm[:, LO:LO + 1],
                    op0=ALU.mult, op1=ALU.add))(), [sm_res], [sm_res])
            S.op("dve", (lambda mq_v=mq_v, sc_v=sc_v: lambda e: e.tensor_scalar(
                out=mq_v, in0=sc_v, scalar1=sm[:, LO:LO + 1], scalar2=None, op0=ALU.is_ge))(),
                [sb_res, sm_res], [mq_res])
            tstate[t] = (qT, q_res, n)

        def prep_b(t):
            qT, q_res, n = tstate[t]
            for b0 in range(0, n, 8):
                nb = min(8, n - b0)
                pbv = psb[:, 0:nb * 128]
                pbr = psb_res[0]
                for j in range(nb):
                    tr(psb[:, j * 128:(j + 1) * 128], maskq[:, (b0 + j) * 128:(b0 + j + 1) * 128],
                       idb, [mq_res, r_const], pbr)
                mtv = maskT[:, b0 * 128:(b0 + nb) * 128]
                S.op("dve", (lambda mtv=mtv, pbv=pbv: lambda e: e.tensor_scalar(out=mtv, in0=pbv, scalar1=-1.0, scalar2=30000.0,
                     op0=ALU.add, op1=ALU.mult))(), [pbr], [mt_res])

        def attend(t):
            qT, q_res, n = tstate[t]
            state = {"kv": None}

            def emit_qk(blk, g):
                kg, j = blk // 4, blk % 4
                if j == 0 and g == 0:
                    nb = min(4, n - blk)
                    ktt, vtt, kvr = kv_slots.next()
                    S.dma("sp", (lambda ktt=ktt, kg=kg, nb=nb: lambda e: e.dma_start(
                        out=ktt[:, :, 0:nb * 128], in_=KTs[:, :, kg * 512:kg * 512 + nb * 128].rearrange("g d k -> d g k")))(),
                        writes=[kvr])
                    S.dma("sp", (lambda vtt=vtt, kg=kg, nb=nb: lambda e: e.dma_start(
                        out=vtt[:, 0:nb, :], in_=Vs[kg * 512:kg * 512 + nb * 128, :].rearrange("(b p) c -> p b c", p=128)))(),
                        writes=[kvr])
                    state["kv"] = (ktt, vtt, kvr)
                ktt, vtt, kvr = state["kv"]
                mbc = maskT[:, blk * 128:(blk + 1) * 128].unsqueeze(1).to_broadcast([128, 4, 128])
                ps, pr = ps_s.next()
                mm(ps[:, :], ktt[:, g, j * 128:(j + 1) * 128], qT[:, 4 * g:4 * g + 4, :], True, False, [kvr, q_res], pr)
                mm(ps[:, :], idb, mbc, False, True, [mt_res, r_const], pr)
                pt, ptr = p_slots.next()
                S.op("act", (lambda pt=pt, ps=ps: lambda e: e.activation(out=pt, in_=ps[:, :], func=AF.Exp, scale=scale))(),
                     [pr], [ptr])
                return (blk, g, j, vtt, kvr, pt, ptr)

            def emit_pv(blk, g, j, vtt, kvr, pt, ptr):
                mm(po[g][0][:, :], vtt[:, j, g * 128:(g + 1) * 128], pt, blk == 0, blk == n - 1, [kvr, ptr], po[g][1])
                mm(pd[:, :], eqb[:, g, :], pt, blk == 0 and g == 0, blk == n - 1 and g == 3, [ptr, r_const], pd_res)

            pending = None
            for blk in range(n):
                for g in range(4):
                    cur = emit_qk(blk, g)
                    if pending is not None:
                        emit_pv(*pending)
                    pending = cur
            emit_pv(*pending)
            S.op("dve", lambda e: e.reciprocal(out=rden, in_=pd[:, :]), [pd_res], [rd_res])
            for g in range(4):
                ps, pr = ps_s.next()
                mm(ps[:, :], sel[:, g, :], rden, True, True, [rd_res, r_const], pr)
                S.op("act", (lambda ps=ps: lambda e: e.copy(out=rbc, in_=ps[:, :]))(), [pr], [rb_res])
                i = o_i[0] % 2
                o_i[0] += 1
                ot, orr = o_slots[i]
                S.op("dve", (lambda ot=ot, g=g: lambda e: e.tensor_tensor(out=ot, in0=po[g][0][:, :], in1=rbc, op=ALU.mult))(),
                     [po[g][1], rb_res], [orr])
                dst = OTs[:, 4 * g:4 * g + 4, t * 128:(t + 1) * 128]
                S.dma("pool", (lambda dst=dst, ot=ot: lambda e: e.dma_start(
                    out=dst, in_=ot.rearrange("p (a b) -> p a b", a=4)))(), reads=[orr], writes=[o_store[i]])

        prep_a(0)
        prep_b(0)
        for t in range(NTL):
            if t + 1 < NTL:
                prep_a(t + 1)
            attend(t)
            if t + 1 < NTL:
                prep_b(t + 1)
        S.barrier()

        C3 = Carver(PBASE)
        hT = C3.f32(KC * 512).rearrange("p (a b) -> p a b", a=KC)
        uT = C3.bf(KC * 512).rearrange("p (a b) -> p a b", a=KC)
        h_res, u_res = S.res("hT3"), S.res("uT3")
        bigw = 32 * 512 // 2
        big_f = C3.f32(bigw)
        big_res = S.res("big")
        hid = big_f.bitcast(BF16).rearrange("p (a b) -> p a b", a=32)
        aT = big_f[:, 0:4096].bitcast(BF16).rearrange("p (a b) -> p a b", a=KC)
        peT = big_f.rearrange("p (a b) -> p a b", a=KC)
        zT = big_f.rearrange("p (a b) -> p a b", a=KC)
        sT = C3.bf(KC * 512).rearrange("p (a b) -> p a b", a=KC)
        s_res3 = S.res("sT")
        pT = C3.bf(2 * 512).rearrange("p (a b) -> p a b", a=2)
        p_res = S.res("pT")
        YW = 30 + 512
        yT = C3.bf(KC * YW).rearrange("p (a b) -> p a b", a=KC)
        y_res = S.res("yT")
        xslots = Rot([(C3.f32(2048), S.res("xs3_%d" % i)) for i in range(2)])
        wslots = Rot([(C3.bf(8192), S.res("ws3_%d" % i)) for i in range(2)])
        tmp1, tmp2 = C3.f32(512), C3.f32(512)
        t_res = S.res("ntmp3")
        e_slots = Rot([(C3.f32(512), S.res("et%d" % i)) for i in range(3)])
        sig4 = C3.f32(4 * 512).rearrange("p (a b) -> p a b", a=4)
        sig_res = S.res("sig4")
        mean_t, rstd_t = C3.f32(512), C3.f32(512)
        ln_res = S.res("ln")
        out_store = [S.res("outst%d" % i) for i in range(2)]
        dg_halves = [(C3.bf(16 * 128).rearrange("p (a b) -> p a b", a=16), S.res("dg%d" % i)) for i in range(2)]
        psrot = Rot([(psf[i], psf_res[i]) for i in range(7)])
        a_store = S.res("aload")

        S.op("dve", lambda e: e.memset(yT[:, :, 0:30], 0.0), [], [y_res])

        def add_res_evac(ntok, bias_name=None):
            def ev(oc, ps, pr):
                if bias_name is None:
                    S.op("dve", lambda e: e.tensor_tensor(out=hT[:, oc, 0:ntok], in0=ps[:, 0:ntok], in1=hT[:, oc, 0:ntok],
                         op=ALU.add), [pr, h_res], [h_res])
                else:
                    b = spc(bias_name, oc)
                    S.op("dve", lambda e: e.scalar_tensor_tensor(out=hT[:, oc, 0:ntok], in0=ps[:, 0:ntok], scalar=b,
                         in1=hT[:, oc, 0:ntok], op0=ALU.add, op1=ALU.add), [pr, h_res, r_const], [h_res])
            return ev

        def mlp(layer, ntok):
            rmsnorm(hT, h_res, "g_mlp%d" % layer, ntok, uT, u_res, tmp1, tmp2, t_res, psrot)
            for half in range(2):
                cnt = [0]

                def hid_evac(oc, ps, pr):
                    et, er = e_slots.next()
                    S.op("act", lambda e: e.activation(out=et[:, 0:ntok], in_=ps[:, 0:ntok], func=AF.Relu), [pr], [er])
                    eng = "dve" if cnt[0] % 2 == 0 else "pool"
                    cnt[0] += 1
                    S.op(eng, lambda e: e.tensor_tensor(out=hid[:, oc, 0:ntok], in0=et[:, 0:ntok], in1=et[:, 0:ntok],
                         op=ALU.mult), [er], [big_res])
                linear(lambda kc: uT[:, kc, 0:ntok], [u_res], mlp_w1[layer], KC, 4096, ntok, wslots, psrot, hid_evac,
                       col_base=half * 4096)
                linear(lambda kc: hid[:, kc, 0:ntok], [big_res], mlp_w2[layer][half * 4096:(half + 1) * 4096, :], 32, D,
                       ntok, wslots, psrot, add_res_evac(ntok))

        def pe_gate_block(layer, ntok, tok0):
            load_xT(pq[layer, tok0:tok0 + ntok, :], ntok, 2, pT, p_res, xslots, psrot)

            def pe_evac(oc, ps, pr):
                S.op("act", lambda e: e.copy(out=peT[:, oc, 0:ntok], in_=ps[:, 0:ntok]), [pr], [big_res])
            linear(lambda kc: pT[:, kc, 0:ntok], [p_res], pe_proj[layer], 2, D, ntok, wslots, psrot, pe_evac)
            rmsnorm(hT, h_res, "g_gate%d" % layer, ntok, uT, u_res, tmp1, tmp2, t_res, psrot)

            def gate_evac(oc, ps, pr):
                et, er = e_slots.next()
                S.op("act", lambda e: e.activation(out=et[:, 0:ntok], in_=ps[:, 0:ntok], func=AF.Sigmoid), [pr], [er])
                S.op("dve", lambda e: e.tensor_tensor(out=et[:, 0:ntok], in0=et[:, 0:ntok], in1=peT[:, oc, 0:ntok],
                     op=ALU.mult), [er, big_res], [er])
                S.op("dve", lambda e: e.tensor_tensor(out=hT[:, oc, 0:ntok], in0=et[:, 0:ntok], in1=hT[:, oc, 0:ntok],
                     op=ALU.add), [er, h_res], [h_res])
            linear(lambda kc: uT[:, kc, 0:ntok], [u_res], pe_gate[layer], KC, D, ntok, wslots, psrot, gate_evac)

        def conv_in_glu(ntok):
            rmsnorm(hT, h_res, "g_mix1", ntok, uT, u_res, tmp1, tmp2, t_res, psrot)
            for tq in range(4):
                def g_evac(oc, ps, pr, tq=tq):
                    j = oc - 4 * tq
                    b = spc("b_in", 16 + oc)
                    S.op("act", lambda e: e.activation(out=sig4[:, j, 0:ntok], in_=ps[:, 0:ntok], func=AF.Sigmoid, bias=b),
                         [pr, r_const], [sig_res])
                linear(lambda kc: uT[:, kc, 0:ntok], [u_res], cw_in, KC, 512, ntok, wslots, psrot,
                       lambda oc, ps, pr, tq=tq: g_evac(oc + 4 * tq, ps, pr), col_base=2048 + tq * 512)

                def a_evac(oc, ps, pr, tq=tq):
                    j = oc - 4 * tq
                    b = spc("b_in", oc)
                    S.op("dve", lambda e: e.scalar_tensor_tensor(out=yT[:, oc, 30:30 + ntok], in0=ps[:, 0:ntok], scalar=b,
                         in1=sig4[:, j, 0:ntok], op0=ALU.add, op1=ALU.mult), [pr, sig_res, r_const], [y_res])
                linear(lambda kc: uT[:, kc, 0:ntok], [u_res], cw_in, KC, 512, ntok, wslots, psrot,
                       lambda oc, ps, pr, tq=tq: a_evac(oc + 4 * tq, ps, pr), col_base=tq * 512)

        def layer0_dense(t0, ntl):
            ntok = ntl * 128
            tok0 = t0 * 128
            load_xT(xq[tok0:tok0 + ntok, :], ntok, KC, hT, h_res, xslots, psrot)
            S.dma("sp", lambda e: e.dma_start(out=aT[:, :, 0:ntok], in_=OTs[:, :, tok0:tok0 + ntok]), writes=[big_res])
            linear(lambda kc: aT[:, kc, 0:ntok], [big_res], w_out, KC, D, ntok, wslots, psrot, add_res_evac(ntok))
            mlp(0, ntok)
            pe_gate_block(0, ntok, tok0)

        otile_i = [0]

        def dense_group(t0, ntl):
            s, i0 = cfg.tiles[t0]
            ntok = ntl * 128
            tok0 = t0 * 128
            layer0_dense(t0, ntl)
            conv_in_glu(ntok)
            if i0 < 0:
                hs = spc("hscale", s)
                S.op("dve", lambda e, hs=hs: e.tensor_scalar(out=yT[:, :, 0:30], in0=yT[:, :, 128:158], scalar1=hs,
                     scalar2=None, op0=ALU.mult), [y_res, r_const], [y_res])
                return
            for c in range(KC):
                ps, pr = psrot.next()
                for hf in range(2):
                    dg, dgr = dg_halves[hf]
                    j0, j1 = (0, 16) if hf == 0 else (16, 31)
                    for j in range(j0, j1):
                        wj = spc("w_dw", c * 31 + j)
                        if j % 2 == 0:
                            S.op("dve", lambda e, dg=dg, j=j, j0=j0, wj=wj: e.tensor_scalar(out=dg[:, j - j0, :], in0=idb, scalar1=wj,
                                 scalar2=None, op0=ALU.mult), [r_const], [dgr])
                        else:
                            S.op("act", lambda e, dg=dg, j=j, j0=j0, wj=wj: e.mul(out=dg[:, j - j0, :], in_=idb, mul=wj),
                                 [r_const], [dgr])
                    for j in range(j0, j1):
                        mm(ps[:, 0:ntok], dg[:, j - j0, :], yT[:, c, j:j + ntok], j == 0, j == 30, [dgr, y_res], pr)
                bdw = spc("b_dw", c)
                S.op("dve", lambda e, c=c, ps=ps, bdw=bdw: e.tensor_scalar(out=zT[:, c, 0:ntok], in0=ps[:, 0:ntok], scalar1=bdw,
                     scalar2=None, op0=ALU.add), [pr, r_const, big_res], [big_res])
            S.op("dve", lambda e: e.tensor_copy(out=yT[:, :, 0:30], in_=yT[:, :, ntok:ntok + 30]), [y_res], [y_res])
            S.op("act", lambda e: e.copy(out=sT[:, :, 0:ntok], in_=zT[:, :, 0:ntok]), [big_res], [s_res3])
            S.op("act", lambda e: e.activation(out=uT[:, :, 0:ntok], in_=zT[:, :, 0:ntok], func=AF.Square), [big_res], [u_res])
            ps, pr = psrot.next()
            for kc in range(KC):
                mm(ps[:, 0:ntok], onesD, sT[:, kc, 0:ntok], kc == 0, kc == KC - 1, [s_res3, r_const], pr)
            ps2, pr2 = psrot.next()
            for kc in range(KC):
                mm(ps2[:, 0:ntok], onesD, uT[:, kc, 0:ntok], kc == 0, kc == KC - 1, [u_res, r_const], pr2)
            S.op("act", lambda e: e.copy(out=mean_t[:, 0:ntok], in_=ps[:, 0:ntok]), [pr], [ln_res])
            S.op("dve", lambda e: e.tensor_tensor(out=tmp1[:, 0:ntok], in0=mean_t[:, 0:ntok], in1=mean_t[:, 0:ntok], op=ALU.mult),
                 [ln_res], [t_res])
            S.op("dve", lambda e: e.tensor_tensor(out=tmp1[:, 0:ntok], in0=ps2[:, 0:ntok], in1=tmp1[:, 0:ntok], op=ALU.subtract),
                 [pr2, t_res], [t_res])
            S.op("dve", lambda e: e.tensor_scalar(out=tmp1[:, 0:ntok], in0=tmp1[:, 0:ntok], scalar1=0.0, scalar2=None,
                 op0=ALU.max), [t_res], [t_res])
            S.op("act", lambda e: e.activation(out=tmp2[:, 0:ntok], in_=tmp1[:, 0:ntok], func=AF.Sqrt, bias=EPS, scale=1.0),
                 [t_res], [t_res])
            S.op("dve", lambda e: e.reciprocal(out=rstd_t[:, 0:ntok], in_=tmp2[:, 0:ntok]), [t_res], [ln_res])
            for c in range(KC):
                et, er = e_slots.next()
                S.op("dve", lambda e, c=c, et=et: e.tensor_tensor(out=et[:, 0:ntok], in0=zT[:, c, 0:ntok], in1=mean_t[:, 0:ntok],
                     op=ALU.subtract), [big_res, ln_res], [er])
                S.op("dve", lambda e, et=et: e.tensor_tensor(out=et[:, 0:ntok], in0=et[:, 0:ntok], in1=rstd_t[:, 0:ntok],
                     op=ALU.mult), [er, ln_res], [er])
                lg, lb = spc("ln_g", c), spc("ln_b", c)
                S.op("act", lambda e, c=c, et=et, lg=lg, lb=lb: e.activation(out=sT[:, c, 0:ntok], in_=et[:, 0:ntok],
                     func=AF.Silu, bias=lb, scale=lg), [er, r_const], [s_res3])
            linear(lambda kc: sT[:, kc, 0:ntok], [s_res3], cw_out, KC, D, ntok, wslots, psrot, add_res_evac(ntok, "b_out"))
            mlp(1, ntok)
            pe_gate_block(1, ntok, tok0)
            outT = peT
            rmsnorm(hT, h_res, "g_final", ntok, uT, u_res, tmp1, tmp2, t_res, psrot, out_view=outT, out_res=big_res)
            seg_out_tile0 = s * cfg.seg_tiles + i0
            for sub in range(ntl):
                xt, xr = xslots.next()
                for d0 in range(0, KC, 4):
                    ps, pr = psrot.next()
                    for j in range(4):
                        tr(ps[:, j * 128:(j + 1) * 128], outT[:, d0 + j, sub * 128:(sub + 1) * 128], idf, [u_res, big_res, r_const], pr)
                    if (d0 // 4) % 2 == 0:
                        S.op("act", lambda e, xt=xt, ps=ps, d0=d0: e.copy(out=xt[:, d0 * 128:(d0 + 4) * 128], in_=ps[:, :]), [pr], [xr])
                    else:
                        S.op("dve", lambda e, xt=xt, ps=ps, d0=d0: e.tensor_copy(out=xt[:, d0 * 128:(d0 + 4) * 128], in_=ps[:, :]), [pr], [xr])
                row0 = (seg_out_tile0 + sub) * 128
                k = otile_i[0] % 2
                otile_i[0] += 1
                S.dma("pool", lambda e, xt=xt, row0=row0: e.dma_start(out=out[row0:row0 + 128, :], in_=xt), reads=[xr],
                      writes=[out_store[k]])

        for (t0, ntl) in groups:
            dense_group(t0, ntl)
        S.wait_all("sp", out_store)
        S.barrier()
        with nc.Block() as block:
            stats = S.finalize(block)
        build_nc.stats = stats
    return nc


def rope_tables(pos):
    half = 16
    inv = (np.float32(500000.0) ** (-np.arange(half, dtype=np.float32) * np.float32(2.0) / np.float32(32))).astype(np.float32)
    ang = pos.astype(np.float32)[None, :] * inv[:, None]
    cs = np.cos(ang).astype(np.float32)
    sn = np.sin(ang).astype(np.float32)
    return np.stack([np.concatenate([cs, cs], 0), np.concatenate([sn, sn], 0)], 0)


def make_inputs(cfg, inp):
    lay, NSP = small_param_layout()
    x = np.asarray(inp["x"], dtype=np.float32)
    p = np.asarray(inp["p"], dtype=np.float32)
    NKEY = cfg.nkg * 512
    ident = np.eye(128, dtype=np.float32)
    rot = np.zeros((32, 32), np.float32)
    for m in range(16):
        rot[m + 16, m] = -1.0
        rot[m, m + 16] = 1.0
    csel = np.zeros((128, 4, 128), np.float32)
    ceq = np.zeros((128, 4, 128), np.float32)
    for g in range(4):
        csel[32 * g, g, :] = 1.0
        ceq[:, g, 32 * g:32 * g + 32] = 1.0
    ropek = rope_tables(np.arange(NKEY))
    shared = {
        "c_ident": ident, "c_rot": rot, "c_sel": csel, "c_eq": ceq, "ropek": ropek,
        "dsa_w_in": np.ascontiguousarray(inp["dsa_w_in"][0]), "dsa_w_out": np.ascontiguousarray(inp["dsa_w_out"][0]),
        "mlp_w1": np.asarray(inp["mlp_w1"]), "mlp_w2": np.asarray(inp["mlp_w2"]),
        "pe_proj": np.asarray(inp["pe_proj"]), "pe_gate": np.asarray(inp["pe_gate"]),
        "conv_w_in": np.ascontiguousarray(inp["conv_w_in"][0]), "conv_w_out": np.ascontiguousarray(inp["conv_w_out"][0]),
    }
    sp_base = np.zeros((128, NSP), np.float32)

    def put(name, arr):
        o, n = lay[name]
        assert arr.shape == (128, n), (name, arr.shape)
        sp_base[:, o:o + n] = arr
    put("g_mix0", fm(inp["mix_norm"][0])); put("g_mlp0", fm(inp["mlp_norm"][0])); put("g_gate0", fm(inp["pe_gate_norm"][0]))
    put("g_mix1", fm(inp["mix_norm"][1])); put("g_mlp1", fm(inp["mlp_norm"][1])); put("g_gate1", fm(inp["pe_gate_norm"][1]))
    put("g_final", fm(inp["final_norm"]))
    put("b_in", fm(inp["conv_b_in"][0])); put("b_dw", fm(inp["conv_b_dw"][0])); put("ln_g", fm(inp["conv_ln_g"][0]))
    put("ln_b", fm(inp["conv_ln_b"][0])); put("b_out", fm(inp["conv_b_out"][0]))
    wdw = np.asarray(inp["conv_w_dw"][0], np.float32)
    wdw_fm = wdw.T.reshape(16, 128, 31).transpose(1, 0, 2).reshape(128, 16 * 31)
    put("w_dw", np.ascontiguousarray(wdw_fm))
    in_maps = []
    tile_blocks = []
    for c in range(8):
        b, half = c // 2, c % 2
        blocks = [cfg.block_of(half, t) for t in range(cfg.ntl)]
        tile_blocks.append(blocks)
        rows = np.concatenate([np.arange(j * 128, (j + 1) * 128) for j in blocks])
        m = dict(shared)
        m["xk"] = np.ascontiguousarray(x[b, 0:NKEY])
        m["xq"] = np.ascontiguousarray(x[b, rows])
        m["pq"] = np.ascontiguousarray(p[:, b][:, rows])
        m["ropeq"] = np.ascontiguousarray(rope_tables(rows))
        mb = np.zeros((2 * cfg.nseg, 128, NB_BIAS * 128), np.float32)
        for t in range(cfg.ntl):
            s, i = cfg.tiles[t]
            pat = 2 * s + (0 if i < 0 else 1)
            if i > 0:
                continue
            n = cfg.nkb[t]
            j = blocks[t]
            dpos = NB_BIAS - (n - j)
            assert 0 <= dpos < NB_BIAS, (c, t, n, j)
            pm = np.zeros((128, NB_BIAS, 128), np.float32)
            pm[:, dpos + 1:, :] = NEG
            pm[0:64, dpos, 64:128] = NEG
            mb[pat] = pm.reshape(128, NB_BIAS * 128)
        m["mbias"] = mb
        sp = sp_base.copy()
        o, _ = lay["hscale"]
        for s in range(cfg.nseg):
            sp[:, o + s] = cfg.halo_scale(half, s)
        m["smallp"] = sp
        in_maps.append(m)
    return in_maps, tile_blocks


_CFG = None


def kernel(**inputs):
    cfg = _CFG or Cfg()
    nc = build_nc(cfg)
    in_maps, tile_blocks = make_inputs(cfg, inputs)
    res = run_bass_kernel_spmd(nc, in_maps, core_ids=list(range(8)))
    B, S_, D_ = inputs["x"].shape
    outp = np.zeros((B, S_, D_), np.float32)
    for c in range(8):
        b = c // 2
        o = np.asarray(res.results[c]["out"])
        k = 0
        for t in range(cfg.ntl):
            s, i = cfg.tiles[t]
            if i < 0:
                continue
            j = tile_blocks[c][t]
            outp[b, j * 128:(j + 1) * 128, :] = o[k * 128:(k + 1) * 128]
            k += 1
    return outp
```

```python
import numpy as np
from contextlib import ExitStack
import concourse.bass as bass
import concourse.mybir as mybir
from concourse.bass_utils import run_bass_kernel_spmd

F32 = mybir.dt.float32
BF16 = mybir.dt.bfloat16
ALU = mybir.AluOpType
AF = mybir.ActivationFunctionType
AX = mybir.AxisListType

D = 2048
KC = 16
SEQ = 8192
NB_BIAS = 17
NEG = -1.0e30
EPS = 1e-6
TOPK = 256.0


class Res:
    __slots__ = ("name", "lw", "rd", "sem", "dcount")

    def __init__(self, name):
        self.name = name
        self.lw = None
        self.rd = []
        self.sem = None
        self.dcount = 0


class Op:
    __slots__ = ("eng", "fn", "deps", "inc", "seq", "dma_res", "dma_val", "idx")

    def __init__(self, eng, fn):
        self.eng = eng
        self.fn = fn
        self.idx = 0
        self.deps = {}
        self.inc = False
        self.seq = 0
        self.dma_res = None
        self.dma_val = 0


ENGS = ("pe", "act", "dve", "pool", "sp")


class Sched:
    def __init__(self, nc, stack):
        self.nc = nc
        self.stack = stack
        self.ops = {e: [] for e in ENGS}
        self.esem = {}
        for e in ("pe", "act", "dve", "pool"):
            self.esem[e] = stack.enter_context(nc.semaphore("sem_" + e))
        self.nres = 0
        self.dma_res = []

    def res(self, name=None):
        self.nres += 1
        return Res(name or ("r%d" % self.nres))

    @staticmethod
    def _add(deps, d):
        if d[0] == "op":
            key = d[1].eng
            cur = deps.get(key)
            if cur is None or cur[1].idx < d[1].idx:
                deps[key] = d
        else:
            key = d[1]
            cur = deps.get(key)
            if cur is None or cur[2] < d[2]:
                deps[key] = d

    def _collect(self, op, reads, writes):
        deps = op.deps
        for r in reads:
            if r.lw is not None:
                self._add(deps, r.lw)
        for w in writes:
            if w.lw is not None:
                self._add(deps, w.lw)
            for d in w.rd:
                self._add(deps, d)

    def op(self, eng, fn, reads=(), writes=()):
        o = Op(eng, fn)
        self._collect(o, reads, writes)
        me = ("op", o)
        for r in reads:
            r.rd = [d for d in r.rd if not (d[0] == "op" and d[1].eng == eng)]
            r.rd.append(me)
        for w in writes:
            w.lw = me
            w.rd = []
        o.idx = len(self.ops[eng])
        self.ops[eng].append(o)
        return o

    def dma(self, queue, fn, reads=(), writes=()):
        o = Op(queue, fn)
        self._collect(o, reads, writes)
        pr = writes[0]
        if pr.sem is None:
            pr.sem = self.stack.enter_context(self.nc.semaphore("d_" + pr.name))
            self.dma_res.append(pr)
        pr.dcount += 16
        o.dma_res = pr
        o.dma_val = pr.dcount
        me = ("dma", pr, pr.dcount)
        for r in reads:
            r.rd.append(me)
        for w in writes:
            w.lw = me
            w.rd = []
        o.idx = len(self.ops[queue])
        self.ops[queue].append(o)
        return o

    def wait_all(self, eng, reads):
        o = Op(eng, None)
        self._collect(o, reads, ())
        o.idx = len(self.ops[eng])
        self.ops[eng].append(o)
        return o

    def barrier(self):
        deps = []
        for e in ("pe", "act", "dve", "pool"):
            if self.ops[e]:
                for o in reversed(self.ops[e]):
                    if o.fn is not None and o.dma_res is None:
                        deps.append(("op", o))
                        break
        for r in self.dma_res:
            deps.append(("dma", r, r.dcount))
        for e in ENGS:
            o = Op(e, None)
            for d in deps:
                self._add(o.deps, d)
            o.idx = len(self.ops[e])
            self.ops[e].append(o)

    def finalize(self, block):
        for e in ENGS:
            for o in self.ops[e]:
                for d in o.deps.values():
                    if d[0] == "op":
                        p = d[1]
                        if p.eng == "pe" and o.eng == "pe":
                            continue
                        p.inc = True
        for e in ENGS:
            n = 0
            for o in self.ops[e]:
                if o.inc:
                    n += 1
                    o.seq = n
        stats = {}

        def emit(engname, engobj):
            seen = {}
            nw = 0
            for o in self.ops[engname]:
                for d in o.deps.values():
                    if d[0] == "op":
                        p = d[1]
                        if p.eng == "pe" and engname == "pe":
                            continue
                        sem = self.esem[p.eng]
                        val = p.seq
                        key = p.eng
                    else:
                        sem = d[1].sem
                        val = d[2]
                        key = d[1]
                    if seen.get(key, 0) >= val:
                        continue
                    seen[key] = val
                    engobj.wait_ge(sem, val)
                    nw += 1
                if o.fn is None:
                    continue
                ins = o.fn(engobj)
                if o.dma_res is not None:
                    ins.then_inc(o.dma_res.sem, 16)
                elif o.inc:
                    ins.then_inc(self.esem[engname], 1)
            stats[engname] = (len(self.ops[engname]), nw)

        @block.tensor
        def _(e):
            emit("pe", e)

        @block.scalar
        def _(e):
            emit("act", e)

        @block.vector
        def _(e):
            emit("dve", e)

        @block.gpsimd
        def _(e):
            emit("pool", e)

        @block.sync
        def _(e):
            emit("sp", e)

        return stats


class Cfg:
    def __init__(self, seg_starts=((0, 48), (16, 32)), seg_tiles=16, nkg=16, niter=16):
        self.seg_starts = seg_starts
        self.nseg = len(seg_starts[0])
        self.seg_tiles = seg_tiles
        self.nkg = nkg
        self.niter = niter
        self.tiles = []
        for s in range(self.nseg):
            self.tiles.append((s, -1))
            for i in range(seg_tiles):
                self.tiles.append((s, i))
        self.ntl = len(self.tiles)
        self.nkb = []
        for (s, i) in self.tiles:
            m = max(seg_starts[0][s], seg_starts[1][s])
            self.nkb.append(max(m + i + 1, 1) if i >= 0 else max(m, 1))
        assert max(self.nkb) <= nkg * 4

    def block_of(self, half, t):
        s, i = self.tiles[t]
        st = self.seg_starts[half][s]
        if i >= 0:
            return st + i
        return st - 1 if st > 0 else 0

    def halo_scale(self, half, s):
        return 1.0 if self.seg_starts[half][s] > 0 else 0.0


def small_param_layout():
    lay = {}
    off = 0
    for nm, n in (("g_mix0", 16), ("g_mlp0", 16), ("g_gate0", 16), ("g_mix1", 16), ("g_mlp1", 16),
                  ("g_gate1", 16), ("g_final", 16), ("b_in", 32), ("b_dw", 16), ("ln_g", 16),
                  ("ln_b", 16), ("b_out", 16), ("w_dw", 16 * 31), ("hscale", 2)):
        lay[nm] = (off, n)
        off += n
    return lay, off


def fm(v):
    v = np.asarray(v, dtype=np.float32)
    return np.ascontiguousarray(v.reshape(-1, 128).T)


def build_nc(cfg):
    nc = bass.Bass("TRN2", target_bir_lowering=False)
    NTL = cfg.ntl
    NTOK = NTL * 128
    NOUT = cfg.nseg * cfg.seg_tiles * 128
    NKEY = cfg.nkg * 512
    lay, NSP = small_param_layout()

    def din(name, shape):
        return nc.dram_tensor(name, list(shape), F32, kind="ExternalInput").ap()

    xk = din("xk", [NKEY, D])
    xq = din("xq", [NTOK, D])
    pq = din("pq", [2, NTOK, 256])
    ropek = din("ropek", [2, 32, NKEY])
    ropeq = din("ropeq", [2, 32, NTOK])
    mbias = din("mbias", [2 * cfg.nseg, 128, NB_BIAS * 128])
    c_ident = din("c_ident", [128, 128])
    c_rot = din("c_rot", [32, 32])
    c_sel = din("c_sel", [128, 4, 128])
    c_eq = din("c_eq", [128, 4, 128])
    smallp = din("smallp", [128, NSP])
    w_in = din("dsa_w_in", [D, 4232])
    w_out = din("dsa_w_out", [D, D])
    mlp_w1 = din("mlp_w1", [2, D, 4 * D])
    mlp_w2 = din("mlp_w2", [2, 4 * D, D])
    pe_proj = din("pe_proj", [2, 256, D])
    pe_gate = din("pe_gate", [2, D, D])
    cw_in = din("conv_w_in", [D, 2 * D])
    cw_out = din("conv_w_out", [D, D])
    out = nc.dram_tensor("out", [NOUT, D], F32, kind="ExternalOutput").ap()

    def dscr(name, shape, dt):
        return nc.dram_tensor(name, list(shape), dt, kind="Internal").ap()

    KTs = dscr("KTs", [4, 128, NKEY], BF16)
    kiTs = dscr("kiTs", [128, NKEY], BF16)
    Vs = dscr("Vs", [NKEY, 512], BF16)
    QTs = dscr("QTs", [128, 24, NTOK], BF16)
    WIs = dscr("WIs", [NTOK, 8], F32)
    OTs = dscr("OTs", [128, 16, NTOK], BF16)

    st = ExitStack()
    with st:
        S = Sched(nc, st)
        ARENA = 52400
        arena = st.enter_context(nc.sbuf_tensor("arena", [128, ARENA], F32))
        psf = [st.enter_context(nc.psum_tensor("psf%d" % i, [128, 512], F32)) for i in range(7)]
        psb = st.enter_context(nc.psum_tensor("psb", [128, 1024], BF16))
        psf_res = [S.res("psf%d" % i) for i in range(7)]
        psb_res = [S.res("psbA"), S.res("psbB")]

        class Carver:
            def __init__(self, base):
                self.pos = base

            def f32(self, n):
                a = arena[:, self.pos:self.pos + n]
                self.pos += n
                assert self.pos <= ARENA, self.pos
                return a

            def bf(self, n):
                w = (n + 1) // 2
                a = arena[:, self.pos:self.pos + w].bitcast(BF16)
                self.pos += w
                assert self.pos <= ARENA, self.pos
                return a

        C0 = Carver(0)
        idf = C0.f32(128)
        idb = C0.bf(128)
        rotb = C0.bf(32)
        sel = C0.f32(512).rearrange("p (a b) -> p a b", a=4)
        eqb = C0.bf(512).rearrange("p (a b) -> p a b", a=4)
        sp_t = C0.f32(NSP)
        onesD = C0.bf(128)
        r_const = S.res("const")
        S.dma("sp", lambda e: e.dma_start(out=idf, in_=c_ident[:, :]), writes=[r_const])
        S.dma("sp", lambda e: e.dma_start(out=sel, in_=c_sel[:, :, :]), writes=[r_const])
        S.dma("sp", lambda e: e.dma_start(out=sp_t, in_=smallp[:, :]), writes=[r_const])
        r_constp = S.res("constp")
        S.dma("pool", lambda e: e.dma_start(out=idb, in_=c_ident[:, :]), writes=[r_constp])
        S.dma("pool", lambda e: e.dma_start(out=rotb[0:32, :], in_=c_rot[:, :]), writes=[r_constp])
        S.dma("pool", lambda e: e.dma_start(out=eqb, in_=c_eq[:, :, :]), writes=[r_constp])
        S.op("dve", lambda e: e.memset(onesD, 1.0 / D), writes=[r_const])
        S.barrier()
        PBASE = C0.pos

        def spc(name, j=0, n=1):
            o, _ = lay[name]
            return sp_t[:, o + j:o + j + n]

        class Rot:
            def __init__(self, items):
                self.items = items
                self.i = 0

            def next(self):
                it = self.items[self.i % len(self.items)]
                self.i += 1
                return it

        def mm(ps_ap, lhsT, rhs, start, stop, reads, wres):
            S.op("pe", lambda e: e.matmul(ps_ap, lhsT=lhsT, rhs=rhs, start=start, stop=stop), reads, [wres])

        def tr(ps_ap, in_ap, ident, reads, wres):
            S.op("pe", lambda e: e.transpose(ps_ap, in_ap, ident), reads, [wres])

        def load_xT(src_rows, ntok, kcx, dst, dst_res, xslots, psrot, out_bf=False):
            nsub = ntok // 128
            cnt = 0
            for sub in range(nsub):
                xt, xr = xslots.next()
                xv = xt[:, 0:kcx * 128]
                rows = src_rows[sub * 128:(sub + 1) * 128, :]
                S.dma("sp", (lambda xv=xv, rows=rows: lambda e: e.dma_start(out=xv, in_=rows))(), writes=[xr])
                for k0 in range(0, kcx, 4):
                    nk = min(4, kcx - k0)
                    ps, pr = psrot.next()
                    for j in range(nk):
                        tr(ps[:, j * 128:(j + 1) * 128], xv[:, (k0 + j) * 128:(k0 + j + 1) * 128], idf, [xr, r_const], pr)
                    src = ps[:, 0:nk * 128].rearrange("p (a b) -> p a b", a=nk)
                    dv = dst[:, k0:k0 + nk, sub * 128:(sub + 1) * 128]
                    if cnt % 2 == 0:
                        S.op("act", (lambda dv=dv, src=src: lambda e: e.copy(out=dv, in_=src))(), [pr], [dst_res])
                    else:
                        S.op("dve", (lambda dv=dv, src=src: lambda e: e.tensor_copy(out=dv, in_=src))(), [pr], [dst_res])
                    cnt += 1

        def rmsnorm(hT, h_res, gname, ntok, uT, u_res, tmp1, tmp2, t_res, psrot, out_view=None, out_res=None):
            hv = hT[:, :, 0:ntok]
            uv = uT[:, :, 0:ntok]
            S.op("act", lambda e: e.activation(out=uv, in_=hv, func=AF.Square), [h_res], [u_res])
            ps, pr = psrot.next()
            for kc in range(KC):
                mm(ps[:, 0:ntok], onesD, uT[:, kc, 0:ntok], kc == 0, kc == KC - 1, [u_res, r_const], pr)
            t1 = tmp1[:, 0:ntok]
            t2 = tmp2[:, 0:ntok]
            S.op("act", lambda e: e.activation(out=t1, in_=ps[:, 0:ntok], func=AF.Sqrt, bias=EPS, scale=1.0), [pr], [t_res])
            S.op("dve", lambda e: e.reciprocal(out=t2, in_=t1), [t_res], [t_res])
            ov = out_view if out_view is not None else uT
            ores = out_res if out_res is not None else u_res
            for kc in range(KC):
                g = spc(gname, kc)
                S.op("dve", (lambda kc=kc, g=g: lambda e: e.scalar_tensor_tensor(
                    out=ov[:, kc, 0:ntok], in0=hT[:, kc, 0:ntok], scalar=g, in1=t2, op0=ALU.mult, op1=ALU.mult))(),
                    [h_res, t_res, r_const], [ores])

        conv_res = {}

        pending_conv = []

        def convert(name, src_ap, shape, defer=False):
            dstb = dscr(name + "_b", shape, BF16)
            r = S.res("cv_" + name)
            conv_res[name + "_b"] = r
            if len(shape) == 3:
                s2 = src_ap.rearrange("a k o -> (a k) o")
                d2 = dstb.rearrange("a k o -> (a k) o")
                rows = shape[0] * shape[1]
            else:
                s2, d2, rows = src_ap, dstb, shape[0]
            rb = max(128, (2 * 1024 * 1024) // shape[-1])
            for r0 in range(0, rows, rb):
                r1 = min(rows, r0 + rb)
                job = (d2[r0:r1, :], s2[r0:r1, :], r)
                if defer:
                    pending_conv.append(job)
                else:
                    issue_conv(job, ())
            return dstb

        def issue_conv(job, after):
            d, s_, r = job
            S.dma("pool", lambda e: e.dma_start(out=d, in_=s_), reads=list(after), writes=[r])

        def drip_conv(k, after):
            for _ in range(k):
                if pending_conv:
                    issue_conv(pending_conv.pop(0), after)

        def load_w(wslots, Wsrc, kcw, col0, ncols):
            wt, wr = wslots.next()
            wv = wt[:, 0:kcw * ncols].rearrange("p (a b) -> p a b", a=kcw)
            src = Wsrc[:, col0:col0 + ncols].rearrange("(kc p) o -> p kc o", p=128)
            S.dma("sp", lambda e: e.dma_start(out=wv, in_=src), reads=[conv_res[Wsrc.name]], writes=[wr])
            return wv, wr

        def linear(in_fn, in_res, Wsrc, kcw, ncols_total, ntok, wslots, psrot, evac, tile_cols=None, col_base=0):
            if tile_cols is None:
                tile_cols = min(ncols_total, 8192 // kcw)
            for c0 in range(0, ncols_total, tile_cols):
                ncols = min(tile_cols, ncols_total - c0)
                wv, wr = load_w(wslots, Wsrc, kcw, col_base + c0, ncols)
                for o in range(ncols // 128):
                    ps, pr = psrot.next()
                    for kc in range(kcw):
                        mm(ps[:, 0:ntok], wv[:, kc, o * 128:(o + 1) * 128], in_fn(kc), kc == 0, kc == kcw - 1,
                           in_res + [wr], pr)
                    evac((c0 // 128) + o, ps, pr)

        def rope(t, t_res, cs, sn, tab_res, ntok, ps, pr, tmpa, tmpb, tmp_res):
            mm(ps[0:32, 0:ntok], rotb[0:32, 0:32], t[0:32, 0:ntok], True, True, [t_res, r_const], pr)
            a = tmpa[0:32, 0:ntok]
            b = tmpb[0:32, 0:ntok]
            S.op("dve", lambda e: e.tensor_tensor(out=a, in0=ps[0:32, 0:ntok], in1=sn[0:32, 0:ntok], op=ALU.mult),
                 [pr, tab_res], [tmp_res])
            S.op("dve", lambda e: e.tensor_tensor(out=b, in0=t[0:32, 0:ntok], in1=cs[0:32, 0:ntok], op=ALU.mult),
                 [t_res, tab_res], [tmp_res])
            S.op("dve", lambda e: e.tensor_tensor(out=t[0:32, 0:ntok], in0=a, in1=b, op=ALU.add), [tmp_res], [t_res])

        C1 = Carver(PBASE)
        hT = C1.f32(KC * 512).rearrange("p (a b) -> p a b", a=KC)
        uT = C1.bf(KC * 512).rearrange("p (a b) -> p a b", a=KC)
        h_res, u_res = S.res("hT"), S.res("uT")
        xslots = Rot([(C1.f32(2048), S.res("xs%d" % i)) for i in range(4)])
        wkv = C1.bf(KC * 1152).rearrange("p (a b) -> p a b", a=KC)
        wkv_res = S.res("wkv")
        wwi = C1.bf(KC * 8).rearrange("p (a b) -> p a b", a=KC)
        wslots = Rot([(C1.bf(8192), S.res("ws%d" % i)) for i in range(2)])
        tmp1, tmp2 = C1.f32(512), C1.f32(512)
        t_res = S.res("ntmp")
        rtA, rtB = C1.f32(512), C1.f32(512)
        rt_res = S.res("rtmp")
        tabs = Rot([(C1.f32(1024), S.res("tab%d" % i)) for i in range(2)])
        kt_slots = [(C1.bf(512), S.res("kt%d" % i)) for i in range(3)]
        vt_slots = [(C1.bf(512), S.res("vt%d" % i)) for i in range(2)]
        wi_slots = [(C1.f32(8), S.res("wit%d" % i)) for i in range(2)]
        kt_store = [S.res("kst%d" % i) for i in range(3)]
        vt_store = [S.res("vst%d" % i) for i in range(2)]
        wi_store = [S.res("wst%d" % i) for i in range(2)]
        psrot = Rot([(psf[i], psf_res[i]) for i in range(7)])

        for (c0, n, dcol) in ((2048, 1024, 0), (4096, 128, 1024)):
            src = w_in[:, c0:c0 + n].rearrange("(kc p) o -> p kc o", p=128)
            dv = wkv[:, :, dcol:dcol + n]
            S.dma("pool", (lambda dv=dv, src=src: lambda e: e.dma_start(out=dv, in_=src))(), writes=[wkv_res])
        wwi_src = w_in[:, 4224:4232].rearrange("(kc p) o -> p kc o", p=128)
        S.dma("pool", lambda e: e.dma_start(out=wwi, in_=wwi_src), writes=[wkv_res])

        w_in = convert("w_in", w_in, [D, 4232])

        kt_i = [0]
        vt_i = [0]
        wi_i = [0]

        def proj_store(ps, pr, ntok, cs, sn, tab_res, dst_ap, do_rope=True):
            i = kt_i[0] % 3
            kt_i[0] += 1
            kt, kr = kt_slots[i]
            ktv = kt[:, 0:ntok]
            S.op("act", lambda e: e.copy(out=ktv, in_=ps[:, 0:ntok]), [pr], [kr])
            if do_rope:
                ps2, pr2 = psrot.next()
                rope(kt, kr, cs, sn, tab_res, ntok, ps2, pr2, rtA, rtB, rt_res)
            S.dma("pool", lambda e: e.dma_start(out=dst_ap, in_=ktv), reads=[kr], writes=[kt_store[i]])

        def load_tabs(src, tok0, ntok):
            tb, tr_ = tabs.next()
            cs = tb[:, 0:512]
            sn = tb[:, 512:1024]
            S.dma("sp", lambda e: e.dma_start(out=cs[0:32, 0:ntok], in_=src[0, :, tok0:tok0 + ntok]), writes=[tr_])
            S.dma("sp", lambda e: e.dma_start(out=sn[0:32, 0:ntok], in_=src[1, :, tok0:tok0 + ntok]), writes=[tr_])
            return cs, sn, tr_

        for kg in range(cfg.nkg):
            tok0 = kg * 512
            load_xT(xk[tok0:tok0 + 512, :], 512, KC, hT, h_res, xslots, psrot)
            rmsnorm(hT, h_res, "g_mix0", 512, uT, u_res, tmp1, tmp2, t_res, psrot)
            cs, sn, tab_res = load_tabs(ropek, tok0, 512)
            for oc in range(5):
                ps, pr = psrot.next()
                col = oc * 128 if oc < 4 else 1024
                for kc in range(KC):
                    mm(ps[:, 0:512], wkv[:, kc, col:col + 128], uT[:, kc, 0:512], kc == 0, kc == KC - 1,
                       [u_res, wkv_res], pr)
                dst = KTs[oc, :, tok0:tok0 + 512] if oc < 4 else kiTs[:, tok0:tok0 + 512]
                proj_store(ps, pr, 512, cs, sn, tab_res, dst)
            for sub in range(4):
                ps, pr = psrot.next()
                for kc in range(KC):
                    mm(ps[:, 0:512], uT[:, kc, sub * 128:(sub + 1) * 128], wkv[:, kc, 512:1024], kc == 0, kc == KC - 1,
                       [u_res, wkv_res], pr)
                i = vt_i[0] % 2
                vt_i[0] += 1
                vt, vr = vt_slots[i]
                S.op("act", (lambda vt=vt, ps=ps: lambda e: e.copy(out=vt, in_=ps[:, 0:512]))(), [pr], [vr])
                dst = Vs[tok0 + sub * 128:tok0 + (sub + 1) * 128, :]
                S.dma("pool", (lambda dst=dst, vt=vt: lambda e: e.dma_start(out=dst, in_=vt))(), reads=[vr],
                      writes=[vt_store[i]])

        w_out = convert("w_out", w_out, [D, D], defer=True)
        mlp_w1 = convert("mlp_w1", mlp_w1, [2, D, 4 * D], defer=True)
        mlp_w2 = convert("mlp_w2", mlp_w2, [2, 4 * D, D], defer=True)
        pe_gate = convert("pe_gate", pe_gate, [2, D, D], defer=True)
        pe_proj = convert("pe_proj", pe_proj, [2, 256, D], defer=True)
        cw_in = convert("cw_in", cw_in, [D, 2 * D], defer=True)
        cw_out = convert("cw_out", cw_out, [D, D], defer=True)

        groups = []
        t = 0
        while t < NTL:
            s, i = cfg.tiles[t]
            if i < 0:
                groups.append((t, 1))
                t += 1
            else:
                groups.append((t, 4))
                t += 4
        for (t0, ntl) in groups:
            ntok = ntl * 128
            tok0 = t0 * 128
            load_xT(xq[tok0:tok0 + ntok, :], ntok, KC, hT, h_res, xslots, psrot)
            rmsnorm(hT, h_res, "g_mix0", ntok, uT, u_res, tmp1, tmp2, t_res, psrot)
            cs, sn, tab_res = load_tabs(ropeq, tok0, ntok)
            drip_conv(2 if t0 == 0 else 1, [u_res])

            def q_evac(base):
                def ev(oc, ps, pr):
                    proj_store(ps, pr, ntok, cs, sn, tab_res, QTs[:, base + oc, tok0:tok0 + ntok])
                return ev
            inq = lambda kc: uT[:, kc, 0:ntok]
            linear(inq, [u_res], w_in, KC, 2048, ntok, wslots, psrot, q_evac(0), col_base=0)
            linear(inq, [u_res], w_in, KC, 1024, ntok, wslots, psrot, q_evac(16), col_base=3072)
            for sub in range(ntl):
                ps, pr = psrot.next()
                for kc in range(KC):
                    mm(ps[:, 0:8], uT[:, kc, sub * 128:(sub + 1) * 128], wwi[:, kc, 0:8], kc == 0, kc == KC - 1,
                       [u_res, wkv_res], pr)
                i = wi_i[0] % 2
                wi_i[0] += 1
                wt_, wr_ = wi_slots[i]
                S.op("act", (lambda wt_=wt_, ps=ps: lambda e: e.copy(out=wt_, in_=ps[:, 0:8]))(), [pr], [wr_])
                dst = WIs[tok0 + sub * 128:tok0 + (sub + 1) * 128, :]
                S.dma("pool", (lambda dst=dst, wt_=wt_: lambda e: e.dma_start(out=dst, in_=wt_))(), reads=[wr_],
                      writes=[wi_store[i]])
        S.barrier()

        C2 = Carver(PBASE)
        score = C2.f32(SEQ)
        maskq = C2.bf(SEQ)
        score_b = C2.bf(SEQ)
        sb_res = S.res("score_b")
        maskT = C2.bf(SEQ)
        mb_t = C2.f32(2 * cfg.nseg * NB_BIAS * 128).rearrange("p (a b) -> p a b", a=2 * cfg.nseg)
        s_res, mq_res, mt_res, mb_res = S.res("score"), S.res("maskq"), S.res("maskT"), S.res("mb")
        q_slots = Rot([(C2.bf(24 * 128).rearrange("p (a b) -> p a b", a=24), C2.f32(8), S.res("qs%d" % i)) for i in range(2)])
        ki_slots = Rot([(C2.bf(512), S.res("kis%d" % i)) for i in range(3)])
        kv_slots = Rot([(C2.bf(2048).rearrange("p (a b) -> p a b", a=4), C2.bf(2048).rearrange("p (a b) -> p a b", a=4),
                         S.res("kvs%d" % i)) for i in range(3)])
        r_slots = Rot([(C2.f32(512), S.res("rs%d" % i)) for i in range(3)])
        p_slots = Rot([(C2.bf(512), S.res("pt%d" % i)) for i in range(4)])
        sm = C2.f32(64)
        sm_res = S.res("sm")
        rden = C2.f32(512)
        rbc = C2.f32(512)
        rd_res, rb_res = S.res("rden"), S.res("rbc")
        o_slots = [(C2.bf(512), S.res("ot%d" % i)) for i in range(2)]
        o_store = [S.res("ost%d" % i) for i in range(2)]
        o_i = [0]
        WABS, WSGN, AMAX, RNG, LO, MID, CNT, DD, ZERO, KTH = 0, 8, 16, 17, 18, 19, 20, 21, 22, 23
        S.op("dve", lambda e: e.memset(sm[:, ZERO:ZERO + 1], 0.0), [], [sm_res])
        S.op("dve", lambda e: e.memset(sm[:, KTH:KTH + 1], TOPK - 0.5), [sm_res], [sm_res])

        for pi in range(2 * cfg.nseg):
            S.dma("sp", (lambda pi=pi: lambda e: e.dma_start(out=mb_t[:, pi, :], in_=mbias[pi, :, :]))(), writes=[mb_res])

        po = [(psf[i], psf_res[i]) for i in range(4)]
        pd, pd_res = psf[4], psf_res[4]
        ps_s = Rot([(psf[5], psf_res[5]), (psf[6], psf_res[6])])
        ps_all = Rot([(psf[i], psf_res[i]) for i in range(7)])
        scale = 128.0 ** -0.5

        tstate = {}

        def prep_a(t):
            s, ti = cfg.tiles[t]
            n = cfg.nkb[t]
            ncol = n * 128
            pat = 2 * s + (0 if ti < 0 else 1)
            qT, wi_t, q_res = q_slots.next()
            S.dma("sp", (lambda qT=qT, t=t: lambda e: e.dma_start(out=qT, in_=QTs[:, :, t * 128:(t + 1) * 128]))(),
                  writes=[q_res])
            S.dma("sp", (lambda wi_t=wi_t, t=t: lambda e: e.dma_start(out=wi_t, in_=WIs[t * 128:(t + 1) * 128, :]))(),
                  writes=[q_res])
            S.op("dve", (lambda wi_t=wi_t: lambda e: e.tensor_scalar(out=sm[:, 24:32], in0=wi_t, scalar1=-1.0,
                 scalar2=None, op0=ALU.mult))(), [q_res], [sm_res])
            S.op("dve", (lambda wi_t=wi_t: lambda e: e.tensor_tensor(out=sm[:, WABS:WABS + 8], in0=wi_t, in1=sm[:, 24:32],
                 op=ALU.max))(), [q_res, sm_res], [sm_res])
            S.op("dve", (lambda wi_t=wi_t: lambda e: e.tensor_scalar(out=sm[:, WSGN:WSGN + 8], in0=wi_t, scalar1=0.0,
                 scalar2=2.0, op0=ALU.is_ge, op1=ALU.mult))(), [q_res], [sm_res])
            S.op("dve", lambda e: e.tensor_scalar(out=sm[:, WSGN:WSGN + 8], in0=sm[:, WSGN:WSGN + 8], scalar1=-1.0,
                 scalar2=None, op0=ALU.add), [sm_res], [sm_res])
            ngrp = (n + 3) // 4
            for kg in range(ngrp):
                nb = min(4, n - kg * 4)
                cols = nb * 128
                kit, kir = ki_slots.next()
                S.dma("sp", (lambda kit=kit, kg=kg, cols=cols: lambda e: e.dma_start(
                    out=kit[:, 0:cols], in_=kiTs[:, kg * 512:kg * 512 + cols]))(), writes=[kir])
                sv = score[:, kg * 512:kg * 512 + cols]
                for h in range(8):
                    ps, pr = ps_all.next()
                    mm(ps[:, 0:cols], qT[:, 16 + h, :], kit[:, 0:cols], True, True, [q_res, kir], pr)
                    rt, rr = r_slots.next()
                    S.op("act", (lambda rt=rt, ps=ps, cols=cols, h=h: lambda e: e.activation(
                        out=rt[:, 0:cols], in_=ps[:, 0:cols], func=AF.Relu, scale=sm[:, WABS + h:WABS + h + 1]))(),
                        [pr, sm_res], [rr])
                    if h == 0:
                        S.op("dve", (lambda rt=rt, sv=sv, cols=cols: lambda e: e.tensor_scalar(
                            out=sv, in0=rt[:, 0:cols], scalar1=sm[:, WSGN:WSGN + 1], scalar2=None, op0=ALU.mult))(),
                            [rr, sm_res], [s_res])
                    else:
                        S.op("dve", (lambda rt=rt, sv=sv, cols=cols, h=h: lambda e: e.scalar_tensor_tensor(
                            out=sv, in0=rt[:, 0:cols], scalar=sm[:, WSGN + h:WSGN + h + 1], in1=sv,
                            op0=ALU.mult, op1=ALU.add))(), [rr, sm_res, s_res], [s_res])
            sc_v = score[:, 0:ncol]
            S.op("dve", lambda e, sc_v=sc_v: e.tensor_reduce(out=sm[:, AMAX:AMAX + 1], in_=sc_v, axis=AX.X, op=ALU.max,
                 apply_absolute_value=True), [s_res], [sm_res])
            S.op("dve", lambda e: e.tensor_scalar(out=sm[:, RNG:RNG + 1], in0=sm[:, AMAX:AMAX + 1], scalar1=2.0,
                 scalar2=None, op0=ALU.mult), [sm_res], [sm_res])
            S.op("dve", lambda e: e.tensor_scalar(out=sm[:, LO:LO + 1], in0=sm[:, AMAX:AMAX + 1], scalar1=-1.0,
                 scalar2=None, op0=ALU.mult), [sm_res], [sm_res])
            nbb = min(n, NB_BIAS)
            bv = score[:, (n - nbb) * 128:ncol]
            mv = mb_t[:, pat, (NB_BIAS - nbb) * 128:NB_BIAS * 128]
            S.op("dve", (lambda bv=bv, mv=mv: lambda e: e.tensor_tensor(out=bv, in0=bv, in1=mv, op=ALU.add))(),
                 [s_res, mb_res], [s_res])
            mq_v = maskq[:, 0:ncol]
            sc_f = sc_v
            sc_v = score_b[:, 0:ncol]
            S.op("act", (lambda sc_f=sc_f, sc_v=sc_v: lambda e: e.copy(out=sc_v, in_=sc_f))(), [s_res], [sb_res])
            for it in range(cfg.niter):
                ci = 0.5 ** (it + 1)
                S.op("dve", (lambda ci=ci: lambda e: e.scalar_tensor_tensor(
                    out=sm[:, MID:MID + 1], in0=sm[:, RNG:RNG + 1], scalar=ci, in1=sm[:, LO:LO + 1],
                    op0=ALU.mult, op1=ALU.add))(), [sm_res], [sm_res])
                S.op("dve", lambda e: e.memset(sm[:, CNT:CNT + 1], 0.0), [sm_res], [sm_res])
                S.op("dve", (lambda mq_v=mq_v, sc_v=sc_v: lambda e: e.tensor_scalar(
                    out=mq_v, in0=sc_v, scalar1=sm[:, MID:MID + 1], scalar2=sm[:, ZERO:ZERO + 1], op0=ALU.is_ge, op1=ALU.add,
                    accum_out=sm[:, CNT:CNT + 1]))(), [sb_res, sm_res, mq_res], [sm_res, mq_res])
                S.op("dve", lambda e: e.tensor_scalar(out=sm[:, DD:DD + 1], in0=sm[:, CNT:CNT + 1], scalar1=sm[:, KTH:KTH + 1],
                     scalar2=sm[:, RNG:RNG + 1], op0=ALU.is_ge, op1=ALU.mult), [sm_res], [sm_res])
                S.op("dve", (lambda ci=ci: lambda e: e.scalar_tensor_tensor(
                    out=sm[:, LO:LO + 1], in0=sm[:, DD:DD + 1], scalar=ci, in1=sm[:, LO:LO + 1],
                    op0=ALU.mult, op1=ALU.add))(), [sm_res], [sm_res])
            S.op("dve", (lambda mq_v=mq_v, sc_v=sc_v: lambda e: e.tensor_scalar(
                out=mq_v, in0=sc_v, scalar1=sm[:, LO:LO + 1], scalar2=None, op0=ALU.is_ge))(),
                [sb_res, sm_res], [mq_res])
            tstate[t] = (qT, q_res, n)

        def prep_b(t):
            qT, q_res, n = tstate[t]
            for b0 in range(0, n, 8):
                nb = min(8, n - b0)
                pbv = psb[:, 0:nb * 128]
                pbr = psb_res[0]
                for j in range(nb):
                    tr(psb[:, j * 128:(j + 1) * 128], maskq[:, (b0 + j) * 128:(b0 + j + 1) * 128],
                       idb, [mq_res, r_const], pbr)
                mtv = maskT[:, b0 * 128:(b0 + nb) * 128]
                S.op("dve", (lambda mtv=mtv, pbv=pbv: lambda e: e.tensor_scalar(out=mtv, in0=pbv, scalar1=-1.0, scalar2=30000.0,
                     op0=ALU.add, op1=ALU.mult))(), [pbr], [mt_res])

        def attend(t):
            qT, q_res, n = tstate[t]
            drip_conv(1 if t < NTL - 1 else 1000, [q_res])
            state = {"kv": None}

            def emit_qk(blk, g):
                kg, j = blk // 4, blk % 4
                if j == 0 and g == 0:
                    nb = min(4, n - blk)
                    ktt, vtt, kvr = kv_slots.next()
                    S.dma("sp", (lambda ktt=ktt, kg=kg, nb=nb: lambda e: e.dma_start(
                        out=ktt[:, :, 0:nb * 128], in_=KTs[:, :, kg * 512:kg * 512 + nb * 128].rearrange("g d k -> d g k")))(),
                        writes=[kvr])
                    S.dma("sp", (lambda vtt=vtt, kg=kg, nb=nb: lambda e: e.dma_start(
                        out=vtt[:, 0:nb, :], in_=Vs[kg * 512:kg * 512 + nb * 128, :].rearrange("(b p) c -> p b c", p=128)))(),
                        writes=[kvr])
                    state["kv"] = (ktt, vtt, kvr)
                ktt, vtt, kvr = state["kv"]
                mbc = maskT[:, blk * 128:(blk + 1) * 128].unsqueeze(1).to_broadcast([128, 4, 128])
                ps, pr = ps_s.next()
                mm(ps[:, :], ktt[:, g, j * 128:(j + 1) * 128], qT[:, 4 * g:4 * g + 4, :], True, False, [kvr, q_res], pr)
                mm(ps[:, :], idb, mbc, False, True, [mt_res, r_const], pr)
                pt, ptr = p_slots.next()
                S.op("act", (lambda pt=pt, ps=ps: lambda e: e.activation(out=pt, in_=ps[:, :], func=AF.Exp, scale=scale))(),
                     [pr], [ptr])
                return (blk, g, j, vtt, kvr, pt, ptr)

            def emit_pv(blk, g, j, vtt, kvr, pt, ptr):
                mm(po[g][0][:, :], vtt[:, j, g * 128:(g + 1) * 128], pt, blk == 0, blk == n - 1, [kvr, ptr], po[g][1])
                mm(pd[:, :], eqb[:, g, :], pt, blk == 0 and g == 0, blk == n - 1 and g == 3, [ptr, r_const], pd_res)

            pending = None
            for blk in range(n):
                for g in range(4):
                    cur = emit_qk(blk, g)
                    if pending is not None:
                        emit_pv(*pending)
                    pending = cur
            emit_pv(*pending)
            S.op("dve", lambda e: e.reciprocal(out=rden, in_=pd[:, :]), [pd_res], [rd_res])
            for g in range(4):
                ps, pr = ps_s.next()
                mm(ps[:, :], sel[:, g, :], rden, True, True, [rd_res, r_const], pr)
                S.op("act", (lambda ps=ps: lambda e: e.copy(out=rbc, in_=ps[:, :]))(), [pr], [rb_res])
                i = o_i[0] % 2
                o_i[0] += 1
                ot, orr = o_slots[i]
                S.op("dve", (lambda ot=ot, g=g: lambda e: e.tensor_tensor(out=ot, in0=po[g][0][:, :], in1=rbc, op=ALU.mult))(),
                     [po[g][1], rb_res], [orr])
                dst = OTs[:, 4 * g:4 * g + 4, t * 128:(t + 1) * 128]
                S.dma("pool", (lambda dst=dst, ot=ot: lambda e: e.dma_start(
                    out=dst, in_=ot.rearrange("p (a b) -> p a b", a=4)))(), reads=[orr], writes=[o_store[i]])

        prep_a(0)
        prep_b(0)
        for t in range(NTL):
            if t + 1 < NTL:
                prep_a(t + 1)
            attend(t)
            if t + 1 < NTL:
                prep_b(t + 1)
        S.barrier()

        C3 = Carver(PBASE)
        hT = C3.f32(KC * 512).rearrange("p (a b) -> p a b", a=KC)
        uT = C3.bf(KC * 512).rearrange("p (a b) -> p a b", a=KC)
        h_res, u_res = S.res("hT3"), S.res("uT3")
        bigw = 32 * 512 // 2
        big_f = C3.f32(bigw)
        big_res = S.res("big")
        hid = big_f.bitcast(BF16).rearrange("p (a b) -> p a b", a=32)
        aT = big_f[:, 0:4096].bitcast(BF16).rearrange("p (a b) -> p a b", a=KC)
        peT = big_f.rearrange("p (a b) -> p a b", a=KC)
        zT = big_f.rearrange("p (a b) -> p a b", a=KC)
        sT = C3.bf(KC * 512).rearrange("p (a b) -> p a b", a=KC)
        s_res3 = S.res("sT")
        pT = C3.bf(2 * 512).rearrange("p (a b) -> p a b", a=2)
        p_res = S.res("pT")
        YW = 30 + 512
        yT = C3.bf(KC * YW).rearrange("p (a b) -> p a b", a=KC)
        y_res = S.res("yT")
        xslots = Rot([(C3.f32(2048), S.res("xs3_%d" % i)) for i in range(2)])
        wslots = Rot([(C3.bf(8192), S.res("ws3_%d" % i)) for i in range(2)])
        tmp1, tmp2 = C3.f32(512), C3.f32(512)
        t_res = S.res("ntmp3")
        e_slots = Rot([(C3.f32(512), S.res("et%d" % i)) for i in range(3)])
        sig4 = C3.f32(4 * 512).rearrange("p (a b) -> p a b", a=4)
        sig_res = S.res("sig4")
        mean_t, rstd_t = C3.f32(512), C3.f32(512)
        ln_res = S.res("ln")
        out_store = [S.res("outst%d" % i) for i in range(2)]
        rstd_n = C3.f32(512)
        rs_res = S.res("rstd_n")

        def rmsnorm_fast(gname, ntok):
            S.op("act", lambda e: e.activation(out=sT[:, :, 0:ntok], in_=hT[:, :, 0:ntok], func=AF.Square), [h_res], [s_res3])
            for kc in range(KC):
                g = spc(gname, kc)
                S.op("dve", lambda e, kc=kc, g=g: e.tensor_scalar(out=uT[:, kc, 0:ntok], in0=hT[:, kc, 0:ntok], scalar1=g,
                     scalar2=None, op0=ALU.mult), [h_res, r_const], [u_res])
            ps, pr = psrot.next()
            for kc in range(KC):
                mm(ps[:, 0:ntok], onesD, sT[:, kc, 0:ntok], kc == 0, kc == KC - 1, [s_res3, r_const], pr)
            S.op("act", lambda e: e.activation(out=tmp1[:, 0:ntok], in_=ps[:, 0:ntok], func=AF.Sqrt, bias=EPS, scale=1.0),
                 [pr], [t_res])
            S.op("dve", lambda e: e.reciprocal(out=rstd_n[:, 0:ntok], in_=tmp1[:, 0:ntok]), [t_res], [rs_res])

        dg_halves = [(C3.bf(16 * 128).rearrange("p (a b) -> p a b", a=16), S.res("dg%d" % i)) for i in range(2)]
        psrot = Rot([(psf[i], psf_res[i]) for i in range(7)])
        a_store = S.res("aload")

        S.op("dve", lambda e: e.memset(yT[:, :, 0:30], 0.0), [], [y_res])

        def add_res_evac(ntok, bias_name=None):
            def ev(oc, ps, pr):
                if bias_name is None:
                    S.op("dve", lambda e: e.tensor_tensor(out=hT[:, oc, 0:ntok], in0=ps[:, 0:ntok], in1=hT[:, oc, 0:ntok],
                         op=ALU.add), [pr, h_res], [h_res])
                else:
                    b = spc(bias_name, oc)
                    S.op("dve", lambda e: e.scalar_tensor_tensor(out=hT[:, oc, 0:ntok], in0=ps[:, 0:ntok], scalar=b,
                         in1=hT[:, oc, 0:ntok], op0=ALU.add, op1=ALU.add), [pr, h_res, r_const], [h_res])
            return ev

        def mlp(layer, ntok):
            rmsnorm_fast("g_mlp%d" % layer, ntok)
            for half in range(2):
                cnt = [0]

                def hid_evac(oc, ps, pr):
                    et, er = e_slots.next()
                    S.op("act", lambda e: e.activation(out=et[:, 0:ntok], in_=ps[:, 0:ntok], func=AF.Relu), [pr], [er])
                    S.op("dve", lambda e: e.tensor_tensor(out=et[:, 0:ntok], in0=et[:, 0:ntok], in1=rstd_n[:, 0:ntok], op=ALU.mult),
                         [er, rs_res], [er])
                    eng = "dve" if cnt[0] % 2 == 0 else "pool"
                    cnt[0] += 1
                    S.op(eng, lambda e: e.tensor_tensor(out=hid[:, oc, 0:ntok], in0=et[:, 0:ntok], in1=et[:, 0:ntok],
                         op=ALU.mult), [er], [big_res])
                linear(lambda kc: uT[:, kc, 0:ntok], [u_res], mlp_w1[layer], KC, 4096, ntok, wslots, psrot, hid_evac,
                       col_base=half * 4096)
                linear(lambda kc: hid[:, kc, 0:ntok], [big_res], mlp_w2[layer][half * 4096:(half + 1) * 4096, :], 32, D,
                       ntok, wslots, psrot, add_res_evac(ntok))

        def pe_gate_block(layer, ntok, tok0):
            load_xT(pq[layer, tok0:tok0 + ntok, :], ntok, 2, pT, p_res, xslots, psrot)

            def pe_evac(oc, ps, pr):
                S.op("act", lambda e: e.copy(out=peT[:, oc, 0:ntok], in_=ps[:, 0:ntok]), [pr], [big_res])
            linear(lambda kc: pT[:, kc, 0:ntok], [p_res], pe_proj[layer], 2, D, ntok, wslots, psrot, pe_evac)
            rmsnorm_fast("g_gate%d" % layer, ntok)

            def gate_evac(oc, ps, pr):
                et, er = e_slots.next()
                S.op("dve", lambda e: e.tensor_tensor(out=et[:, 0:ntok], in0=ps[:, 0:ntok], in1=rstd_n[:, 0:ntok], op=ALU.mult),
                     [pr, rs_res], [er])
                S.op("act", lambda e: e.activation(out=et[:, 0:ntok], in_=et[:, 0:ntok], func=AF.Sigmoid), [er], [er])
                S.op("dve", lambda e: e.tensor_tensor(out=et[:, 0:ntok], in0=et[:, 0:ntok], in1=peT[:, oc, 0:ntok],
                     op=ALU.mult), [er, big_res], [er])
                S.op("dve", lambda e: e.tensor_tensor(out=hT[:, oc, 0:ntok], in0=et[:, 0:ntok], in1=hT[:, oc, 0:ntok],
                     op=ALU.add), [er, h_res], [h_res])
            linear(lambda kc: uT[:, kc, 0:ntok], [u_res], pe_gate[layer], KC, D, ntok, wslots, psrot, gate_evac)

        def conv_in_glu(ntok):
            rmsnorm_fast("g_mix1", ntok)
            for tq in range(4):
                def g_evac(oc, ps, pr, tq=tq):
                    j = oc - 4 * tq
                    b = spc("b_in", 16 + oc)
                    S.op("dve", lambda e: e.tensor_tensor(out=sig4[:, j, 0:ntok], in0=ps[:, 0:ntok], in1=rstd_n[:, 0:ntok], op=ALU.mult),
                         [pr, rs_res], [sig_res])
                    S.op("act", lambda e: e.activation(out=sig4[:, j, 0:ntok], in_=sig4[:, j, 0:ntok], func=AF.Sigmoid, bias=b),
                         [sig_res, r_const], [sig_res])
                linear(lambda kc: uT[:, kc, 0:ntok], [u_res], cw_in, KC, 512, ntok, wslots, psrot,
                       lambda oc, ps, pr, tq=tq: g_evac(oc + 4 * tq, ps, pr), col_base=2048 + tq * 512)

                def a_evac(oc, ps, pr, tq=tq):
                    j = oc - 4 * tq
                    b = spc("b_in", oc)
                    et, er = e_slots.next()
                    S.op("dve", lambda e: e.tensor_tensor(out=et[:, 0:ntok], in0=ps[:, 0:ntok], in1=rstd_n[:, 0:ntok], op=ALU.mult),
                         [pr, rs_res], [er])
                    S.op("dve", lambda e: e.scalar_tensor_tensor(out=yT[:, oc, 30:30 + ntok], in0=et[:, 0:ntok], scalar=b,
                         in1=sig4[:, j, 0:ntok], op0=ALU.add, op1=ALU.mult), [er, sig_res, r_const], [y_res])
                linear(lambda kc: uT[:, kc, 0:ntok], [u_res], cw_in, KC, 512, ntok, wslots, psrot,
                       lambda oc, ps, pr, tq=tq: a_evac(oc + 4 * tq, ps, pr), col_base=tq * 512)

        def layer0_dense(t0, ntl):
            ntok = ntl * 128
            tok0 = t0 * 128
            load_xT(xq[tok0:tok0 + ntok, :], ntok, KC, hT, h_res, xslots, psrot)
            S.dma("sp", lambda e: e.dma_start(out=aT[:, :, 0:ntok], in_=OTs[:, :, tok0:tok0 + ntok]), writes=[big_res])
            linear(lambda kc: aT[:, kc, 0:ntok], [big_res], w_out, KC, D, ntok, wslots, psrot, add_res_evac(ntok))
            mlp(0, ntok)
            pe_gate_block(0, ntok, tok0)

        otile_i = [0]

        def dense_group(t0, ntl):
            s, i0 = cfg.tiles[t0]
            ntok = ntl * 128
            tok0 = t0 * 128
            layer0_dense(t0, ntl)
            conv_in_glu(ntok)
            if i0 < 0:
                hs = spc("hscale", s)
                S.op("dve", lambda e, hs=hs: e.tensor_scalar(out=yT[:, :, 0:30], in0=yT[:, :, 128:158], scalar1=hs,
                     scalar2=None, op0=ALU.mult), [y_res, r_const], [y_res])
                return
            for c in range(KC):
                ps, pr = psrot.next()
                for hf in range(2):
                    dg, dgr = dg_halves[hf]
                    j0, j1 = (0, 16) if hf == 0 else (16, 31)
                    for j in range(j0, j1):
                        wj = spc("w_dw", c * 31 + j)
                        if j % 2 == 0:
                            S.op("dve", lambda e, dg=dg, j=j, j0=j0, wj=wj: e.tensor_scalar(out=dg[:, j - j0, :], in0=idb, scalar1=wj,
                                 scalar2=None, op0=ALU.mult), [r_const], [dgr])
                        else:
                            S.op("act", lambda e, dg=dg, j=j, j0=j0, wj=wj: e.mul(out=dg[:, j - j0, :], in_=idb, mul=wj),
                                 [r_const], [dgr])
                    for j in range(j0, j1):
                        mm(ps[:, 0:ntok], dg[:, j - j0, :], yT[:, c, j:j + ntok], j == 0, j == 30, [dgr, y_res], pr)
                bdw = spc("b_dw", c)
                S.op("dve", lambda e, c=c, ps=ps, bdw=bdw: e.tensor_scalar(out=zT[:, c, 0:ntok], in0=ps[:, 0:ntok], scalar1=bdw,
                     scalar2=None, op0=ALU.add), [pr, r_const, big_res], [big_res])
            S.op("dve", lambda e: e.tensor_copy(out=yT[:, :, 0:30], in_=yT[:, :, ntok:ntok + 30]), [y_res], [y_res])
            S.op("act", lambda e: e.copy(out=sT[:, :, 0:ntok], in_=zT[:, :, 0:ntok]), [big_res], [s_res3])
            S.op("act", lambda e: e.activation(out=uT[:, :, 0:ntok], in_=zT[:, :, 0:ntok], func=AF.Square), [big_res], [u_res])
            ps, pr = psrot.next()
            for kc in range(KC):
                mm(ps[:, 0:ntok], onesD, sT[:, kc, 0:ntok], kc == 0, kc == KC - 1, [s_res3, r_const], pr)
            ps2, pr2 = psrot.next()
            for kc in range(KC):
                mm(ps2[:, 0:ntok], onesD, uT[:, kc, 0:ntok], kc == 0, kc == KC - 1, [u_res, r_const], pr2)
            S.op("act", lambda e: e.copy(out=mean_t[:, 0:ntok], in_=ps[:, 0:ntok]), [pr], [ln_res])
            S.op("dve", lambda e: e.tensor_tensor(out=tmp1[:, 0:ntok], in0=mean_t[:, 0:ntok], in1=mean_t[:, 0:ntok], op=ALU.mult),
                 [ln_res], [t_res])
            S.op("dve", lambda e: e.tensor_tensor(out=tmp1[:, 0:ntok], in0=ps2[:, 0:ntok], in1=tmp1[:, 0:ntok], op=ALU.subtract),
                 [pr2, t_res], [t_res])
            S.op("dve", lambda e: e.tensor_scalar(out=tmp1[:, 0:ntok], in0=tmp1[:, 0:ntok], scalar1=0.0, scalar2=None,
                 op0=ALU.max), [t_res], [t_res])
            S.op("act", lambda e: e.activation(out=tmp2[:, 0:ntok], in_=tmp1[:, 0:ntok], func=AF.Sqrt, bias=EPS, scale=1.0),
                 [t_res], [t_res])
            S.op("dve", lambda e: e.reciprocal(out=rstd_t[:, 0:ntok], in_=tmp2[:, 0:ntok]), [t_res], [ln_res])
            for c in range(KC):
                et, er = e_slots.next()
                S.op("dve", lambda e, c=c, et=et: e.tensor_tensor(out=et[:, 0:ntok], in0=zT[:, c, 0:ntok], in1=mean_t[:, 0:ntok],
                     op=ALU.subtract), [big_res, ln_res], [er])
                S.op("dve", lambda e, et=et: e.tensor_tensor(out=et[:, 0:ntok], in0=et[:, 0:ntok], in1=rstd_t[:, 0:ntok],
                     op=ALU.mult), [er, ln_res], [er])
                lg, lb = spc("ln_g", c), spc("ln_b", c)
                S.op("act", lambda e, c=c, et=et, lg=lg, lb=lb: e.activation(out=sT[:, c, 0:ntok], in_=et[:, 0:ntok],
                     func=AF.Silu, bias=lb, scale=lg), [er, r_const], [s_res3])
            linear(lambda kc: sT[:, kc, 0:ntok], [s_res3], cw_out, KC, D, ntok, wslots, psrot, add_res_evac(ntok, "b_out"))
            mlp(1, ntok)
            pe_gate_block(1, ntok, tok0)
            outT = peT
            rmsnorm(hT, h_res, "g_final", ntok, uT, u_res, tmp1, tmp2, t_res, psrot, out_view=outT, out_res=big_res)
            seg_out_tile0 = s * cfg.seg_tiles + i0
            for sub in range(ntl):
                xt, xr = xslots.next()
                for d0 in range(0, KC, 4):
                    ps, pr = psrot.next()
                    for j in range(4):
                        tr(ps[:, j * 128:(j + 1) * 128], outT[:, d0 + j, sub * 128:(sub + 1) * 128], idf, [u_res, big_res, r_const], pr)
                    if (d0 // 4) % 2 == 0:
                        S.op("act", lambda e, xt=xt, ps=ps, d0=d0: e.copy(out=xt[:, d0 * 128:(d0 + 4) * 128], in_=ps[:, :]), [pr], [xr])
                    else:
                        S.op("dve", lambda e, xt=xt, ps=ps, d0=d0: e.tensor_copy(out=xt[:, d0 * 128:(d0 + 4) * 128], in_=ps[:, :]), [pr], [xr])
                row0 = (seg_out_tile0 + sub) * 128
                k = otile_i[0] % 2
                otile_i[0] += 1
                S.dma("pool", lambda e, xt=xt, row0=row0: e.dma_start(out=out[row0:row0 + 128, :], in_=xt), reads=[xr],
                      writes=[out_store[k]])

        for (t0, ntl) in groups:
            dense_group(t0, ntl)
        S.wait_all("sp", out_store)
        S.barrier()
        with nc.Block() as block:
            stats = S.finalize(block)
        build_nc.stats = stats
    return nc


def rope_tables(pos):
    half = 16
    inv = (np.float32(500000.0) ** (-np.arange(half, dtype=np.float32) * np.float32(2.0) / np.float32(32))).astype(np.float32)
    ang = pos.astype(np.float32)[None, :] * inv[:, None]
    cs = np.cos(ang).astype(np.float32)
    sn = np.sin(ang).astype(np.float32)
    return np.stack([np.concatenate([cs, cs], 0), np.concatenate([sn, sn], 0)], 0)


def make_inputs(cfg, inp):
    lay, NSP = small_param_layout()
    x = np.asarray(inp["x"], dtype=np.float32)
    p = np.asarray(inp["p"], dtype=np.float32)
    NKEY = cfg.nkg * 512
    ident = np.eye(128, dtype=np.float32)
    rot = np.zeros((32, 32), np.float32)
    for m in range(16):
        rot[m + 16, m] = -1.0
        rot[m, m + 16] = 1.0
    csel = np.zeros((128, 4, 128), np.float32)
    ceq = np.zeros((128, 4, 128), np.float32)
    for g in range(4):
        csel[32 * g, g, :] = 1.0
        ceq[:, g, 32 * g:32 * g + 32] = 1.0
    ropek = rope_tables(np.arange(NKEY))
    shared = {
        "c_ident": ident, "c_rot": rot, "c_sel": csel, "c_eq": ceq, "ropek": ropek,
        "dsa_w_in": np.ascontiguousarray(inp["dsa_w_in"][0]), "dsa_w_out": np.ascontiguousarray(inp["dsa_w_out"][0]),
        "mlp_w1": np.asarray(inp["mlp_w1"]), "mlp_w2": np.asarray(inp["mlp_w2"]),
        "pe_proj": np.asarray(inp["pe_proj"]), "pe_gate": np.asarray(inp["pe_gate"]),
        "conv_w_in": np.ascontiguousarray(inp["conv_w_in"][0]), "conv_w_out": np.ascontiguousarray(inp["conv_w_out"][0]),
    }
    sp_base = np.zeros((128, NSP), np.float32)

    def put(name, arr):
        o, n = lay[name]
        assert arr.shape == (128, n), (name, arr.shape)
        sp_base[:, o:o + n] = arr
    put("g_mix0", fm(inp["mix_norm"][0])); put("g_mlp0", fm(inp["mlp_norm"][0])); put("g_gate0", fm(inp["pe_gate_norm"][0]))
    put("g_mix1", fm(inp["mix_norm"][1])); put("g_mlp1", fm(inp["mlp_norm"][1])); put("g_gate1", fm(inp["pe_gate_norm"][1]))
    put("g_final", fm(inp["final_norm"]))
    put("b_in", fm(inp["conv_b_in"][0])); put("b_dw", fm(inp["conv_b_dw"][0])); put("ln_g", fm(inp["conv_ln_g"][0]))
    put("ln_b", fm(inp["conv_ln_b"][0])); put("b_out", fm(inp["conv_b_out"][0]))
    wdw = np.asarray(inp["conv_w_dw"][0], np.float32)
    wdw_fm = wdw.T.reshape(16, 128, 31).transpose(1, 0, 2).reshape(128, 16 * 31)
    put("w_dw", np.ascontiguousarray(wdw_fm))
    in_maps = []
    tile_blocks = []
    for c in range(8):
        b, half = c // 2, c % 2
        blocks = [cfg.block_of(half, t) for t in range(cfg.ntl)]
        tile_blocks.append(blocks)
        rows = np.concatenate([np.arange(j * 128, (j + 1) * 128) for j in blocks])
        m = dict(shared)
        m["xk"] = np.ascontiguousarray(x[b, 0:NKEY])
        m["xq"] = np.ascontiguousarray(x[b, rows])
        m["pq"] = np.ascontiguousarray(p[:, b][:, rows])
        m["ropeq"] = np.ascontiguousarray(rope_tables(rows))
        mb = np.zeros((2 * cfg.nseg, 128, NB_BIAS * 128), np.float32)
        for t in range(cfg.ntl):
            s, i = cfg.tiles[t]
            pat = 2 * s + (0 if i < 0 else 1)
            if i > 0:
                continue
            n = cfg.nkb[t]
            j = blocks[t]
            dpos = NB_BIAS - (n - j)
            assert 0 <= dpos < NB_BIAS, (c, t, n, j)
            pm = np.zeros((128, NB_BIAS, 128), np.float32)
            pm[:, dpos + 1:, :] = NEG
            pm[0:64, dpos, 64:128] = NEG
            mb[pat] = pm.reshape(128, NB_BIAS * 128)
        m["mbias"] = mb
        sp = sp_base.copy()
        o, _ = lay["hscale"]
        for s in range(cfg.nseg):
            sp[:, o + s] = cfg.halo_scale(half, s)
        m["smallp"] = sp
        in_maps.append(m)
    return in_maps, tile_blocks


_CFG = None


def kernel(**inputs):
    cfg = _CFG or Cfg()
    nc = build_nc(cfg)
    in_maps, tile_blocks = make_inputs(cfg, inputs)
    res = run_bass_kernel_spmd(nc, in_maps, core_ids=list(range(8)))
    B, S_, D_ = inputs["x"].shape
    outp = np.zeros((B, S_, D_), np.float32)
    for c in range(8):
        b = c // 2
        o = np.asarray(res.results[c]["out"])
        k = 0
        for t in range(cfg.ntl):
            s, i = cfg.tiles[t]
            if i < 0:
                continue
            j = tile_blocks[c][t]
            outp[b, j * 128:(j + 1) * 128, :] = o[k * 128:(k + 1) * 128]
            k += 1
    return outp
```

```python
import numpy as np
from contextlib import ExitStack
import concourse.bass as bass
import concourse.mybir as mybir
from concourse.bass_utils import run_bass_kernel_spmd

F32 = mybir.dt.float32
BF16 = mybir.dt.bfloat16
ALU = mybir.AluOpType
AF = mybir.ActivationFunctionType
AX = mybir.AxisListType

D = 2048
KC = 16
SEQ = 8192
NB_BIAS = 17
NEG = -1.0e30
EPS = 1e-6
TOPK = 256.0


class Res:
    __slots__ = ("name", "lw", "rd", "sem", "dcount")

    def __init__(self, name):
        self.name = name
        self.lw = None
        self.rd = []
        self.sem = None
        self.dcount = 0


class Op:
    __slots__ = ("eng", "fn", "deps", "inc", "seq", "dma_res", "dma_val", "idx")

    def __init__(self, eng, fn):
        self.eng = eng
        self.fn = fn
        self.idx = 0
        self.deps = {}
        self.inc = False
        self.seq = 0
        self.dma_res = None
        self.dma_val = 0


ENGS = ("pe", "act", "dve", "pool", "sp")


class Sched:
    def __init__(self, nc, stack):
        self.nc = nc
        self.stack = stack
        self.ops = {e: [] for e in ENGS}
        self.esem = {}
        for e in ("pe", "act", "dve", "pool"):
            self.esem[e] = stack.enter_context(nc.semaphore("sem_" + e))
        self.nres = 0
        self.dma_res = []

    def res(self, name=None):
        self.nres += 1
        return Res(name or ("r%d" % self.nres))

    @staticmethod
    def _add(deps, d):
        if d[0] == "op":
            key = d[1].eng
            cur = deps.get(key)
            if cur is None or cur[1].idx < d[1].idx:
                deps[key] = d
        else:
            key = d[1]
            cur = deps.get(key)
            if cur is None or cur[2] < d[2]:
                deps[key] = d

    def _collect(self, op, reads, writes):
        deps = op.deps
        for r in reads:
            if r.lw is not None:
                self._add(deps, r.lw)
        for w in writes:
            if w.lw is not None:
                self._add(deps, w.lw)
            for d in w.rd:
                self._add(deps, d)

    def op(self, eng, fn, reads=(), writes=()):
        o = Op(eng, fn)
        self._collect(o, reads, writes)
        me = ("op", o)
        for r in reads:
            r.rd = [d for d in r.rd if not (d[0] == "op" and d[1].eng == eng)]
            r.rd.append(me)
        for w in writes:
            w.lw = me
            w.rd = []
        o.idx = len(self.ops[eng])
        self.ops[eng].append(o)
        return o

    def dma(self, queue, fn, reads=(), writes=()):
        o = Op(queue, fn)
        self._collect(o, reads, writes)
        pr = writes[0]
        if pr.sem is None:
            pr.sem = self.stack.enter_context(self.nc.semaphore("d_" + pr.name))
            self.dma_res.append(pr)
        pr.dcount += 16
        o.dma_res = pr
        o.dma_val = pr.dcount
        me = ("dma", pr, pr.dcount)
        for r in reads:
            r.rd.append(me)
        for w in writes:
            w.lw = me
            w.rd = []
        o.idx = len(self.ops[queue])
        self.ops[queue].append(o)
        return o

    def wait_all(self, eng, reads):
        o = Op(eng, None)
        self._collect(o, reads, ())
        o.idx = len(self.ops[eng])
        self.ops[eng].append(o)
        return o

    def barrier(self):
        deps = []
        for e in ("pe", "act", "dve", "pool"):
            if self.ops[e]:
                for o in reversed(self.ops[e]):
                    if o.fn is not None and o.dma_res is None:
                        deps.append(("op", o))
                        break
        for r in self.dma_res:
            deps.append(("dma", r, r.dcount))
        for e in ENGS:
            o = Op(e, None)
            for d in deps:
                self._add(o.deps, d)
            o.idx = len(self.ops[e])
            self.ops[e].append(o)

    def finalize(self, block):
        for e in ENGS:
            for o in self.ops[e]:
                for d in o.deps.values():
                    if d[0] == "op":
                        p = d[1]
                        if p.eng == "pe" and o.eng == "pe":
                            continue
                        p.inc = True
        for e in ENGS:
            n = 0
            for o in self.ops[e]:
                if o.inc:
                    n += 1
                    o.seq = n
        stats = {}

        def emit(engname, engobj):
            seen = {}
            nw = 0
            for o in self.ops[engname]:
                for d in o.deps.values():
                    if d[0] == "op":
                        p = d[1]
                        if p.eng == "pe" and engname == "pe":
                            continue
                        sem = self.esem[p.eng]
                        val = p.seq
                        key = p.eng
                    else:
                        sem = d[1].sem
                        val = d[2]
                        key = d[1]
                    if seen.get(key, 0) >= val:
                        continue
                    seen[key] = val
                    engobj.wait_ge(sem, val)
                    nw += 1
                if o.fn is None:
                    continue
                ins = o.fn(engobj)
                if o.dma_res is not None:
                    ins.then_inc(o.dma_res.sem, 16)
                elif o.inc:
                    ins.then_inc(self.esem[engname], 1)
            stats[engname] = (len(self.ops[engname]), nw)

        @block.tensor
        def _(e):
            emit("pe", e)

        @block.scalar
        def _(e):
            emit("act", e)

        @block.vector
        def _(e):
            emit("dve", e)

        @block.gpsimd
        def _(e):
            emit("pool", e)

        @block.sync
        def _(e):
            emit("sp", e)

        return stats


class Cfg:
    def __init__(self, seg_starts=((0, 48), (16, 32)), seg_tiles=16, nkg=16, niter=16):
        self.seg_starts = seg_starts
        self.nseg = len(seg_starts[0])
        self.seg_tiles = seg_tiles
        self.nkg = nkg
        self.niter = niter
        self.tiles = []
        for s in range(self.nseg):
            self.tiles.append((s, -1))
            for i in range(seg_tiles):
                self.tiles.append((s, i))
        self.ntl = len(self.tiles)
        self.nkb = []
        for (s, i) in self.tiles:
            m = max(seg_starts[0][s], seg_starts[1][s])
            self.nkb.append(max(m + i + 1, 1) if i >= 0 else max(m, 1))
        assert max(self.nkb) <= nkg * 4

    def block_of(self, half, t):
        s, i = self.tiles[t]
        st = self.seg_starts[half][s]
        if i >= 0:
            return st + i
        return st - 1 if st > 0 else 0

    def halo_scale(self, half, s):
        return 1.0 if self.seg_starts[half][s] > 0 else 0.0


def small_param_layout():
    lay = {}
    off = 0
    for nm, n in (("g_mix0", 16), ("g_mlp0", 16), ("g_gate0", 16), ("g_mix1", 16), ("g_mlp1", 16),
                  ("g_gate1", 16), ("g_final", 16), ("b_in", 32), ("b_dw", 16), ("ln_g", 16),
                  ("ln_b", 16), ("b_out", 16), ("w_dw", 16 * 31), ("hscale", 2)):
        lay[nm] = (off, n)
        off += n
    return lay, off


def fm(v):
    v = np.asarray(v, dtype=np.float32)
    return np.ascontiguousarray(v.reshape(-1, 128).T)


def build_nc(cfg):
    nc = bass.Bass("TRN2", target_bir_lowering=False)
    NTL = cfg.ntl
    NTOK = NTL * 128
    NOUT = cfg.nseg * cfg.seg_tiles * 128
    NKEY = cfg.nkg * 512
    lay, NSP = small_param_layout()

    def din(name, shape):
        return nc.dram_tensor(name, list(shape), F32, kind="ExternalInput").ap()

    xk = din("xk", [NKEY, D])
    xq = din("xq", [NTOK, D])
    pq = din("pq", [2, NTOK, 256])
    ropek = din("ropek", [2, 32, NKEY])
    ropeq = din("ropeq", [2, 32, NTOK])
    mbias = din("mbias", [2 * cfg.nseg, 128, NB_BIAS * 128])
    c_ident = din("c_ident", [128, 128])
    c_rot = din("c_rot", [32, 32])
    c_sel = din("c_sel", [128, 4, 128])
    c_eq = din("c_eq", [128, 4, 128])
    smallp = din("smallp", [128, NSP])
    w_in = din("dsa_w_in", [D, 4232])
    w_out = din("dsa_w_out", [D, D])
    mlp_w1 = din("mlp_w1", [2, D, 4 * D])
    mlp_w2 = din("mlp_w2", [2, 4 * D, D])
    pe_proj = din("pe_proj", [2, 256, D])
    pe_gate = din("pe_gate", [2, D, D])
    cw_in = din("conv_w_in", [D, 2 * D])
    cw_out = din("conv_w_out", [D, D])
    out = nc.dram_tensor("out", [NOUT, D], F32, kind="ExternalOutput").ap()

    def dscr(name, shape, dt):
        return nc.dram_tensor(name, list(shape), dt, kind="Internal").ap()

    KTs = dscr("KTs", [4, 128, NKEY], BF16)
    kiTs = dscr("kiTs", [128, NKEY], BF16)
    Vs = dscr("Vs", [NKEY, 512], BF16)
    QTs = dscr("QTs", [128, 24, NTOK], BF16)
    WIs = dscr("WIs", [NTOK, 8], F32)
    OTs = dscr("OTs", [128, 16, NTOK], BF16)

    st = ExitStack()
    with st:
        S = Sched(nc, st)
        ARENA = 52400
        arena = st.enter_context(nc.sbuf_tensor("arena", [128, ARENA], F32))
        psf = [st.enter_context(nc.psum_tensor("psf%d" % i, [128, 512], F32)) for i in range(7)]
        psb = st.enter_context(nc.psum_tensor("psb", [128, 1024], BF16))
        psf_res = [S.res("psf%d" % i) for i in range(7)]
        psb_res = [S.res("psbA"), S.res("psbB")]

        class Carver:
            def __init__(self, base):
                self.pos = base

            def f32(self, n):
                a = arena[:, self.pos:self.pos + n]
                self.pos += n
                assert self.pos <= ARENA, self.pos
                return a

            def bf(self, n):
                w = (n + 1) // 2
                a = arena[:, self.pos:self.pos + w].bitcast(BF16)
                self.pos += w
                assert self.pos <= ARENA, self.pos
                return a

        C0 = Carver(0)
        idf = C0.f32(128)
        idb = C0.bf(128)
        rotb = C0.bf(32)
        sel = C0.f32(512).rearrange("p (a b) -> p a b", a=4)
        eqb = C0.bf(512).rearrange("p (a b) -> p a b", a=4)
        sp_t = C0.f32(NSP)
        onesD = C0.bf(128)
        r_const = S.res("const")
        S.dma("sp", lambda e: e.dma_start(out=idf, in_=c_ident[:, :]), writes=[r_const])
        S.dma("sp", lambda e: e.dma_start(out=sel, in_=c_sel[:, :, :]), writes=[r_const])
        S.dma("sp", lambda e: e.dma_start(out=sp_t, in_=smallp[:, :]), writes=[r_const])
        r_constp = S.res("constp")
        S.dma("pool", lambda e: e.dma_start(out=idb, in_=c_ident[:, :]), writes=[r_constp])
        S.dma("pool", lambda e: e.dma_start(out=rotb[0:32, :], in_=c_rot[:, :]), writes=[r_constp])
        S.dma("pool", lambda e: e.dma_start(out=eqb, in_=c_eq[:, :, :]), writes=[r_constp])
        S.op("dve", lambda e: e.memset(onesD, 1.0 / D), writes=[r_const])
        S.barrier()
        PBASE = C0.pos

        def spc(name, j=0, n=1):
            o, _ = lay[name]
            return sp_t[:, o + j:o + j + n]

        class Rot:
            def __init__(self, items):
                self.items = items
                self.i = 0

            def next(self):
                it = self.items[self.i % len(self.items)]
                self.i += 1
                return it

        def mm(ps_ap, lhsT, rhs, start, stop, reads, wres):
            S.op("pe", lambda e: e.matmul(ps_ap, lhsT=lhsT, rhs=rhs, start=start, stop=stop), reads, [wres])

        def tr(ps_ap, in_ap, ident, reads, wres):
            S.op("pe", lambda e: e.transpose(ps_ap, in_ap, ident), reads, [wres])

        def load_xT(src_rows, ntok, kcx, dst, dst_res, xslots, psrot, out_bf=False):
            nsub = ntok // 128
            cnt = 0
            for sub in range(nsub):
                xt, xr = xslots.next()
                xv = xt[:, 0:kcx * 128]
                rows = src_rows[sub * 128:(sub + 1) * 128, :]
                S.dma("sp", (lambda xv=xv, rows=rows: lambda e: e.dma_start(out=xv, in_=rows))(), writes=[xr])
                for k0 in range(0, kcx, 4):
                    nk = min(4, kcx - k0)
                    ps, pr = psrot.next()
                    for j in range(nk):
                        tr(ps[:, j * 128:(j + 1) * 128], xv[:, (k0 + j) * 128:(k0 + j + 1) * 128], idf, [xr, r_const], pr)
                    src = ps[:, 0:nk * 128].rearrange("p (a b) -> p a b", a=nk)
                    dv = dst[:, k0:k0 + nk, sub * 128:(sub + 1) * 128]
                    if cnt % 2 == 0:
                        S.op("act", (lambda dv=dv, src=src: lambda e: e.copy(out=dv, in_=src))(), [pr], [dst_res])
                    else:
                        S.op("dve", (lambda dv=dv, src=src: lambda e: e.tensor_copy(out=dv, in_=src))(), [pr], [dst_res])
                    cnt += 1

        def rmsnorm(hT, h_res, gname, ntok, uT, u_res, tmp1, tmp2, t_res, psrot, out_view=None, out_res=None):
            hv = hT[:, :, 0:ntok]
            uv = uT[:, :, 0:ntok]
            S.op("act", lambda e: e.activation(out=uv, in_=hv, func=AF.Square), [h_res], [u_res])
            ps, pr = psrot.next()
            for kc in range(KC):
                mm(ps[:, 0:ntok], onesD, uT[:, kc, 0:ntok], kc == 0, kc == KC - 1, [u_res, r_const], pr)
            t1 = tmp1[:, 0:ntok]
            t2 = tmp2[:, 0:ntok]
            S.op("act", lambda e: e.activation(out=t1, in_=ps[:, 0:ntok], func=AF.Sqrt, bias=EPS, scale=1.0), [pr], [t_res])
            S.op("dve", lambda e: e.reciprocal(out=t2, in_=t1), [t_res], [t_res])
            ov = out_view if out_view is not None else uT
            ores = out_res if out_res is not None else u_res
            for kc in range(KC):
                g = spc(gname, kc)
                S.op("dve", (lambda kc=kc, g=g: lambda e: e.scalar_tensor_tensor(
                    out=ov[:, kc, 0:ntok], in0=hT[:, kc, 0:ntok], scalar=g, in1=t2, op0=ALU.mult, op1=ALU.mult))(),
                    [h_res, t_res, r_const], [ores])

        conv_res = {}

        pending_conv = []

        def convert(name, src_ap, shape, defer=False):
            dstb = dscr(name + "_b", shape, BF16)
            r = S.res("cv_" + name)
            conv_res[name + "_b"] = r
            if len(shape) == 3:
                s2 = src_ap.rearrange("a k o -> (a k) o")
                d2 = dstb.rearrange("a k o -> (a k) o")
                rows = shape[0] * shape[1]
            else:
                s2, d2, rows = src_ap, dstb, shape[0]
            rb = max(128, (2 * 1024 * 1024) // shape[-1])
            for r0 in range(0, rows, rb):
                r1 = min(rows, r0 + rb)
                job = (d2[r0:r1, :], s2[r0:r1, :], r)
                if defer:
                    pending_conv.append(job)
                else:
                    issue_conv(job, ())
            return dstb

        def issue_conv(job, after):
            d, s_, r = job
            S.dma("pool", lambda e: e.dma_start(out=d, in_=s_), reads=list(after), writes=[r])

        def drip_conv(k, after):
            for _ in range(k):
                if pending_conv:
                    issue_conv(pending_conv.pop(0), after)

        def load_w(wslots, Wsrc, kcw, col0, ncols):
            wt, wr = wslots.next()
            wv = wt[:, 0:kcw * ncols].rearrange("p (a b) -> p a b", a=kcw)
            src = Wsrc[:, col0:col0 + ncols].rearrange("(kc p) o -> p kc o", p=128)
            S.dma("sp", lambda e: e.dma_start(out=wv, in_=src), reads=[conv_res[Wsrc.name]], writes=[wr])
            return wv, wr

        def linear(in_fn, in_res, Wsrc, kcw, ncols_total, ntok, wslots, psrot, evac, tile_cols=None, col_base=0):
            if tile_cols is None:
                tile_cols = min(ncols_total, 8192 // kcw)
            for c0 in range(0, ncols_total, tile_cols):
                ncols = min(tile_cols, ncols_total - c0)
                wv, wr = load_w(wslots, Wsrc, kcw, col_base + c0, ncols)
                for o in range(ncols // 128):
                    ps, pr = psrot.next()
                    for kc in range(kcw):
                        mm(ps[:, 0:ntok], wv[:, kc, o * 128:(o + 1) * 128], in_fn(kc), kc == 0, kc == kcw - 1,
                           in_res + [wr], pr)
                    evac((c0 // 128) + o, ps, pr)

        def rope(t, t_res, cs, sn, tab_res, ntok, ps, pr, tmpa, tmpb, tmp_res):
            mm(ps[0:32, 0:ntok], rotb[0:32, 0:32], t[0:32, 0:ntok], True, True, [t_res, r_const], pr)
            a = tmpa[0:32, 0:ntok]
            b = tmpb[0:32, 0:ntok]
            S.op("dve", lambda e: e.tensor_tensor(out=a, in0=ps[0:32, 0:ntok], in1=sn[0:32, 0:ntok], op=ALU.mult),
                 [pr, tab_res], [tmp_res])
            S.op("dve", lambda e: e.tensor_tensor(out=b, in0=t[0:32, 0:ntok], in1=cs[0:32, 0:ntok], op=ALU.mult),
                 [t_res, tab_res], [tmp_res])
            S.op("dve", lambda e: e.tensor_tensor(out=t[0:32, 0:ntok], in0=a, in1=b, op=ALU.add), [tmp_res], [t_res])

        C1 = Carver(PBASE)
        hT = C1.f32(KC * 512).rearrange("p (a b) -> p a b", a=KC)
        uT = C1.bf(KC * 512).rearrange("p (a b) -> p a b", a=KC)
        h_res, u_res = S.res("hT"), S.res("uT")
        xslots = Rot([(C1.f32(2048), S.res("xs%d" % i)) for i in range(4)])
        wkv = C1.bf(KC * 1152).rearrange("p (a b) -> p a b", a=KC)
        wkv_res = S.res("wkv")
        wwi = C1.bf(KC * 8).rearrange("p (a b) -> p a b", a=KC)
        wslots = Rot([(C1.bf(8192), S.res("ws%d" % i)) for i in range(2)])
        tmp1, tmp2 = C1.f32(512), C1.f32(512)
        t_res = S.res("ntmp")
        rtA, rtB = C1.f32(512), C1.f32(512)
        rt_res = S.res("rtmp")
        tabs = Rot([(C1.f32(1024), S.res("tab%d" % i)) for i in range(2)])
        kt_slots = [(C1.bf(512), S.res("kt%d" % i)) for i in range(3)]
        vt_slots = [(C1.bf(512), S.res("vt%d" % i)) for i in range(2)]
        wi_slots = [(C1.f32(8), S.res("wit%d" % i)) for i in range(2)]
        kt_store = [S.res("kst%d" % i) for i in range(3)]
        vt_store = [S.res("vst%d" % i) for i in range(2)]
        wi_store = [S.res("wst%d" % i) for i in range(2)]
        psrot = Rot([(psf[i], psf_res[i]) for i in range(7)])

        for (c0, n, dcol) in ((2048, 1024, 0), (4096, 128, 1024)):
            src = w_in[:, c0:c0 + n].rearrange("(kc p) o -> p kc o", p=128)
            dv = wkv[:, :, dcol:dcol + n]
            S.dma("pool", (lambda dv=dv, src=src: lambda e: e.dma_start(out=dv, in_=src))(), writes=[wkv_res])
        wwi_src = w_in[:, 4224:4232].rearrange("(kc p) o -> p kc o", p=128)
        S.dma("pool", lambda e: e.dma_start(out=wwi, in_=wwi_src), writes=[wkv_res])

        w_in = convert("w_in", w_in, [D, 4232])

        kt_i = [0]
        vt_i = [0]
        wi_i = [0]

        def proj_store(ps, pr, ntok, cs, sn, tab_res, dst_ap, do_rope=True):
            i = kt_i[0] % 3
            kt_i[0] += 1
            kt, kr = kt_slots[i]
            ktv = kt[:, 0:ntok]
            S.op("act", lambda e: e.copy(out=ktv, in_=ps[:, 0:ntok]), [pr], [kr])
            if do_rope:
                ps2, pr2 = psrot.next()
                rope(kt, kr, cs, sn, tab_res, ntok, ps2, pr2, rtA, rtB, rt_res)
            S.dma("pool", lambda e: e.dma_start(out=dst_ap, in_=ktv), reads=[kr], writes=[kt_store[i]])

        def load_tabs(src, tok0, ntok):
            tb, tr_ = tabs.next()
            cs = tb[:, 0:512]
            sn = tb[:, 512:1024]
            S.dma("sp", lambda e: e.dma_start(out=cs[0:32, 0:ntok], in_=src[0, :, tok0:tok0 + ntok]), writes=[tr_])
            S.dma("sp", lambda e: e.dma_start(out=sn[0:32, 0:ntok], in_=src[1, :, tok0:tok0 + ntok]), writes=[tr_])
            return cs, sn, tr_

        for kg in range(cfg.nkg):
            tok0 = kg * 512
            load_xT(xk[tok0:tok0 + 512, :], 512, KC, hT, h_res, xslots, psrot)
            rmsnorm(hT, h_res, "g_mix0", 512, uT, u_res, tmp1, tmp2, t_res, psrot)
            cs, sn, tab_res = load_tabs(ropek, tok0, 512)
            for oc in range(5):
                ps, pr = psrot.next()
                col = oc * 128 if oc < 4 else 1024
                for kc in range(KC):
                    mm(ps[:, 0:512], wkv[:, kc, col:col + 128], uT[:, kc, 0:512], kc == 0, kc == KC - 1,
                       [u_res, wkv_res], pr)
                dst = KTs[oc, :, tok0:tok0 + 512] if oc < 4 else kiTs[:, tok0:tok0 + 512]
                proj_store(ps, pr, 512, cs, sn, tab_res, dst)
            for sub in range(4):
                ps, pr = psrot.next()
                for kc in range(KC):
                    mm(ps[:, 0:512], uT[:, kc, sub * 128:(sub + 1) * 128], wkv[:, kc, 512:1024], kc == 0, kc == KC - 1,
                       [u_res, wkv_res], pr)
                i = vt_i[0] % 2
                vt_i[0] += 1
                vt, vr = vt_slots[i]
                S.op("act", (lambda vt=vt, ps=ps: lambda e: e.copy(out=vt, in_=ps[:, 0:512]))(), [pr], [vr])
                dst = Vs[tok0 + sub * 128:tok0 + (sub + 1) * 128, :]
                S.dma("pool", (lambda dst=dst, vt=vt: lambda e: e.dma_start(out=dst, in_=vt))(), reads=[vr],
                      writes=[vt_store[i]])

        w_out = convert("w_out", w_out, [D, D], defer=True)
        mlp_w1 = convert("mlp_w1", mlp_w1, [2, D, 4 * D], defer=True)
        mlp_w2 = convert("mlp_w2", mlp_w2, [2, 4 * D, D], defer=True)
        pe_gate = convert("pe_gate", pe_gate, [2, D, D], defer=True)
        pe_proj = convert("pe_proj", pe_proj, [2, 256, D], defer=True)
        cw_in = convert("cw_in", cw_in, [D, 2 * D], defer=True)
        cw_out = convert("cw_out", cw_out, [D, D], defer=True)

        groups = []
        t = 0
        while t < NTL:
            s, i = cfg.tiles[t]
            if i < 0:
                groups.append((t, 1))
                t += 1
            else:
                groups.append((t, 4))
                t += 4
        for (t0, ntl) in groups:
            ntok = ntl * 128
            tok0 = t0 * 128
            load_xT(xq[tok0:tok0 + ntok, :], ntok, KC, hT, h_res, xslots, psrot)
            rmsnorm(hT, h_res, "g_mix0", ntok, uT, u_res, tmp1, tmp2, t_res, psrot)
            cs, sn, tab_res = load_tabs(ropeq, tok0, ntok)
            drip_conv(2 if t0 == 0 else 1, [u_res])

            def q_evac(base):
                def ev(oc, ps, pr):
                    proj_store(ps, pr, ntok, cs, sn, tab_res, QTs[:, base + oc, tok0:tok0 + ntok])
                return ev
            inq = lambda kc: uT[:, kc, 0:ntok]
            linear(inq, [u_res], w_in, KC, 2048, ntok, wslots, psrot, q_evac(0), col_base=0)
            linear(inq, [u_res], w_in, KC, 1024, ntok, wslots, psrot, q_evac(16), col_base=3072)
            for sub in range(ntl):
                ps, pr = psrot.next()
                for kc in range(KC):
                    mm(ps[:, 0:8], uT[:, kc, sub * 128:(sub + 1) * 128], wwi[:, kc, 0:8], kc == 0, kc == KC - 1,
                       [u_res, wkv_res], pr)
                i = wi_i[0] % 2
                wi_i[0] += 1
                wt_, wr_ = wi_slots[i]
                S.op("act", (lambda wt_=wt_, ps=ps: lambda e: e.copy(out=wt_, in_=ps[:, 0:8]))(), [pr], [wr_])
                dst = WIs[tok0 + sub * 128:tok0 + (sub + 1) * 128, :]
                S.dma("pool", (lambda dst=dst, wt_=wt_: lambda e: e.dma_start(out=dst, in_=wt_))(), reads=[wr_],
                      writes=[wi_store[i]])
        S.barrier()

        C2 = Carver(PBASE)
        score = C2.f32(SEQ)
        maskq = C2.bf(SEQ)
        score_b = C2.bf(SEQ)
        sb_res = S.res("score_b")
        maskT = C2.bf(SEQ)
        mb_t = C2.f32(2 * cfg.nseg * NB_BIAS * 128).rearrange("p (a b) -> p a b", a=2 * cfg.nseg)
        s_res, mq_res, mt_res, mb_res = S.res("score"), S.res("maskq"), S.res("maskT"), S.res("mb")
        q_slots = Rot([(C2.bf(24 * 128).rearrange("p (a b) -> p a b", a=24), C2.f32(8), S.res("qs%d" % i)) for i in range(2)])
        ki_slots = Rot([(C2.bf(512), S.res("kis%d" % i)) for i in range(3)])
        kv_slots = Rot([(C2.bf(2048).rearrange("p (a b) -> p a b", a=4), C2.bf(2048).rearrange("p (a b) -> p a b", a=4),
                         S.res("kvs%d" % i)) for i in range(3)])
        r_slots = Rot([(C2.f32(512), S.res("rs%d" % i)) for i in range(3)])
        p_slots = Rot([(C2.bf(512), S.res("pt%d" % i)) for i in range(4)])
        sm = C2.f32(64)
        sm_res = S.res("sm")
        rden = C2.f32(512)
        rbc = C2.f32(512)
        rd_res, rb_res = S.res("rden"), S.res("rbc")
        o_slots = [(C2.bf(512), S.res("ot%d" % i)) for i in range(2)]
        o_store = [S.res("ost%d" % i) for i in range(2)]
        o_i = [0]
        WABS, WSGN, AMAX, RNG, LO, MID, CNT, DD, ZERO, KTH = 0, 8, 16, 17, 18, 19, 20, 21, 22, 23
        S.op("dve", lambda e: e.memset(sm[:, ZERO:ZERO + 1], 0.0), [], [sm_res])
        S.op("dve", lambda e: e.memset(sm[:, KTH:KTH + 1], TOPK - 0.5), [sm_res], [sm_res])

        for pi in range(2 * cfg.nseg):
            S.dma("sp", (lambda pi=pi: lambda e: e.dma_start(out=mb_t[:, pi, :], in_=mbias[pi, :, :]))(), writes=[mb_res])

        po = [(psf[i], psf_res[i]) for i in range(4)]
        pd, pd_res = psf[4], psf_res[4]
        ps_s = Rot([(psf[5], psf_res[5]), (psf[6], psf_res[6])])
        ps_all = Rot([(psf[i], psf_res[i]) for i in range(7)])
        scale = 128.0 ** -0.5

        tstate = {}

        def prep_a(t):
            s, ti = cfg.tiles[t]
            n = cfg.nkb[t]
            ncol = n * 128
            pat = 2 * s + (0 if ti < 0 else 1)
            qT, wi_t, q_res = q_slots.next()
            S.dma("sp", (lambda qT=qT, t=t: lambda e: e.dma_start(out=qT, in_=QTs[:, :, t * 128:(t + 1) * 128]))(),
                  writes=[q_res])
            S.dma("sp", (lambda wi_t=wi_t, t=t: lambda e: e.dma_start(out=wi_t, in_=WIs[t * 128:(t + 1) * 128, :]))(),
                  writes=[q_res])
            S.op("dve", (lambda wi_t=wi_t: lambda e: e.tensor_scalar(out=sm[:, 24:32], in0=wi_t, scalar1=-1.0,
                 scalar2=None, op0=ALU.mult))(), [q_res], [sm_res])
            S.op("dve", (lambda wi_t=wi_t: lambda e: e.tensor_tensor(out=sm[:, WABS:WABS + 8], in0=wi_t, in1=sm[:, 24:32],
                 op=ALU.max))(), [q_res, sm_res], [sm_res])
            S.op("dve", (lambda wi_t=wi_t: lambda e: e.tensor_scalar(out=sm[:, WSGN:WSGN + 8], in0=wi_t, scalar1=0.0,
                 scalar2=2.0, op0=ALU.is_ge, op1=ALU.mult))(), [q_res], [sm_res])
            S.op("dve", lambda e: e.tensor_scalar(out=sm[:, WSGN:WSGN + 8], in0=sm[:, WSGN:WSGN + 8], scalar1=-1.0,
                 scalar2=None, op0=ALU.add), [sm_res], [sm_res])
            ngrp = (n + 3) // 4
            for kg in range(ngrp):
                nb = min(4, n - kg * 4)
                cols = nb * 128
                kit, kir = ki_slots.next()
                S.dma("sp", (lambda kit=kit, kg=kg, cols=cols: lambda e: e.dma_start(
                    out=kit[:, 0:cols], in_=kiTs[:, kg * 512:kg * 512 + cols]))(), writes=[kir])
                sv = score[:, kg * 512:kg * 512 + cols]
                for h in range(8):
                    ps, pr = ps_all.next()
                    mm(ps[:, 0:cols], qT[:, 16 + h, :], kit[:, 0:cols], True, True, [q_res, kir], pr)
                    rt, rr = r_slots.next()
                    S.op("act", (lambda rt=rt, ps=ps, cols=cols, h=h: lambda e: e.activation(
                        out=rt[:, 0:cols], in_=ps[:, 0:cols], func=AF.Relu, scale=sm[:, WABS + h:WABS + h + 1]))(),
                        [pr, sm_res], [rr])
                    if h == 0:
                        S.op("dve", (lambda rt=rt, sv=sv, cols=cols: lambda e: e.tensor_scalar(
                            out=sv, in0=rt[:, 0:cols], scalar1=sm[:, WSGN:WSGN + 1], scalar2=None, op0=ALU.mult))(),
                            [rr, sm_res], [s_res])
                    else:
                        S.op("dve", (lambda rt=rt, sv=sv, cols=cols, h=h: lambda e: e.scalar_tensor_tensor(
                            out=sv, in0=rt[:, 0:cols], scalar=sm[:, WSGN + h:WSGN + h + 1], in1=sv,
                            op0=ALU.mult, op1=ALU.add))(), [rr, sm_res, s_res], [s_res])
            sc_v = score[:, 0:ncol]
            S.op("dve", lambda e, sc_v=sc_v: e.tensor_reduce(out=sm[:, AMAX:AMAX + 1], in_=sc_v, axis=AX.X, op=ALU.max,
                 apply_absolute_value=True), [s_res], [sm_res])
            S.op("dve", lambda e: e.tensor_scalar(out=sm[:, RNG:RNG + 1], in0=sm[:, AMAX:AMAX + 1], scalar1=2.0,
                 scalar2=None, op0=ALU.mult), [sm_res], [sm_res])
            S.op("dve", lambda e: e.tensor_scalar(out=sm[:, LO:LO + 1], in0=sm[:, AMAX:AMAX + 1], scalar1=-1.0,
                 scalar2=None, op0=ALU.mult), [sm_res], [sm_res])
            nbb = min(n, NB_BIAS)
            bv = score[:, (n - nbb) * 128:ncol]
            mv = mb_t[:, pat, (NB_BIAS - nbb) * 128:NB_BIAS * 128]
            S.op("dve", (lambda bv=bv, mv=mv: lambda e: e.tensor_tensor(out=bv, in0=bv, in1=mv, op=ALU.add))(),
                 [s_res, mb_res], [s_res])
            mq_v = maskq[:, 0:ncol]
            sc_f = sc_v
            sc_v = score_b[:, 0:ncol]
            S.op("act", (lambda sc_f=sc_f, sc_v=sc_v: lambda e: e.copy(out=sc_v, in_=sc_f))(), [s_res], [sb_res])
            for it in range(cfg.niter):
                ci = 0.5 ** (it + 1)
                S.op("dve", (lambda ci=ci: lambda e: e.scalar_tensor_tensor(
                    out=sm[:, MID:MID + 1], in0=sm[:, RNG:RNG + 1], scalar=ci, in1=sm[:, LO:LO + 1],
                    op0=ALU.mult, op1=ALU.add))(), [sm_res], [sm_res])
                S.op("dve", lambda e: e.memset(sm[:, CNT:CNT + 1], 0.0), [sm_res], [sm_res])
                S.op("dve", (lambda mq_v=mq_v, sc_v=sc_v: lambda e: e.tensor_scalar(
                    out=mq_v, in0=sc_v, scalar1=sm[:, MID:MID + 1], scalar2=sm[:, ZERO:ZERO + 1], op0=ALU.is_ge, op1=ALU.add,
                    accum_out=sm[:, CNT:CNT + 1]))(), [sb_res, sm_res, mq_res], [sm_res, mq_res])
                S.op("dve", lambda e: e.tensor_scalar(out=sm[:, DD:DD + 1], in0=sm[:, CNT:CNT + 1], scalar1=sm[:, KTH:KTH + 1],
                     scalar2=sm[:, RNG:RNG + 1], op0=ALU.is_ge, op1=ALU.mult), [sm_res], [sm_res])
                S.op("dve", (lambda ci=ci: lambda e: e.scalar_tensor_tensor(
                    out=sm[:, LO:LO + 1], in0=sm[:, DD:DD + 1], scalar=ci, in1=sm[:, LO:LO + 1],
                    op0=ALU.mult, op1=ALU.add))(), [sm_res], [sm_res])
            S.op("dve", (lambda mq_v=mq_v, sc_v=sc_v: lambda e: e.tensor_scalar(
                out=mq_v, in0=sc_v, scalar1=sm[:, LO:LO + 1], scalar2=None, op0=ALU.is_ge))(),
                [sb_res, sm_res], [mq_res])
            tstate[t] = (qT, q_res, n)

        def prep_b(t):
            qT, q_res, n = tstate[t]
            for b0 in range(0, n, 8):
                nb = min(8, n - b0)
                pbv = psb[:, 0:nb * 128]
                pbr = psb_res[0]
                for j in range(nb):
                    tr(psb[:, j * 128:(j + 1) * 128], maskq[:, (b0 + j) * 128:(b0 + j + 1) * 128],
                       idb, [mq_res, r_const], pbr)
                mtv = maskT[:, b0 * 128:(b0 + nb) * 128]
                S.op("dve", (lambda mtv=mtv, pbv=pbv: lambda e: e.tensor_scalar(out=mtv, in0=pbv, scalar1=-1.0, scalar2=30000.0,
                     op0=ALU.add, op1=ALU.mult))(), [pbr], [mt_res])

        def attend(t):
            qT, q_res, n = tstate[t]
            drip_conv(1 if t < NTL - 1 else 1000, [q_res])
            state = {"kv": None}

            def emit_qk(blk, g):
                kg, j = blk // 4, blk % 4
                if j == 0 and g == 0:
                    nb = min(4, n - blk)
                    ktt, vtt, kvr = kv_slots.next()
                    S.dma("sp", (lambda ktt=ktt, kg=kg, nb=nb: lambda e: e.dma_start(
                        out=ktt[:, :, 0:nb * 128], in_=KTs[:, :, kg * 512:kg * 512 + nb * 128].rearrange("g d k -> d g k")))(),
                        writes=[kvr])
                    S.dma("sp", (lambda vtt=vtt, kg=kg, nb=nb: lambda e: e.dma_start(
                        out=vtt[:, 0:nb, :], in_=Vs[kg * 512:kg * 512 + nb * 128, :].rearrange("(b p) c -> p b c", p=128)))(),
                        writes=[kvr])
                    state["kv"] = (ktt, vtt, kvr)
                ktt, vtt, kvr = state["kv"]
                mbc = maskT[:, blk * 128:(blk + 1) * 128].unsqueeze(1).to_broadcast([128, 4, 128])
                ps, pr = ps_s.next()
                mm(ps[:, :], ktt[:, g, j * 128:(j + 1) * 128], qT[:, 4 * g:4 * g + 4, :], True, False, [kvr, q_res], pr)
                mm(ps[:, :], idb, mbc, False, True, [mt_res, r_const], pr)
                pt, ptr = p_slots.next()
                S.op("act", (lambda pt=pt, ps=ps: lambda e: e.activation(out=pt, in_=ps[:, :], func=AF.Exp, scale=scale))(),
                     [pr], [ptr])
                return (blk, g, j, vtt, kvr, pt, ptr)

            def emit_pv(blk, g, j, vtt, kvr, pt, ptr):
                mm(po[g][0][:, :], vtt[:, j, g * 128:(g + 1) * 128], pt, blk == 0, blk == n - 1, [kvr, ptr], po[g][1])
                mm(pd[:, :], eqb[:, g, :], pt, blk == 0 and g == 0, blk == n - 1 and g == 3, [ptr, r_const], pd_res)

            pending = None
            for blk in range(n):
                for g in range(4):
                    cur = emit_qk(blk, g)
                    if pending is not None:
                        emit_pv(*pending)
                    pending = cur
            emit_pv(*pending)
            S.op("dve", lambda e: e.reciprocal(out=rden, in_=pd[:, :]), [pd_res], [rd_res])
            for g in range(4):
                ps, pr = ps_s.next()
                mm(ps[:, :], sel[:, g, :], rden, True, True, [rd_res, r_const], pr)
                S.op("act", (lambda ps=ps: lambda e: e.copy(out=rbc, in_=ps[:, :]))(), [pr], [rb_res])
                i = o_i[0] % 2
                o_i[0] += 1
                ot, orr = o_slots[i]
                S.op("dve", (lambda ot=ot, g=g: lambda e: e.tensor_tensor(out=ot, in0=po[g][0][:, :], in1=rbc, op=ALU.mult))(),
                     [po[g][1], rb_res], [orr])
                dst = OTs[:, 4 * g:4 * g + 4, t * 128:(t + 1) * 128]
                S.dma("pool", (lambda dst=dst, ot=ot: lambda e: e.dma_start(
                    out=dst, in_=ot.rearrange("p (a b) -> p a b", a=4)))(), reads=[orr], writes=[o_store[i]])

        prep_a(0)
        prep_b(0)
        for t in range(NTL):
            if t + 1 < NTL:
                prep_a(t + 1)
            attend(t)
            if t + 1 < NTL:
                prep_b(t + 1)
        S.barrier()

        C3 = Carver(PBASE)
        hT = C3.f32(KC * 512).rearrange("p (a b) -> p a b", a=KC)
        uT = C3.bf(KC * 512).rearrange("p (a b) -> p a b", a=KC)
        h_res, u_res = S.res("hT3"), S.res("uT3")
        bigw = 32 * 512 // 2
        big_f = C3.f32(bigw)
        big_res = S.res("big")
        hid = big_f.bitcast(BF16).rearrange("p (a b) -> p a b", a=32)
        aT = big_f[:, 0:4096].bitcast(BF16).rearrange("p (a b) -> p a b", a=KC)
        peT = big_f.rearrange("p (a b) -> p a b", a=KC)
        zT = big_f.rearrange("p (a b) -> p a b", a=KC)
        sT = C3.bf(KC * 512).rearrange("p (a b) -> p a b", a=KC)
        s_res3 = S.res("sT")
        pT = C3.bf(2 * 512).rearrange("p (a b) -> p a b", a=2)
        p_res = S.res("pT")
        YW = 30 + 512
        yT = C3.bf(KC * YW).rearrange("p (a b) -> p a b", a=KC)
        y_res = S.res("yT")
        xslots = Rot([(C3.f32(2048), S.res("xs3_%d" % i)) for i in range(2)])
        wslots = Rot([(C3.bf(8192), S.res("ws3_%d" % i)) for i in range(2)])
        tmp1, tmp2 = C3.f32(512), C3.f32(512)
        t_res = S.res("ntmp3")
        e_slots = Rot([(C3.f32(512), S.res("et%d" % i)) for i in range(3)])
        sig4 = C3.f32(4 * 512).rearrange("p (a b) -> p a b", a=4)
        sig_res = S.res("sig4")
        mean_t, rstd_t = C3.f32(512), C3.f32(512)
        ln_res = S.res("ln")
        out_store = [S.res("outst%d" % i) for i in range(2)]
        rstd_n = C3.f32(512)
        rs_res = S.res("rstd_n")

        def rmsnorm_fast(gname, ntok):
            S.op("act", lambda e: e.activation(out=sT[:, :, 0:ntok], in_=hT[:, :, 0:ntok], func=AF.Square), [h_res], [s_res3])
            for kc in range(KC):
                g = spc(gname, kc)
                S.op("dve", lambda e, kc=kc, g=g: e.tensor_scalar(out=uT[:, kc, 0:ntok], in0=hT[:, kc, 0:ntok], scalar1=g,
                     scalar2=None, op0=ALU.mult), [h_res, r_const], [u_res])
            ps, pr = psrot.next()
            for kc in range(KC):
                mm(ps[:, 0:ntok], onesD, sT[:, kc, 0:ntok], kc == 0, kc == KC - 1, [s_res3, r_const], pr)
            S.op("act", lambda e: e.activation(out=tmp1[:, 0:ntok], in_=ps[:, 0:ntok], func=AF.Sqrt, bias=EPS, scale=1.0),
                 [pr], [t_res])
            S.op("dve", lambda e: e.reciprocal(out=rstd_n[:, 0:ntok], in_=tmp1[:, 0:ntok]), [t_res], [rs_res])

        dg_halves = [(C3.bf(16 * 128).rearrange("p (a b) -> p a b", a=16), S.res("dg%d" % i)) for i in range(2)]
        dg_tap_res = [S.res("dgt%d" % j) for j in range(31)]
        psrot = Rot([(psf[i], psf_res[i]) for i in range(7)])
        a_store = S.res("aload")

        S.op("dve", lambda e: e.memset(yT[:, :, 0:30], 0.0), [], [y_res])

        def add_res_evac(ntok, bias_name=None):
            def ev(oc, ps, pr):
                if bias_name is None:
                    S.op("dve", lambda e: e.tensor_tensor(out=hT[:, oc, 0:ntok], in0=ps[:, 0:ntok], in1=hT[:, oc, 0:ntok],
                         op=ALU.add), [pr, h_res], [h_res])
                else:
                    b = spc(bias_name, oc)
                    S.op("dve", lambda e: e.scalar_tensor_tensor(out=hT[:, oc, 0:ntok], in0=ps[:, 0:ntok], scalar=b,
                         in1=hT[:, oc, 0:ntok], op0=ALU.add, op1=ALU.add), [pr, h_res, r_const], [h_res])
            return ev

        def mlp(layer, ntok):
            rmsnorm_fast("g_mlp%d" % layer, ntok)
            for half in range(2):
                cnt = [0]

                def hid_evac(oc, ps, pr):
                    et, er = e_slots.next()
                    S.op("act", lambda e: e.activation(out=et[:, 0:ntok], in_=ps[:, 0:ntok], func=AF.Relu), [pr], [er])
                    S.op("dve", lambda e: e.tensor_tensor(out=et[:, 0:ntok], in0=et[:, 0:ntok], in1=rstd_n[:, 0:ntok], op=ALU.mult),
                         [er, rs_res], [er])
                    eng = "dve" if cnt[0] % 2 == 0 else "pool"
                    cnt[0] += 1
                    S.op(eng, lambda e: e.tensor_tensor(out=hid[:, oc, 0:ntok], in0=et[:, 0:ntok], in1=et[:, 0:ntok],
                         op=ALU.mult), [er], [big_res])
                linear(lambda kc: uT[:, kc, 0:ntok], [u_res], mlp_w1[layer], KC, 4096, ntok, wslots, psrot, hid_evac,
                       col_base=half * 4096)
                linear(lambda kc: hid[:, kc, 0:ntok], [big_res], mlp_w2[layer][half * 4096:(half + 1) * 4096, :], 32, D,
                       ntok, wslots, psrot, add_res_evac(ntok))

        def pe_gate_block(layer, ntok, tok0):
            load_xT(pq[layer, tok0:tok0 + ntok, :], ntok, 2, pT, p_res, xslots, psrot)

            def pe_evac(oc, ps, pr):
                S.op("act", lambda e: e.copy(out=peT[:, oc, 0:ntok], in_=ps[:, 0:ntok]), [pr], [big_res])
            linear(lambda kc: pT[:, kc, 0:ntok], [p_res], pe_proj[layer], 2, D, ntok, wslots, psrot, pe_evac)
            rmsnorm_fast("g_gate%d" % layer, ntok)

            def gate_evac(oc, ps, pr):
                et, er = e_slots.next()
                S.op("dve", lambda e: e.tensor_tensor(out=et[:, 0:ntok], in0=ps[:, 0:ntok], in1=rstd_n[:, 0:ntok], op=ALU.mult),
                     [pr, rs_res], [er])
                S.op("act", lambda e: e.activation(out=et[:, 0:ntok], in_=et[:, 0:ntok], func=AF.Sigmoid), [er], [er])
                S.op("dve", lambda e: e.tensor_tensor(out=et[:, 0:ntok], in0=et[:, 0:ntok], in1=peT[:, oc, 0:ntok],
                     op=ALU.mult), [er, big_res], [er])
                S.op("dve", lambda e: e.tensor_tensor(out=hT[:, oc, 0:ntok], in0=et[:, 0:ntok], in1=hT[:, oc, 0:ntok],
                     op=ALU.add), [er, h_res], [h_res])
            linear(lambda kc: uT[:, kc, 0:ntok], [u_res], pe_gate[layer], KC, D, ntok, wslots, psrot, gate_evac)

        def conv_in_glu(ntok):
            rmsnorm_fast("g_mix1", ntok)
            for tq in range(4):
                def g_evac(oc, ps, pr, tq=tq):
                    j = oc - 4 * tq
                    b = spc("b_in", 16 + oc)
                    S.op("dve", lambda e: e.tensor_tensor(out=sig4[:, j, 0:ntok], in0=ps[:, 0:ntok], in1=rstd_n[:, 0:ntok], op=ALU.mult),
                         [pr, rs_res], [sig_res])
                    S.op("act", lambda e: e.activation(out=sig4[:, j, 0:ntok], in_=sig4[:, j, 0:ntok], func=AF.Sigmoid, bias=b),
                         [sig_res, r_const], [sig_res])
                linear(lambda kc: uT[:, kc, 0:ntok], [u_res], cw_in, KC, 512, ntok, wslots, psrot,
                       lambda oc, ps, pr, tq=tq: g_evac(oc + 4 * tq, ps, pr), col_base=2048 + tq * 512)

                def a_evac(oc, ps, pr, tq=tq):
                    j = oc - 4 * tq
                    b = spc("b_in", oc)
                    et, er = e_slots.next()
                    S.op("dve", lambda e: e.tensor_tensor(out=et[:, 0:ntok], in0=ps[:, 0:ntok], in1=rstd_n[:, 0:ntok], op=ALU.mult),
                         [pr, rs_res], [er])
                    S.op("dve", lambda e: e.scalar_tensor_tensor(out=yT[:, oc, 30:30 + ntok], in0=et[:, 0:ntok], scalar=b,
                         in1=sig4[:, j, 0:ntok], op0=ALU.add, op1=ALU.mult), [er, sig_res, r_const], [y_res])
                linear(lambda kc: uT[:, kc, 0:ntok], [u_res], cw_in, KC, 512, ntok, wslots, psrot,
                       lambda oc, ps, pr, tq=tq: a_evac(oc + 4 * tq, ps, pr), col_base=tq * 512)

        def layer0_dense(t0, ntl):
            ntok = ntl * 128
            tok0 = t0 * 128
            load_xT(xq[tok0:tok0 + ntok, :], ntok, KC, hT, h_res, xslots, psrot)
            S.dma("sp", lambda e: e.dma_start(out=aT[:, :, 0:ntok], in_=OTs[:, :, tok0:tok0 + ntok]), writes=[big_res])
            linear(lambda kc: aT[:, kc, 0:ntok], [big_res], w_out, KC, D, ntok, wslots, psrot, add_res_evac(ntok))
            mlp(0, ntok)
            pe_gate_block(0, ntok, tok0)

        otile_i = [0]

        def dense_group(t0, ntl):
            s, i0 = cfg.tiles[t0]
            ntok = ntl * 128
            tok0 = t0 * 128
            layer0_dense(t0, ntl)
            conv_in_glu(ntok)
            if i0 < 0:
                hs = spc("hscale", s)
                S.op("dve", lambda e, hs=hs: e.tensor_scalar(out=yT[:, :, 0:30], in0=yT[:, :, 128:158], scalar1=hs,
                     scalar2=None, op0=ALU.mult), [y_res, r_const], [y_res])
                return
            for c in range(KC):
                ps, pr = psrot.next()
                for hf in range(2):
                    dg, _ = dg_halves[hf]
                    j0, j1 = (0, 16) if hf == 0 else (16, 31)
                    for j in range(j0, j1):
                        wj = spc("w_dw", c * 31 + j)
                        tr_ = dg_tap_res[j]
                        if hf == 0:
                            S.op("dve", lambda e, dg=dg, j=j, j0=j0, wj=wj: e.tensor_scalar(out=dg[:, j - j0, :], in0=idb, scalar1=wj,
                                 scalar2=None, op0=ALU.mult), [r_const], [tr_])
                        else:
                            S.op("act", lambda e, dg=dg, j=j, j0=j0, wj=wj: e.mul(out=dg[:, j - j0, :], in_=idb, mul=wj),
                                 [r_const], [tr_])
                for hf in range(2):
                    dg, _ = dg_halves[hf]
                    j0, j1 = (0, 16) if hf == 0 else (16, 31)
                    for j in range(j0, j1):
                        mm(ps[:, 0:ntok], dg[:, j - j0, :], yT[:, c, j:j + ntok], j == 0, j == 30, [dg_tap_res[j], y_res], pr)
                bdw = spc("b_dw", c)
                S.op("dve", lambda e, c=c, ps=ps, bdw=bdw: e.tensor_scalar(out=zT[:, c, 0:ntok], in0=ps[:, 0:ntok], scalar1=bdw,
                     scalar2=None, op0=ALU.add), [pr, r_const, big_res], [big_res])
            S.op("dve", lambda e: e.tensor_copy(out=yT[:, :, 0:30], in_=yT[:, :, ntok:ntok + 30]), [y_res], [y_res])
            S.op("act", lambda e: e.copy(out=sT[:, :, 0:ntok], in_=zT[:, :, 0:ntok]), [big_res], [s_res3])
            S.op("act", lambda e: e.activation(out=uT[:, :, 0:ntok], in_=zT[:, :, 0:ntok], func=AF.Square), [big_res], [u_res])
            ps, pr = psrot.next()
            for kc in range(KC):
                mm(ps[:, 0:ntok], onesD, sT[:, kc, 0:ntok], kc == 0, kc == KC - 1, [s_res3, r_const], pr)
            ps2, pr2 = psrot.next()
            for kc in range(KC):
                mm(ps2[:, 0:ntok], onesD, uT[:, kc, 0:ntok], kc == 0, kc == KC - 1, [u_res, r_const], pr2)
            S.op("act", lambda e: e.copy(out=mean_t[:, 0:ntok], in_=ps[:, 0:ntok]), [pr], [ln_res])
            S.op("dve", lambda e: e.tensor_tensor(out=tmp1[:, 0:ntok], in0=mean_t[:, 0:ntok], in1=mean_t[:, 0:ntok], op=ALU.mult),
                 [ln_res], [t_res])
            S.op("dve", lambda e: e.tensor_tensor(out=tmp1[:, 0:ntok], in0=ps2[:, 0:ntok], in1=tmp1[:, 0:ntok], op=ALU.subtract),
                 [pr2, t_res], [t_res])
            S.op("dve", lambda e: e.tensor_scalar(out=tmp1[:, 0:ntok], in0=tmp1[:, 0:ntok], scalar1=0.0, scalar2=None,
                 op0=ALU.max), [t_res], [t_res])
            S.op("act", lambda e: e.activation(out=tmp2[:, 0:ntok], in_=tmp1[:, 0:ntok], func=AF.Sqrt, bias=EPS, scale=1.0),
                 [t_res], [t_res])
            S.op("dve", lambda e: e.reciprocal(out=rstd_t[:, 0:ntok], in_=tmp2[:, 0:ntok]), [t_res], [ln_res])
            for c in range(KC):
                et, er = e_slots.next()
                S.op("dve", lambda e, c=c, et=et: e.tensor_tensor(out=et[:, 0:ntok], in0=zT[:, c, 0:ntok], in1=mean_t[:, 0:ntok],
                     op=ALU.subtract), [big_res, ln_res], [er])
                S.op("dve", lambda e, et=et: e.tensor_tensor(out=et[:, 0:ntok], in0=et[:, 0:ntok], in1=rstd_t[:, 0:ntok],
                     op=ALU.mult), [er, ln_res], [er])
                lg, lb = spc("ln_g", c), spc("ln_b", c)
                S.op("act", lambda e, c=c, et=et, lg=lg, lb=lb: e.activation(out=sT[:, c, 0:ntok], in_=et[:, 0:ntok],
                     func=AF.Silu, bias=lb, scale=lg), [er, r_const], [s_res3])
            linear(lambda kc: sT[:, kc, 0:ntok], [s_res3], cw_out, KC, D, ntok, wslots, psrot, add_res_evac(ntok, "b_out"))
            mlp(1, ntok)
            pe_gate_block(1, ntok, tok0)
            outT = peT
            rmsnorm(hT, h_res, "g_final", ntok, uT, u_res, tmp1, tmp2, t_res, psrot, out_view=outT, out_res=big_res)
            seg_out_tile0 = s * cfg.seg_tiles + i0
            for sub in range(ntl):
                xt, xr = xslots.next()
                for d0 in range(0, KC, 4):
                    ps, pr = psrot.next()
                    for j in range(4):
                        tr(ps[:, j * 128:(j + 1) * 128], outT[:, d0 + j, sub * 128:(sub + 1) * 128], idf, [u_res, big_res, r_const], pr)
                    if (d0 // 4) % 2 == 0:
                        S.op("act", lambda e, xt=xt, ps=ps, d0=d0: e.copy(out=xt[:, d0 * 128:(d0 + 4) * 128], in_=ps[:, :]), [pr], [xr])
                    else:
                        S.op("dve", lambda e, xt=xt, ps=ps, d0=d0: e.tensor_copy(out=xt[:, d0 * 128:(d0 + 4) * 128], in_=ps[:, :]), [pr], [xr])
                row0 = (seg_out_tile0 + sub) * 128
                k = otile_i[0] % 2
                otile_i[0] += 1
                S.dma("pool", lambda e, xt=xt, row0=row0: e.dma_start(out=out[row0:row0 + 128, :], in_=xt), reads=[xr],
                      writes=[out_store[k]])

        for (t0, ntl) in groups:
            dense_group(t0, ntl)
        S.wait_all("sp", out_store)
        S.barrier()
        with nc.Block() as block:
            stats = S.finalize(block)
        build_nc.stats = stats
    return nc


def rope_tables(pos):
    half = 16
    inv = (np.float32(500000.0) ** (-np.arange(half, dtype=np.float32) * np.float32(2.0) / np.float32(32))).astype(np.float32)
    ang = pos.astype(np.float32)[None, :] * inv[:, None]
    cs = np.cos(ang).astype(np.float32)
    sn = np.sin(ang).astype(np.float32)
    return np.stack([np.concatenate([cs, cs], 0), np.concatenate([sn, sn], 0)], 0)


def make_inputs(cfg, inp):
    lay, NSP = small_param_layout()
    x = np.asarray(inp["x"], dtype=np.float32)
    p = np.asarray(inp["p"], dtype=np.float32)
    NKEY = cfg.nkg * 512
    ident = np.eye(128, dtype=np.float32)
    rot = np.zeros((32, 32), np.float32)
    for m in range(16):
        rot[m + 16, m] = -1.0
        rot[m, m + 16] = 1.0
    csel = np.zeros((128, 4, 128), np.float32)
    ceq = np.zeros((128, 4, 128), np.float32)
    for g in range(4):
        csel[32 * g, g, :] = 1.0
        ceq[:, g, 32 * g:32 * g + 32] = 1.0
    ropek = rope_tables(np.arange(NKEY))
    shared = {
        "c_ident": ident, "c_rot": rot, "c_sel": csel, "c_eq": ceq, "ropek": ropek,
        "dsa_w_in": np.ascontiguousarray(inp["dsa_w_in"][0]), "dsa_w_out": np.ascontiguousarray(inp["dsa_w_out"][0]),
        "mlp_w1": np.asarray(inp["mlp_w1"]), "mlp_w2": np.asarray(inp["mlp_w2"]),
        "pe_proj": np.asarray(inp["pe_proj"]), "pe_gate": np.asarray(inp["pe_gate"]),
        "conv_w_in": np.ascontiguousarray(inp["conv_w_in"][0]), "conv_w_out": np.ascontiguousarray(inp["conv_w_out"][0]),
    }
    sp_base = np.zeros((128, NSP), np.float32)

    def put(name, arr):
        o, n = lay[name]
        assert arr.shape == (128, n), (name, arr.shape)
        sp_base[:, o:o + n] = arr
    put("g_mix0", fm(inp["mix_norm"][0])); put("g_mlp0", fm(inp["mlp_norm"][0])); put("g_gate0", fm(inp["pe_gate_norm"][0]))
    put("g_mix1", fm(inp["mix_norm"][1])); put("g_mlp1", fm(inp["mlp_norm"][1])); put("g_gate1", fm(inp["pe_gate_norm"][1]))
    put("g_final", fm(inp["final_norm"]))
    put("b_in", fm(inp["conv_b_in"][0])); put("b_dw", fm(inp["conv_b_dw"][0])); put("ln_g", fm(inp["conv_ln_g"][0]))
    put("ln_b", fm(inp["conv_ln_b"][0])); put("b_out", fm(inp["conv_b_out"][0]))
    wdw = np.asarray(inp["conv_w_dw"][0], np.float32)
    wdw_fm = wdw.T.reshape(16, 128, 31).transpose(1, 0, 2).reshape(128, 16 * 31)
    put("w_dw", np.ascontiguousarray(wdw_fm))
    in_maps = []
    tile_blocks = []
    for c in range(8):
        b, half = c // 2, c % 2
        blocks = [cfg.block_of(half, t) for t in range(cfg.ntl)]
        tile_blocks.append(blocks)
        rows = np.concatenate([np.arange(j * 128, (j + 1) * 128) for j in blocks])
        m = dict(shared)
        m["xk"] = np.ascontiguousarray(x[b, 0:NKEY])
        m["xq"] = np.ascontiguousarray(x[b, rows])
        m["pq"] = np.ascontiguousarray(p[:, b][:, rows])
        m["ropeq"] = np.ascontiguousarray(rope_tables(rows))
        mb = np.zeros((2 * cfg.nseg, 128, NB_BIAS * 128), np.float32)
        for t in range(cfg.ntl):
            s, i = cfg.tiles[t]
            pat = 2 * s + (0 if i < 0 else 1)
            if i > 0:
                continue
            n = cfg.nkb[t]
            j = blocks[t]
            dpos = NB_BIAS - (n - j)
            assert 0 <= dpos < NB_BIAS, (c, t, n, j)
            pm = np.zeros((128, NB_BIAS, 128), np.float32)
            pm[:, dpos + 1:, :] = NEG
            pm[0:64, dpos, 64:128] = NEG
            mb[pat] = pm.reshape(128, NB_BIAS * 128)
        m["mbias"] = mb
        sp = sp_base.copy()
        o, _ = lay["hscale"]
        for s in range(cfg.nseg):
            sp[:, o + s] = cfg.halo_scale(half, s)
        m["smallp"] = sp
        in_maps.append(m)
    return in_maps, tile_blocks


_CFG = None


def kernel(**inputs):
    cfg = _CFG or Cfg()
    nc = build_nc(cfg)
    in_maps, tile_blocks = make_inputs(cfg, inputs)
    res = run_bass_kernel_spmd(nc, in_maps, core_ids=list(range(8)))
    B, S_, D_ = inputs["x"].shape
    outp = np.zeros((B, S_, D_), np.float32)
    for c in range(8):
        b = c // 2
        o = np.asarray(res.results[c]["out"])
        k = 0
        for t in range(cfg.ntl):
            s, i = cfg.tiles[t]
            if i < 0:
                continue
            j = tile_blocks[c][t]
            outp[b, j * 128:(j + 1) * 128, :] = o[k * 128:(k + 1) * 128]
            k += 1
    return outp
```
